# Optimizing a Trainium2 kernel written in Bass

```python
import math
import jax
import jax.numpy as jnp
from jax import lax
import numpy as np

D_MODEL = 2048
BATCH = 8
SEQ = 2048
DEPTH = 2

GRID_W = 64
CTX_LEN = 256
EPS = 1e-6

A_WIDTH = D_MODEL // 2
A_GROUP_DIM = 128
A_GROUPS = A_WIDTH // A_GROUP_DIM
CHUNK = 128
B_WIDTH = D_MODEL // 2
B_GROUP_DIM = 128
B_GROUPS = B_WIDTH // B_GROUP_DIM
AB_IN = 2 * A_WIDTH + B_WIDTH
AB_OUT = A_WIDTH + B_WIDTH

D_INNER = 2 * D_MODEL
HEADDIM = 64
N_SSD_HEADS = D_INNER // HEADDIM
D_STATE = 128
N_BC_GROUPS = 8
HEADS_PER_GROUP = N_SSD_HEADS // N_BC_GROUPS
D_CONV = 5
SSD_CHUNK = 128
GN = N_BC_GROUPS * D_STATE
CONV_DIM = D_INNER + 2 * GN
SSD_IN = D_INNER + CONV_DIM + 2 * N_SSD_HEADS

N_EXPERTS = 16
D_EXPERT = D_MODEL
CAPACITY_FACTOR = 2

N_EVEN = (DEPTH + 1) // 2
N_ODD = DEPTH // 2

kernel_name = "hybrid_gmlp_fnet_ssd_ecmoe_dit"

F32 = jnp.float32


def rmsnorm(x, g):
    xf = x.astype(F32)
    y = xf * lax.rsqrt(jnp.mean(xf * xf, axis=-1, keepdims=True) + EPS)
    return (y * g.astype(F32)).astype(x.dtype)


def modulate(x, g, shift, scale):
    return rmsnorm(x, g) * (1 + scale) + shift


def adaln(cond, w, b):
    m = jnp.dot(jax.nn.silu(cond), w) + b
    return jnp.split(m[..., None, :], 6, axis=-1)


def sincos_2d(rows, cols, dim):
    quarter = dim // 4
    omega = 1.0 / (10000.0 ** (jnp.arange(quarter, dtype=F32) / quarter))
    r = jnp.repeat(jnp.arange(rows, dtype=F32), cols)[:, None] * omega
    cl = jnp.tile(jnp.arange(cols, dtype=F32), rows)[:, None] * omega
    return jnp.concatenate([jnp.sin(r), jnp.cos(r), jnp.sin(cl), jnp.cos(cl)], axis=-1)


def chunk_gmlp(u, v, v_g, w_s, b_s):
    bsz, t, _ = v.shape
    vn = rmsnorm(v, v_g).reshape(bsz, t // CHUNK, CHUNK, A_GROUPS, A_GROUP_DIM)
    s = jnp.einsum('gij,bcjgd->bcigd', w_s, vn) + b_s.T[:, :, None]
    return u * s.reshape(bsz, t, A_WIDTH)


def fourier_mix(z):
    bsz, t, _ = z.shape
    zg = z.astype(F32).reshape(bsz, t, B_GROUPS, B_GROUP_DIM)
    f = jnp.fft.fft2(zg, axes=(1, 3), norm='ortho').real
    return f.reshape(bsz, t, B_WIDTH).astype(z.dtype)


def mixer_ab(h, w_in, w_out, v_g, w_s, b_s):
    p = jnp.dot(h, w_in)
    a = jax.nn.gelu(p[..., :2 * A_WIDTH], approximate=False)
    ya = chunk_gmlp(a[..., :A_WIDTH], a[..., A_WIDTH:], v_g, w_s, b_s)
    yb = fourier_mix(p[..., 2 * A_WIDTH:])
    return jnp.dot(jnp.concatenate([ya, yb], axis=-1), w_out)


def dwconv(z, w, b):
    ch = z.shape[-1]
    y = lax.conv_general_dilated(z, w[:, None, :].astype(z.dtype), window_strides=(1,),
                                 padding=[(D_CONV // 2, D_CONV // 2)],
                                 dimension_numbers=('NWC', 'WIO', 'NWC'), feature_group_count=ch)
    return jax.nn.silu(y + b)


def ssd_scan(x, dt, a, bm, cm, s0):
    bsz, t = x.shape[:2]
    nc, L = t // SSD_CHUNK, SSD_CHUNK
    xg = (x * dt[..., None]).reshape(bsz, nc, L, N_BC_GROUPS, HEADS_PER_GROUP, HEADDIM)
    cum = jnp.cumsum((dt * a).reshape(bsz, nc, L, N_BC_GROUPS, HEADS_PER_GROUP), axis=2)
    bm = bm.reshape(bsz, nc, L, N_BC_GROUPS, D_STATE)
    cm = cm.reshape(bsz, nc, L, N_BC_GROUPS, D_STATE)
    mask = jnp.tril(jnp.ones((L, L), dtype=bool))[:, :, None, None]
    seg = cum[:, :, :, None] - cum[:, :, None, :]
    decay = jnp.exp(jnp.where(mask, seg, -jnp.inf))
    cb = jnp.einsum('bcign,bcjgn->bcijg', cm, bm)
    y_intra = jnp.einsum('bcijg,bcijgk,bcjgkp->bcigkp', cb, decay, xg)
    decay_end = jnp.exp(cum[:, :, -1:] - cum)
    states = jnp.einsum('bclgn,bclgk,bclgkp->bcgkpn', bm, decay_end, xg)
    chunk_decay = jnp.exp(cum[:, :, -1])

    def step(s, inp):
        st, dec = inp
        return s * dec[..., None, None] + st, s

    s_fin, s_starts = lax.scan(step, s0, (jnp.moveaxis(states, 1, 0), jnp.moveaxis(chunk_decay, 1, 0)))
    s_starts = jnp.moveaxis(s_starts, 0, 1)
    y_inter = jnp.einsum('bclgn,bclgk,bcgkpn->bclgkp', cm, jnp.exp(cum), s_starts)
    return (y_intra + y_inter).reshape(bsz, t, N_SSD_HEADS, HEADDIM), s_fin


def ssd_final_state(x, dt, a, bm):
    bsz, t = x.shape[:2]
    cum = jnp.cumsum(dt * a, axis=1)
    w = jnp.exp(cum[:, -1:] - cum) * dt
    xg = (x * w[..., None]).reshape(bsz, t, N_BC_GROUPS, HEADS_PER_GROUP, HEADDIM)
    return jnp.einsum('btgn,btgkp->bgkpn', bm, xg)


def ssd_mix(h_lat, h_ctx, w_in, conv_w, conv_b, dt_bias, a_log, d_skip, norm_g, w_out, need_ctx_out):
    a = -jnp.exp(a_log.astype(F32))
    flip = lambda q: jnp.flip(q, axis=1)

    def to_dt(raw):
        b_, t_ = raw.shape[:2]
        return jax.nn.softplus(raw.astype(F32).reshape(b_, t_, 2, N_SSD_HEADS) + dt_bias.astype(F32))

    def full(h, s_init):
        b_, t_ = h.shape[:2]
        p = jnp.dot(h, w_in)
        z = p[..., :D_INNER]
        xbc = dwconv(p[..., D_INNER:D_INNER + CONV_DIM], conv_w, conv_b).astype(F32)
        dt = to_dt(p[..., D_INNER + CONV_DIM:])
        xs = xbc[..., :D_INNER].reshape(b_, t_, N_SSD_HEADS, HEADDIM)
        bm = xbc[..., D_INNER:D_INNER + GN].reshape(b_, t_, N_BC_GROUPS, D_STATE)
        cm = xbc[..., D_INNER + GN:].reshape(b_, t_, N_BC_GROUPS, D_STATE)
        y_f, s_f = ssd_scan(xs, dt[:, :, 0], a[0], bm, cm, s_init[0])
        y_b, s_b = ssd_scan(flip(xs), flip(dt[:, :, 1]), a[1], flip(bm), flip(cm), s_init[1])
        y = y_f + flip(y_b) + xs * d_skip.astype(F32)[:, None]
        y = y.reshape(b_, t_, D_INNER) * jax.nn.silu(z.astype(F32))
        return jnp.dot(rmsnorm(y, norm_g).astype(h.dtype), w_out), (s_f, s_b)

    def states_only(h):
        b_, t_ = h.shape[:2]
        w_sel = jnp.concatenate([w_in[:, D_INNER:2 * D_INNER + GN], w_in[:, D_INNER + CONV_DIM:]], axis=1)
        p = jnp.dot(h, w_sel)
        xb = dwconv(p[..., :D_INNER + GN], conv_w[:, :D_INNER + GN], conv_b[:D_INNER + GN]).astype(F32)
        dt = to_dt(p[..., D_INNER + GN:])
        xs = xb[..., :D_INNER].reshape(b_, t_, N_SSD_HEADS, HEADDIM)
        bm = xb[..., D_INNER:].reshape(b_, t_, N_BC_GROUPS, D_STATE)
        s_f = ssd_final_state(xs, dt[:, :, 0], a[0], bm)
        s_b = ssd_final_state(flip(xs), flip(dt[:, :, 1]), a[1], flip(bm))
        return (s_f, s_b)

    if need_ctx_out:
        zero = jnp.zeros((h_ctx.shape[0], N_BC_GROUPS, HEADS_PER_GROUP, HEADDIM, D_STATE), F32)
        y_ctx, s_ctx = full(h_ctx, (zero, zero))
    else:
        y_ctx, s_ctx = None, states_only(h_ctx)
    y_lat, _ = full(h_lat, s_ctx)
    return y_lat, y_ctx


def expert_choice_ffn(h, w_router, w_gate, w_up, w_down):
    bsz, t, _ = h.shape
    cap = CAPACITY_FACTOR * t // N_EXPERTS
    probs = jax.nn.softmax(jnp.dot(h, w_router).astype(F32), axis=-1)
    gate, idx = lax.top_k(jnp.swapaxes(probs, 1, 2), cap)
    bidx = jnp.arange(bsz)[:, None, None]
    xe = h[bidx, idx]
    hid = jax.nn.silu(jnp.einsum('becd,edf->becf', xe, w_gate)) * jnp.einsum('becd,edf->becf', xe, w_up)
    ye = jnp.einsum('becf,efd->becd', hid, w_down) * gate[..., None].astype(h.dtype)
    return jnp.zeros_like(h).at[bidx, idx].add(ye)


def setup_inputs(seed: int = 0):
    key = jax.random.key(seed)
    k = jax.random.split(key, 26)
    nrm = lambda i, shape, s: jax.random.normal(k[i], shape, F32) * s
    dt0 = jnp.exp(jax.random.uniform(k[16], (N_ODD, 2, N_SSD_HEADS), F32, math.log(1e-3), math.log(1e-1)))
    return {
        'x': nrm(0, (BATCH, SEQ, D_MODEL), 1.0),
        'c': nrm(1, (BATCH, D_MODEL), 1.0),
        'ctx': nrm(2, (BATCH, CTX_LEN, D_MODEL), 1.0),
        'c_ctx': nrm(3, (D_MODEL,), 1.0),
        'mod_w': nrm(4, (DEPTH, D_MODEL, 6 * D_MODEL), 0.5 * D_MODEL ** -0.5),
        'mod_b': nrm(5, (DEPTH, 6 * D_MODEL), 0.01),
        'norm_g': 1.0 + nrm(6, (DEPTH, 2, D_MODEL), 0.02),
        'final_g': 1.0 + nrm(7, (D_MODEL,), 0.02),
        'ab_w_in': nrm(8, (N_EVEN, D_MODEL, AB_IN), D_MODEL ** -0.5),
        'ab_w_out': nrm(9, (N_EVEN, AB_OUT, D_MODEL), AB_OUT ** -0.5),
        'gm_v_g': 1.0 + nrm(10, (N_EVEN, A_WIDTH), 0.02),
        'gm_w_s': nrm(11, (N_EVEN, A_GROUPS, CHUNK, CHUNK), CHUNK ** -0.5),
        'gm_b_s': 1.0 + nrm(12, (N_EVEN, A_GROUPS, CHUNK), 0.01),
        'ssd_w_in': nrm(13, (N_ODD, D_MODEL, SSD_IN), D_MODEL ** -0.5),
        'ssd_conv_w': nrm(14, (N_ODD, D_CONV, CONV_DIM), D_CONV ** -0.5),
        'ssd_conv_b': nrm(15, (N_ODD, CONV_DIM), 0.01),
        'ssd_dt_bias': dt0 + jnp.log(-jnp.expm1(-dt0)),
        'ssd_a_log': jnp.log(jax.random.uniform(k[17], (N_ODD, 2, N_SSD_HEADS), F32, 1.0, 16.0)),
        'ssd_d': 1.0 + nrm(18, (N_ODD, N_SSD_HEADS), 0.02),
        'ssd_norm_g': 1.0 + nrm(19, (N_ODD, D_INNER), 0.02),
        'ssd_w_out': nrm(20, (N_ODD, D_INNER, D_MODEL), D_INNER ** -0.5),
        'moe_w_router': nrm(21, (DEPTH, D_MODEL, N_EXPERTS), D_MODEL ** -0.5),
        'moe_w_gate': nrm(22, (DEPTH, N_EXPERTS, D_MODEL, D_EXPERT), D_MODEL ** -0.5),
        'moe_w_up': nrm(23, (DEPTH, N_EXPERTS, D_MODEL, D_EXPERT), D_MODEL ** -0.5),
        'moe_w_down': nrm(24, (DEPTH, N_EXPERTS, D_EXPERT, D_MODEL), D_EXPERT ** -0.5),
    }


def reference(x, c, ctx, c_ctx, mod_w, mod_b, norm_g, final_g, ab_w_in, ab_w_out, gm_v_g, gm_w_s, gm_b_s,
              ssd_w_in, ssd_conv_w, ssd_conv_b, ssd_dt_bias, ssd_a_log, ssd_d, ssd_norm_g, ssd_w_out,
              moe_w_router, moe_w_gate, moe_w_up, moe_w_down):
    n = x.shape[1]
    rows = n // GRID_W
    h = x + sincos_2d(rows, GRID_W, D_MODEL).astype(x.dtype)[None]
    hc = ctx
    for i in range(DEPTH):
        last = i == DEPTH - 1
        even = i % 2 == 0
        j = i // 2
        ctx_reaches_latent = (not last) or (not even)
        sh1, sc1, g1, sh2, sc2, g2 = adaln(c, mod_w[i], mod_b[i])
        xn = modulate(h, norm_g[i, 0], sh1, sc1)
        if ctx_reaches_latent:
            csh1, csc1, cg1, csh2, csc2, cg2 = adaln(c_ctx, mod_w[i], mod_b[i])
            xc = modulate(hc, norm_g[i, 0], csh1, csc1)
        if even:
            y = mixer_ab(xn, ab_w_in[j], ab_w_out[j], gm_v_g[j], gm_w_s[j], gm_b_s[j])
            if not last:
                yc = mixer_ab(xc, ab_w_in[j], ab_w_out[j], gm_v_g[j], gm_w_s[j], gm_b_s[j])
        else:
            y, yc = ssd_mix(xn, xc, ssd_w_in[j], ssd_conv_w[j], ssd_conv_b[j], ssd_dt_bias[j], ssd_a_log[j],
                            ssd_d[j], ssd_norm_g[j], ssd_w_out[j], not last)
        h = h + g1 * y
        h = h + g2 * expert_choice_ffn(modulate(h, norm_g[i, 1], sh2, sc2),
                                       moe_w_router[i], moe_w_gate[i], moe_w_up[i], moe_w_down[i])
        if not last:
            hc = hc + cg1 * yc
            hc = hc + cg2 * expert_choice_ffn(modulate(hc, norm_g[i, 1], csh2, csc2),
                                              moe_w_router[i], moe_w_gate[i], moe_w_up[i], moe_w_down[i])
    return rmsnorm(h, final_g)
```

```python
import os
import numpy as np
import ml_dtypes
from contextlib import ExitStack
import concourse.bass as bass
import concourse.mybir as mybir
from concourse.bass_utils import run_bass_kernel_spmd

F32 = mybir.dt.float32
BF16 = mybir.dt.bfloat16
AF = mybir.ActivationFunctionType
ALU = mybir.AluOpType
AX = mybir.AxisListType

ENGS = ("pe", "act", "dve", "pool", "sp")
NRING = 8

T = 2048
TCX = 256
NTOK = T + TCX
D = 2048
NTC = NTOK // 128
NLAT = T // 128
EPS = 1e-6
NE = 16
CAP = 256
CAPC = 32
NSLOT = CAP + CAPC


class Buf:
    __slots__ = ("lw", "rs")

    def __init__(self):
        self.lw = None
        self.rs = []


class Op:
    __slots__ = ("eng", "fn", "deps", "signal", "count", "dma", "ring", "rval", "phase")


class Sched:
    def __init__(self, nc, sems, rings):
        self.nc = nc
        self.sems = sems
        self.rings = rings
        self.ops = {e: [] for e in ENGS}
        self.ndma = {e: 0 for e in ENGS}
        self.cnt = {e: 0 for e in ENGS}
        self.waited = {e: {} for e in ENGS}
        self.phase = 0
        self.lastring = {}
        self.bufs = {}

    def B(self, *key):
        b = self.bufs.get(key)
        if b is None:
            b = self.bufs[key] = Buf()
        return b

    def op(self, eng, fn, reads=(), writes=(), dma=False):
        o = Op()
        o.eng, o.fn, o.dma, o.signal, o.count, o.phase = eng, fn, dma, False, 0, self.phase
        o.ring = o.rval = 0
        o.deps = []
        seen = set()
        cand = []
        for b in reads:
            if b.lw is not None:
                cand.append(b.lw)
        for b in writes:
            if b.lw is not None:
                cand.append(b.lw)
            cand.extend(b.rs)
        for d in cand:
            if id(d) in seen or d.phase != self.phase:
                continue
            seen.add(id(d))
            if d.eng == eng and eng == "pe" and not d.dma and not dma:
                continue
            o.deps.append(d)
            d.signal = True
        for b in reads:
            b.rs.append(o)
        for b in writes:
            b.lw = o
            b.rs = []
        if dma:
            i = self.ndma[eng]
            self.ndma[eng] += 1
            o.ring = i % NRING
            o.rval = 16 * (i // NRING + 1)
            self.lastring[(eng, o.ring)] = o.rval
        self.ops[eng].append(o)
        return o

    def do(self, eng, meth, reads, writes, *a, **kw):
        return self.op(eng, lambda e: getattr(e, meth)(*a, **kw), reads, writes)

    def dma(self, eng, out, in_, reads, writes):
        return self.op(eng, lambda e: e.dma_start(out=out, in_=in_), reads, writes, dma=True)

    def _wait(self, e, eh, need):
        w = self.waited[e]
        for key, (sem, val) in need.items():
            if w.get(key, 0) >= val:
                continue
            eh.wait_ge(sem, val)
            w[key] = val

    def emit_engine(self, e, eh, final):
        w = self.waited[e]
        for o in self.ops[e]:
            need = {}
            for d in o.deps:
                if d.dma:
                    key, sem, val = ("r", d.eng, d.ring), self.rings[d.eng][d.ring], d.rval
                else:
                    key, sem, val = ("c", d.eng), self.sems[d.eng], d.count
                if key not in need or need[key][1] < val:
                    need[key] = (sem, val)
            if o.dma and o.rval > 16:
                key = ("r", e, o.ring)
                if key not in need or need[key][1] < o.rval - 16:
                    need[key] = (self.rings[e][o.ring], o.rval - 16)
            self._wait(e, eh, need)
            ins = o.fn(eh)
            if o.dma:
                ins.then_inc(self.rings[e][o.ring], 16)
            elif o.signal:
                ins.then_inc(self.sems[e], 1)
        need = {}
        for p in ENGS:
            if final[p] > 0:
                need[("c", p)] = (self.sems[p], final[p])
        for (p, r), val in self.lastring.items():
            need[("r", p, r)] = (self.rings[p][r], val)
        self._wait(e, eh, need)

    def end_phase(self, block):
        final = {}
        for e in ENGS:
            comp = [o for o in self.ops[e] if not o.dma]
            if comp:
                comp[-1].signal = True
            c = self.cnt[e]
            for o in comp:
                if o.signal:
                    c += 1
                    o.count = c
            self.cnt[e] = c
            final[e] = c
        S = self

        @block.tensor
        def _(eh):
            S.emit_engine("pe", eh, final)

        @block.scalar
        def _(eh):
            S.emit_engine("act", eh, final)

        @block.vector
        def _(eh):
            S.emit_engine("dve", eh, final)

        @block.gpsimd
        def _(eh):
            S.emit_engine("pool", eh, final)

        @block.sync
        def _(eh):
            S.emit_engine("sp", eh, final)

        self.ops = {e: [] for e in ENGS}
        self.phase += 1


class Phase:
    def __init__(self, K, name):
        self.K = K
        K.uid += 1
        self.name = f"{name}u{K.uid}"
        self.es = ExitStack()
        self.n = 0

    def __enter__(self):
        self.es.__enter__()
        return self

    def sb(self, shape, dt, name=None):
        self.n += 1
        return self.es.enter_context(self.K.nc.sbuf_tensor(f"{self.name}_{name or 't'}{self.n}", list(shape), dt))

    def ps(self, shape, dt=F32, name=None):
        self.n += 1
        return self.es.enter_context(self.K.nc.psum_tensor(f"{self.name}_{name or 'p'}{self.n}", list(shape), dt))

    def __exit__(self, *a):
        with self.K.nc.Block() as block:
            self.K.S.end_phase(block)
        return self.es.__exit__(*a)


class Ring:
    def __init__(self, tiles):
        self.tiles = tiles
        self.bufs = [Buf() for _ in tiles]
        self.i = -1

    def next(self):
        self.i = (self.i + 1) % len(self.tiles)
        return self.tiles[self.i], self.bufs[self.i]


class KB:
    def __init__(self, dbg=(), stop_after=None):
        self.dbg = set(dbg)
        self.stop_after = stop_after
        self.nc = bass.Bass("TRN2", target_bir_lowering=False)
        self.es = ExitStack()
        self.dram = {}
        self.uid = 0
        self.rows = (0, 1)
        self.sfx = ""

    def inp(self, name, shape, dt=F32):
        self.dram[name] = self.nc.dram_tensor(name, list(shape), dt, kind="ExternalInput").ap()
        return self.dram[name]

    def scratch(self, name, shape, dt=F32):
        kind = "ExternalOutput" if name in self.dbg else "Internal"
        name = name + self.sfx
        self.dram[name] = self.nc.dram_tensor(name, list(shape), dt, kind=kind).ap()
        return self.dram[name]

    def sbuf(self, es, name, shape, dt):
        self.uid += 1
        return es.enter_context(self.nc.sbuf_tensor(f"{name}u{self.uid}", list(shape), dt))

    def phase(self, name):
        return Phase(self, name)


def wpiece(w2d, n0, ncols, kc=16):
    return w2d.rearrange("(c p) n -> p c n", p=128)[:, :, n0:n0 + ncols]


def load_mod_rows(K, P, modD, layer, which, g_row_ap, nr=2):
    S = K.S
    sh_off = 0 if which == 1 else 3 * D
    sc_off = sh_off + D
    A, Bt, bA, bB = [], [], [], []
    gt = P.sb([128, D], F32)
    bg = Buf()
    S.dma("sp", gt[:], g_row_ap.broadcast_to([128, D]), [], [bg])
    for r in range(nr):
        a = P.sb([128, D], F32)
        b = P.sb([128, D], F32)
        ba, bb = Buf(), Buf()
        S.dma("sp", a[:], modD[layer, K.rows[r]:K.rows[r] + 1, sc_off:sc_off + D].broadcast_to([128, D]), [], [ba])
        S.dma("sp", b[:], modD[layer, K.rows[r]:K.rows[r] + 1, sh_off:sh_off + D].broadcast_to([128, D]), [], [bb])
        S.do("dve", "scalar_tensor_tensor", [ba, bg], [ba], out=a[:], in0=a[:], scalar=1.0, in1=gt[:], op0=ALU.add, op1=ALU.mult)
        A.append(a), Bt.append(b), bA.append(ba), bB.append(bb)
    return A, Bt, bA, bB


def load_gate_rows(K, P, modD, layer, which, nr=2):
    S = K.S
    off = 2 * D if which == 1 else 5 * D
    G, bG = [], []
    for r in range(nr):
        g = P.sb([128, D], F32)
        bg = Buf()
        S.dma("sp", g[:], modD[layer, K.rows[r]:K.rows[r] + 1, off:off + D].broadcast_to([128, D]), [], [bg])
        G.append(g), bG.append(bg)
    return G, bG


def rms_rstd(S, P, hbuf, bh, junk, bj, ssq, rstd, bs, tc, dim):
    S.do("act", "activation", [bh], [bj, bs], out=junk, in_=hbuf, func=AF.Square, accum_out=ssq[:, tc:tc + 1])
    S.do("act", "activation", [bs], [bs], out=rstd[:, tc:tc + 1], in_=ssq[:, tc:tc + 1], func=AF.Sqrt, scale=1.0 / dim, bias=EPS)
    S.do("dve", "reciprocal", [bs], [bs], out=rstd[:, tc:tc + 1], in_=rstd[:, tc:tc + 1])


def phase_ab(K, layer, x, ctx, pos, H, modD, norm_g, w_in, w_out, v_g, w_s, b_s, cs128_d, dftT_d, dftC_d, catT, P12,
             ident, identb):
    S = K.S
    nc = K.nc
    ntc = NTC
    with ExitStack() as outer:
      xnT = K.sbuf(outer, "xnT_ab", [128, 16, NTOK], BF16)
      b_xnT = [Buf() for _ in range(ntc)]
      with K.phase("ab1a") as P:
        A, Bt, bA, bB = load_mod_rows(K, P, modD, layer, 1, norm_g[layer, 0:1, :])
        ssq = P.sb([128, ntc], F32)
        rstd = P.sb([128, ntc], F32)
        hr = Ring([P.sb([128, D], F32) for _ in range(2)])
        pr_ = Ring([P.sb([128, D], F32) for _ in range(2)])
        junk = P.sb([128, D], BF16)
        bj = Buf()
        t1 = P.sb([128, D], F32)
        bt1 = Buf()
        xr = Ring([P.sb([128, D], BF16) for _ in range(2)])
        ptr = Ring([P.ps([128, 8, 128], BF16) for _ in range(2)])
        for tc in range(ntc):
            r = 0 if tc < NLAT else 1
            h, bh = hr.next()
            if tc < NLAT:
                pt, bp = pr_.next()
                S.dma("sp", h[:], x[tc * 128:(tc + 1) * 128, :], [], [bh])
                S.dma("sp", pt[:], pos[tc * 128:(tc + 1) * 128, :], [], [bp])
                S.do("pool", "tensor_tensor", [bh, bp], [bh], out=h[:], in0=h[:], in1=pt[:], op=ALU.add)
            else:
                S.dma("sp", h[:], ctx[(tc - NLAT) * 128:(tc - NLAT + 1) * 128, :], [], [bh])
            S.dma("sp", H[tc * 128:(tc + 1) * 128, :], h[:], [bh], [S.B("H", tc)])
            bs = Buf()
            rms_rstd(S, P, h[:], bh, junk[:], bj, ssq, rstd, bs, tc, D)
            S.do("dve", "scalar_tensor_tensor", [bh, bs, bA[r]], [bt1], out=t1[:], in0=h[:], scalar=rstd[:, tc:tc + 1], in1=A[r][:],
                 op0=ALU.mult, op1=ALU.mult)
            xt, bx = xr.next()
            S.do("dve", "tensor_tensor", [bt1, bB[r]], [bx], out=xt[:], in0=t1[:], in1=Bt[r][:], op=ALU.add)
            for half in range(2):
                ps, bp = ptr.next()
                for j in range(8):
                    c = half * 8 + j
                    S.do("pe", "transpose", [bx], [bp], ps[:, j, :], xt[:, c * 128:(c + 1) * 128], identb[:])
                S.do("act", "copy", [bp], [b_xnT[tc]], out=xnT[:, half * 8:(half + 1) * 8, tc * 128:(tc + 1) * 128], in_=ps[:])
        if "xnT" in K.dbg:
            dd = K.scratch("xnT", [D, NTOK], BF16)
            S.dma("sp", dd.rearrange("(c p) t -> p c t", p=128), xnT[:], b_xnT, [Buf()])
      if K.stop_after == "xn":
        return
      with K.phase("ab1b") as P:
        junk = P.sb([128, 512], BF16)
        bj = Buf()
        wr = Ring([P.sb([128, 16, 512], BF16) for _ in range(2)])
        pmm = Ring([P.ps([128, 512], F32) for _ in range(2)])
        vtok = P.sb([128, ntc, 1024], BF16)
        b_v = [Buf() for _ in range(ntc)]
        vss = P.sb([128, ntc, 2], F32)
        vr = P.sb([128, ntc], F32)
        bvs = Buf()
        for pc in range(2):
            w, bw = wr.next()
            S.dma("pool", w[:], wpiece(w_in, 1024 + pc * 512, 512), [], [bw])
            for tc in range(ntc):
                ps, bp = pmm.next()
                for c in range(16):
                    S.do("pe", "matmul", [bw, b_xnT[tc]], [bp], ps[:], xnT[:, c, tc * 128:(tc + 1) * 128], w[:, c, :],
                         start=(c == 0), stop=(c == 15))
                S.do("act", "activation", [bp], [b_v[tc]], out=vtok[:, tc, pc * 512:(pc + 1) * 512], in_=ps[:], func=AF.Gelu)
                S.do("act", "activation", [b_v[tc]], [bj, bvs], out=junk[:, 0:512], in_=vtok[:, tc, pc * 512:(pc + 1) * 512],
                     func=AF.Square, accum_out=vss[:, tc, pc:pc + 1])
        S.do("dve", "tensor_tensor", [bvs], [bvs], out=vr[:], in0=vss[:, :, 0], in1=vss[:, :, 1], op=ALU.add)
        S.do("act", "activation", [bvs], [bvs], out=vr[:], in_=vr[:], func=AF.Sqrt, scale=1.0 / 1024, bias=EPS)
        S.do("dve", "reciprocal", [bvs], [bvs], out=vr[:], in_=vr[:])
        for tc in range(ntc):
            S.do("dve", "tensor_scalar", [bvs, b_v[tc]], [b_v[tc]], out=vtok[:, tc, :], in0=vtok[:, tc, :], scalar1=vr[:, tc:tc + 1],
                 scalar2=None, op0=ALU.mult)
        wsf = P.sb([128, 8, 128], F32)
        wsT = P.sb([128, 8, 128], BF16)
        vg8 = P.sb([8, 128], F32)
        vgT = P.sb([128, 8], F32)
        bsb = P.sb([128, 8, 128], F32)
        b_ws, b_wsT, b_vg, b_vgT, b_bsb = Buf(), Buf(), Buf(), Buf(), Buf()
        S.dma("sp", wsf[:], w_s.rearrange("g i j -> i g j"), [], [b_ws])
        S.dma("sp", vg8[:], v_g.rearrange("(g p) -> g p", p=128), [], [b_vg])
        S.dma("sp", bsb[:].rearrange("p g i -> p (g i)"), b_s.rearrange("g i -> (g i)").unsqueeze(0).broadcast_to([128, 1024]), [], [b_bsb])
        pss = Ring([P.ps([128, 512], F32) for _ in range(2)])
        pst = pss.tiles[0][:].rearrange("p (j i) -> p j i", j=4)
        b_pst = pss.bufs[0]
        for hf in range(2):
            for j in range(4):
                g = hf * 4 + j
                S.do("pe", "transpose", [b_ws], [b_pst], pst[:, j, :], wsf[:, g, :], ident[:])
            S.do("dve", "tensor_copy", [b_pst], [b_wsT], out=wsT[:, hf * 4:(hf + 1) * 4, :], in_=pst[:])
        S.do("pe", "transpose", [b_vg], [b_pst], pst[:, 0, 0:8], vg8[:], ident[0:8, 0:8])
        S.do("dve", "tensor_copy", [b_pst], [b_vgT], out=vgT[:], in_=pst[:, 0, 0:8])
        ur = Ring([P.sb([128, 512], F32) for _ in range(2)])
        sr = Ring([P.sb([128, 512], F32) for _ in range(2)])
        yr = Ring([P.sb([128, 512], BF16) for _ in range(2)])
        ttiles = [(i * 512, 512) for i in range(4)] + [(T, TCX)]
        for pc in range(2):
            w, bw = wr.next()
            S.dma("pool", w[:], wpiece(w_in, pc * 512, 512), [], [bw])
            for oc in range(4):
                g = pc * 4 + oc
                for (t0, tn) in ttiles:
                    tcs = list(range(t0 // 128, (t0 + tn) // 128))
                    ps, bp = pmm.next()
                    for c in range(16):
                        S.do("pe", "matmul", [bw] + [b_xnT[q] for q in tcs], [bp], ps[:, 0:tn], w[:, c, oc * 128:(oc + 1) * 128],
                             xnT[:, c, t0:t0 + tn], start=(c == 0), stop=(c == 15))
                    ut, bu = ur.next()
                    S.do("act", "activation", [bp], [bu], out=ut[:, 0:tn], in_=ps[:, 0:tn], func=AF.Gelu)
                    ps2, bp2 = pss.next()
                    for k, q in enumerate(tcs):
                        S.do("pe", "matmul", [b_v[q], b_wsT], [bp2], ps2[:, k * 128:(k + 1) * 128], vtok[:, q, g * 128:(g + 1) * 128],
                             wsT[:, g, :], start=True, stop=True)
                    st, bs_ = sr.next()
                    nk = len(tcs)
                    S.do("dve", "scalar_tensor_tensor", [bp2, b_vgT, b_bsb], [bs_], out=st[:, 0:tn].rearrange("p (k i) -> p k i", k=nk),
                         in0=ps2[:, 0:tn].rearrange("p (k i) -> p k i", k=nk), scalar=vgT[:, g:g + 1],
                         in1=bsb[:, g:g + 1, :].broadcast_to([128, nk, 128]), op0=ALU.mult, op1=ALU.add)
                    yt, by = yr.next()
                    S.do("dve", "tensor_tensor", [bs_, bu], [by], out=yt[:, 0:tn], in0=st[:, 0:tn], in1=ut[:, 0:tn], op=ALU.mult)
                    S.dma("sp", catT[g * 128:(g + 1) * 128, t0:t0 + tn], yt[:, 0:tn], [by], [S.B("catT", g, t0)])
        cs = P.sb([128, 256], BF16)
        b_cs = Buf()
        S.dma("sp", cs[:], cs128_d[:, :], [], [b_cs])
        zr = Ring([P.sb([128, 512], BF16) for _ in range(2)])
        p12r = Ring([P.sb([128, 4, 256], BF16) for _ in range(2)])
        pz = Ring([P.ps([128, 2, 256], F32) for _ in range(2)])
        for pc in range(2):
            w, bw = wr.next()
            S.dma("pool", w[:], wpiece(w_in, 2048 + pc * 512, 512), [], [bw])
            for oc in range(4):
                g = pc * 4 + oc
                for (t0, tn) in ttiles:
                    tcs = list(range(t0 // 128, (t0 + tn) // 128))
                    ps, bp = pmm.next()
                    for c in range(16):
                        S.do("pe", "matmul", [bw] + [b_xnT[q] for q in tcs], [bp], ps[:, 0:tn], w[:, c, oc * 128:(oc + 1) * 128],
                             xnT[:, c, t0:t0 + tn], start=(c == 0), stop=(c == 15))
                    zt, bz = zr.next()
                    S.do("act", "copy", [bp], [bz], out=zt[:, 0:tn], in_=ps[:, 0:tn])
                    pt, bpt = p12r.next()
                    for k2 in range(0, len(tcs), 2):
                        psz, bpz = pz.next()
                        for k in range(k2, k2 + 2):
                            S.do("pe", "matmul", [bz, b_cs], [bpz], psz[:, k - k2, :], zt[:, k * 128:(k + 1) * 128], cs[:],
                                 start=True, stop=True)
                        S.do("dve", "tensor_copy", [bpz], [bpt], out=pt[:, k2:k2 + 2, :], in_=psz[:])
                    nk = len(tcs)
                    S.dma("sp", P12[g, t0:t0 + tn, :].rearrange("(k p) n -> p k n", p=128), pt[:, 0:nk, :], [bpt], [S.B("P12", g, t0)])
    if K.stop_after == "ab1":
        return
    scale = 1.0 / float(np.sqrt(T * 128.0))
    scale_c = 1.0 / float(np.sqrt(TCX * 128.0))
    with K.phase("ab3") as P:
        p12 = P.sb([128, 8, NLAT, 256], BF16)
        b_p12 = Buf()
        for g in range(8):
            S.dma("sp", p12[:, g, :, :], P12[g, 0:T, :].rearrange("(k p) n -> p k n", p=128), [], [b_p12])
        p12c = P.sb([128, 8, 2, 256], BF16)
        b_p12c = Buf()
        for g in range(8):
            S.dma("sp", p12c[:, g, :, :], P12[g, T:NTOK, :].rearrange("(k p) n -> p k n", p=128), [], [b_p12c])
        dr = Ring([P.sb([128, 2, NLAT, 512], BF16) for _ in range(2)])
        dc = P.sb([128, 2, 2, 256], BF16)
        b_dc = Buf()
        for m in range(2):
            S.dma("sp", dc[:, m, :, :], dftC_d[m].rearrange("(k p) t -> p k t", p=128), [], [b_dc])
        pf = Ring([P.ps([128, 512], F32) for _ in range(3)])
        yr = Ring([P.sb([128, 512], BF16) for _ in range(3)])
        for tt in range(4):
            dt_, bd = dr.next()
            for m in range(2):
                S.dma("sp", dt_[:, m, :, :], dftT_d[m].rearrange("(k p) t -> p k t", p=128)[:, :, tt * 512:(tt + 1) * 512], [], [bd])
            for g in range(8):
                ps, bp = pf.next()
                n = 0
                for m in range(2):
                    for k in range(NLAT):
                        S.do("pe", "matmul", [bd, b_p12], [bp], ps[:], p12[:, g, k, m * 128:(m + 1) * 128], dt_[:, m, k, :],
                             start=(n == 0), stop=(n == 2 * NLAT - 1))
                        n += 1
                yt, by = yr.next()
                S.do("act", "mul", [bp], [by], out=yt[:], in_=ps[:], mul=scale)
                S.dma("sp", catT[1024 + g * 128:1024 + (g + 1) * 128, tt * 512:(tt + 1) * 512], yt[:], [by], [Buf()])
        for g in range(8):
            ps, bp = pf.next()
            n = 0
            for m in range(2):
                for k in range(2):
                    S.do("pe", "matmul", [b_dc, b_p12c], [bp], ps[:, 0:TCX], p12c[:, g, k, m * 128:(m + 1) * 128], dc[:, m, k, :],
                         start=(n == 0), stop=(n == 3))
                    n += 1
            yt, by = yr.next()
            S.do("act", "mul", [bp], [by], out=yt[:, 0:TCX], in_=ps[:, 0:TCX], mul=scale_c)
            S.dma("sp", catT[1024 + g * 128:1024 + (g + 1) * 128, T:NTOK], yt[:, 0:TCX], [by], [Buf()])
    if K.stop_after == "ab3":
        return
    phase_outproj(K, layer, catT, 16, w_out, H, modD, NTC)


def phase_outproj(K, layer, yT_d, kc, w_out, H, modD, ntc):
    S = K.S
    with K.phase("oproj") as P:
        nr = 2 if ntc > NLAT else 1
        G, bG = load_gate_rows(K, P, modD, layer, 1, nr)
        ntok = ntc * 128
        half = ntok // 2 if kc > 16 else ntok
        wr = Ring([P.sb([128, 16, 512], BF16) for _ in range(4 if kc > 16 else 2)])
        pm = Ring([P.ps([128, 512], F32) for _ in range(3)])
        hr = Ring([P.sb([128, 512], F32) for _ in range(3)])
        tr = Ring([P.sb([128, 512], F32) for _ in range(3)])
        yT = P.sb([128, kc, half], BF16)
        b_y = Buf()
        for t0 in range(0, ntok, half):
            for c0 in range(0, kc, 8):
                S.dma("sp", yT[:, c0:c0 + 8, :], yT_d.rearrange("(c p) t -> p c t", p=128)[:, c0:c0 + 8, t0:t0 + half], [], [b_y])
            for n in range(4):
                ws = []
                for kh in range(kc // 16):
                    w, bw = wr.next()
                    S.dma("pool", w[:], w_out.rearrange("(c p) n -> p c n", p=128)[:, kh * 16:(kh + 1) * 16, n * 512:(n + 1) * 512], [], [bw])
                    ws.append((w, bw))
                for tcl in range(half // 128):
                    tc = t0 // 128 + tcl
                    r = 0 if tc < NLAT else 1
                    ps, bp = pm.next()
                    for c in range(kc):
                        w, bw = ws[c // 16]
                        S.do("pe", "matmul", [bw, b_y], [bp], ps[:], yT[:, c, tcl * 128:(tcl + 1) * 128], w[:, c % 16, :],
                             start=(c == 0), stop=(c == kc - 1))
                    ht, bh = hr.next()
                    hb = S.B("H", tc, n)
                    S.dma("sp", ht[:], H[tc * 128:(tc + 1) * 128, n * 512:(n + 1) * 512], [hb], [bh])
                    tt, bt = tr.next()
                    S.do("dve", "tensor_tensor", [bp, bG[r]], [bt], out=tt[:], in0=ps[:], in1=G[r][:, n * 512:(n + 1) * 512], op=ALU.mult)
                    S.do("pool", "tensor_tensor", [bt, bh], [bh], out=ht[:], in0=tt[:], in1=ht[:], op=ALU.add)
                    S.dma("sp", H[tc * 128:(tc + 1) * 128, n * 512:(n + 1) * 512], ht[:], [bh], [hb])


def phase_moe_pre(K, layer, has_ctx, H, modD, norm_g, w_router, tris_d, iota_d, ident, identb, XE, SELT, SELTC):
    S = K.S
    nc = K.nc
    ntc = NTC if has_ctx else NLAT
    ntok = ntc * 128
    nslot = NSLOT if has_ctx else CAP
    with ExitStack() as outer:
        xm = K.sbuf(outer, "xm", [128, ntc, D], BF16)
        b_xm = [Buf() for _ in range(ntc)]
        R = K.sbuf(outer, "R", [128, ntc, NE], F32)
        Mk = K.sbuf(outer, "Mk", [128, ntc, NE], F32)
        GM = K.sbuf(outer, "GM", [128, ntc, NE], F32)
        with K.phase(f"e1a{layer}") as P:
            A, Bt, bA, bB = load_mod_rows(K, P, modD, layer, 2, norm_g[layer, 1:2, :], 2 if has_ctx else 1)
            wr32 = P.sb([128, 16, NE], F32)
            b_wr = Buf()
            S.dma("sp", wr32[:], w_router.rearrange("(c p) e -> p c e", p=128), [], [b_wr])
            ssq = P.sb([128, ntc], F32)
            rstd = P.sb([128, ntc], F32)
            hr = Ring([P.sb([128, D], F32) for _ in range(2)])
            t1r = Ring([P.sb([128, D], F32) for _ in range(2)])
            junk = P.sb([128, D], BF16)
            bj = Buf()
            xtr = Ring([P.sb([128, 16, 128], F32) for _ in range(2)])
            ptr = Ring([P.ps([128, 4, 128], F32) for _ in range(4)])
            plg = Ring([P.ps([128, NE], F32) for _ in range(2)])
            Pt = P.sb([128, ntc, NE], F32)
            b_Pt = [Buf() for _ in range(ntc)]
            mx = P.sb([128, ntc], F32)
            sm = P.sb([128, ntc], F32)
            for tc in range(ntc):
                r = 0 if tc < NLAT else 1
                h, bh = hr.next()
                S.dma("sp", h[:], H[tc * 128:(tc + 1) * 128, :], [], [bh])
                bs = Buf()
                rms_rstd(S, P, h[:], bh, junk[:], bj, ssq, rstd, bs, tc, D)
                t1, bt1 = t1r.next()
                S.do("dve", "scalar_tensor_tensor", [bh, bs, bA[r]], [bt1], out=t1[:], in0=h[:], scalar=rstd[:, tc:tc + 1], in1=A[r][:],
                     op0=ALU.mult, op1=ALU.mult)
                S.do("dve", "tensor_tensor", [bt1, bB[r]], [bt1], out=t1[:], in0=t1[:], in1=Bt[r][:], op=ALU.add)
                S.do("act", "copy", [bt1], [b_xm[tc]], out=xm[:, tc, :], in_=t1[:])
                xt, bx = xtr.next()
                for q in range(4):
                    ps, bp = ptr.next()
                    for j in range(4):
                        c = q * 4 + j
                        S.do("pe", "transpose", [bt1], [bp], ps[:, j, :], t1[:, c * 128:(c + 1) * 128], ident[:])
                    S.do("act" if q % 2 else "dve", "copy" if q % 2 else "tensor_copy", [bp], [bx], out=xt[:, q * 4:(q + 1) * 4, :], in_=ps[:])
                pl, bl = plg.next()
                for c in range(16):
                    S.do("pe", "matmul", [bx, b_wr], [bl], pl[:], xt[:, c, :], wr32[:, c, :], start=(c == 0), stop=(c == 15))
                bm = Buf()
                S.do("dve", "tensor_reduce", [bl], [bm], out=mx[:, tc:tc + 1], in_=pl[:], axis=AX.X, op=ALU.max)
                S.do("dve", "tensor_scalar", [bm], [bm], out=mx[:, tc:tc + 1], in0=mx[:, tc:tc + 1], scalar1=-1.0, scalar2=None, op0=ALU.mult)
                S.do("act", "activation", [bl, bm], [b_Pt[tc], bm], out=Pt[:, tc, :], in_=pl[:], func=AF.Exp, bias=mx[:, tc:tc + 1], scale=1.0,
                     accum_out=sm[:, tc:tc + 1])
                S.do("dve", "reciprocal", [bm], [bm], out=sm[:, tc:tc + 1], in_=sm[:, tc:tc + 1])
                S.do("dve", "tensor_scalar", [bm, b_Pt[tc]], [b_Pt[tc]], out=Pt[:, tc, :], in0=Pt[:, tc, :], scalar1=sm[:, tc:tc + 1], scalar2=None,
                     op0=ALU.mult)
            PT = P.sb([NE, ntok], F32)
            b_PT = Buf()
            for q in range(0, ntc, 4):
                ps, bp = ptr.next()
                nq = min(4, ntc - q)
                for j in range(nq):
                    S.do("pe", "transpose", [b_Pt[q + j]], [bp], ps[0:NE, j, :], Pt[:, q + j, :], ident[:])
                S.do("dve", "tensor_copy", [bp], [b_PT], out=PT[:, q * 128:(q + nq) * 128], in_=ps[0:NE, 0:nq, :])
            wa = P.sb([NE, T], F32)
            wb = P.sb([NE, T], F32)
            m8 = P.sb([NE, 8], F32)
            tau = P.sb([NE, 2], F32)
            b_wa, b_wb, b_m8, b_tau = Buf(), Buf(), Buf(), Buf()
            seqs = [(0, T, CAP, 0)] + ([(T, TCX, CAPC, 1)] if has_ctx else [])
            maskT = P.sb([NE, ntok], F32)
            b_mT = Buf()
            for (t0, tn, cap, si) in seqs:
                cur, bc, oth, bo = PT[:, t0:t0 + tn], b_PT, wa[:, 0:tn], b_wa
                for rd in range(cap // 8):
                    S.do("dve", "max", [bc], [b_m8], out=m8[:], in_=cur)
                    if rd < cap // 8 - 1:
                        S.do("dve", "match_replace", [bc, b_m8], [bo], out=oth, in_to_replace=m8[:], in_values=cur, imm_value=-1.0)
                        if rd == 0:
                            cur, bc, oth, bo = wa[:, 0:tn], b_wa, wb[:, 0:tn], b_wb
                        else:
                            cur, bc, oth, bo = oth, bo, cur, bc
                S.do("dve", "tensor_copy", [b_m8], [b_tau], out=tau[:, si:si + 1], in_=m8[:, 7:8])
                S.do("dve", "tensor_scalar", [b_PT, b_tau], [b_mT], out=maskT[:, t0:t0 + tn], in0=PT[:, t0:t0 + tn], scalar1=tau[:, si:si + 1],
                     scalar2=None, op0=ALU.is_ge)
            pk = P.ps([128, ntc, NE], F32)
            b_pk = Buf()
            for tc in range(ntc):
                S.do("pe", "transpose", [b_mT], [b_pk], pk[:, tc, :], maskT[:, tc * 128:(tc + 1) * 128], ident[0:NE, 0:NE])
            b_Mk, b_GM, b_R = Buf(), Buf(), Buf()
            S.do("dve", "tensor_copy", [b_pk], [b_Mk], out=Mk[:], in_=pk[:])
            S.do("dve", "tensor_tensor", [b_Mk] + b_Pt, [b_GM], out=GM[:], in0=Mk[:], in1=Pt[:], op=ALU.mult)
            Mb = P.sb([128, ntc, NE], BF16)
            b_Mb = Buf()
            S.do("dve", "tensor_copy", [b_Mk], [b_Mb], out=Mb[:], in_=Mk[:])
            trf = P.sb([128, 128], F32)
            trb = P.sb([128, 128], BF16)
            oneb = P.sb([128, 128], BF16)
            b_tr, b_one = Buf(), Buf()
            S.dma("sp", trf[:], tris_d[0], [], [b_tr])
            S.do("dve", "tensor_copy", [b_tr], [b_tr], out=trb[:], in_=trf[:])
            S.do("dve", "memset", [], [b_one], oneb[:], 1.0)
            pr2 = P.ps([128, ntc, NE], F32)
            b_pr2 = Buf()
            for tc in range(ntc):
                first = 0 if tc < NLAT else NLAT
                for t2 in range(first, tc + 1):
                    S.do("pe", "matmul", [b_Mb, b_tr, b_one], [b_pr2], pr2[:, tc, :], (trb if t2 == tc else oneb)[:], Mb[:, t2, :],
                         start=(t2 == first), stop=(t2 == tc))
            S.do("dve", "tensor_copy", [b_pr2], [b_R], out=R[:], in_=pr2[:])
            if has_ctx:
                S.do("dve", "tensor_scalar", [b_R], [b_R], out=R[:, NLAT:, :], in0=R[:, NLAT:, :], scalar1=float(CAP), scalar2=None, op0=ALU.add)
            if "Pt" in K.dbg:
                dd = K.scratch("Pt", [ntok, NE])
                S.dma("sp", dd.rearrange("(k p) e -> p k e", p=128), Pt[:], b_Pt, [Buf()])
                dd = K.scratch("Mk", [ntok, NE])
                S.dma("sp", dd.rearrange("(k p) e -> p k e", p=128), Mk[:], [b_Mk], [Buf()])
                dd = K.scratch("Rk", [ntok, NE])
                S.dma("sp", dd.rearrange("(k p) e -> p k e", p=128), R[:], [b_R], [Buf()])
        if K.stop_after == "e1a":
            return
        with K.phase(f"e1b{layer}") as P:
            iot = P.sb([128, NSLOT], F32)
            b_io = Buf()
            S.dma("sp", iot[:], iota_d[:, :], [], [b_io])
            selr = Ring([P.sb([128, NLAT, CAP], BF16) for _ in range(2)])
            sgr = Ring([P.sb([128, NLAT, CAP], BF16) for _ in range(2)])
            selc = Ring([P.sb([128, 2, CAPC], BF16) for _ in range(2)])
            sgc = Ring([P.sb([128, 2, CAPC], BF16) for _ in range(2)])
            xer = Ring([P.sb([128, 16, nslot], BF16) for _ in range(2)])
            str_ = Ring([P.sb([128, NLAT, 2, 128], BF16) for _ in range(2)])
            stc = Ring([P.sb([CAPC, 2, 128], BF16) for _ in range(2)])
            pg = Ring([P.ps([128, 512], F32) for _ in range(3)])
            pt2 = Ring([P.ps([128, 8, 128], BF16) for _ in range(3)])
            for e in range(NE):
                sel, bsel = selr.next()
                sg, bsg = sgr.next()
                for tc in range(NLAT):
                    S.do("dve", "tensor_scalar", [b_io], [bsel], out=sel[:, tc, :], in0=iot[:, 0:CAP], scalar1=R[:, tc, e:e + 1],
                         scalar2=Mk[:, tc, e:e + 1], op0=ALU.is_equal, op1=ALU.mult)
                    S.do("pool", "tensor_scalar", [b_io], [bsg], out=sg[:, tc, :], in0=iot[:, 0:CAP], scalar1=R[:, tc, e:e + 1],
                         scalar2=GM[:, tc, e:e + 1], op0=ALU.is_equal, op1=ALU.mult)
                if has_ctx:
                    sc_, bsc = selc.next()
                    sgc_, bsgc = sgc.next()
                    for k in range(2):
                        S.do("dve", "tensor_scalar", [b_io], [bsc], out=sc_[:, k, :], in0=iot[:, CAP:NSLOT], scalar1=R[:, NLAT + k, e:e + 1],
                             scalar2=Mk[:, NLAT + k, e:e + 1], op0=ALU.is_equal, op1=ALU.mult)
                        S.do("dve", "tensor_scalar", [b_io], [bsgc], out=sgc_[:, k, :], in0=iot[:, CAP:NSLOT], scalar1=R[:, NLAT + k, e:e + 1],
                             scalar2=GM[:, NLAT + k, e:e + 1], op0=ALU.is_equal, op1=ALU.mult)
                xe, bxe = xer.next()
                for c in range(16):
                    ps, bp = pg.next()
                    for tc in range(NLAT):
                        S.do("pe", "matmul", [bsel, b_xm[tc]], [bp], ps[:, 0:CAP], xm[:, tc, c * 128:(c + 1) * 128], sel[:, tc, :],
                             start=(tc == 0), stop=(tc == NLAT - 1))
                    if has_ctx:
                        for k in range(2):
                            S.do("pe", "matmul", [bsc, b_xm[NLAT + k]], [bp], ps[:, CAP:NSLOT], xm[:, NLAT + k, c * 128:(c + 1) * 128], sc_[:, k, :],
                                 start=(k == 0), stop=(k == 1))
                    S.do("act", "copy", [bp], [bxe], out=xe[:, c, :], in_=ps[:, 0:nslot])
                S.dma("sp", XE[e].rearrange("(c p) s -> p c s", p=128)[:, :, 0:nslot], xe[:], [bxe], [S.B("XE", layer, e)])
                st, bst = str_.next()
                for q in range(0, NLAT, 4):
                    ps, bp = pt2.next()
                    for j in range(4):
                        for s2 in range(2):
                            S.do("pe", "transpose", [bsg], [bp], ps[:, j * 2 + s2, :], sg[:, q + j, s2 * 128:(s2 + 1) * 128], identb[:])
                    S.do("dve", "tensor_copy", [bp], [bst], out=st[:, q:q + 4, :, :], in_=ps[:].rearrange("p (j s) t -> p j s t", s=2))
                S.dma("sp", SELT[:, :, e * 2:(e + 1) * 2, :].rearrange("k p s t -> p k s t"), st[:], [bst], [S.B("SELT", layer, e)])
                if has_ctx:
                    stc_, bstc = stc.next()
                    ps, bp = pt2.next()
                    for k in range(2):
                        S.do("pe", "transpose", [bsgc], [bp], ps[0:CAPC, k, :], sgc_[:, k, :], identb[:])
                    S.do("dve", "tensor_copy", [bp], [bstc], out=stc_[:], in_=ps[0:CAPC, 0:2, :])
                    S.dma("sp", SELTC[:, :, e, :].rearrange("k p t -> p k t"), stc_[:], [bstc], [S.B("SELTC", layer, e)])


def modulate_xnT(K, pname, layer, H, modD, g_row, xnT, b_xnT, identb, ntc):
    S = K.S
    with K.phase(pname) as P:
        A, Bt, bA, bB = load_mod_rows(K, P, modD, layer, 1, g_row)
        ssq = P.sb([128, ntc], F32)
        rstd = P.sb([128, ntc], F32)
        hr = Ring([P.sb([128, D], F32) for _ in range(2)])
        junk = P.sb([128, D], BF16)
        bj = Buf()
        t1 = P.sb([128, D], F32)
        bt1 = Buf()
        xr = Ring([P.sb([128, D], BF16) for _ in range(2)])
        ptr = Ring([P.ps([128, 8, 128], BF16) for _ in range(2)])
        for tc in range(ntc):
            r = 0 if tc < NLAT else 1
            h, bh = hr.next()
            S.dma("sp", h[:], H[tc * 128:(tc + 1) * 128, :], [], [bh])
            bs = Buf()
            rms_rstd(S, P, h[:], bh, junk[:], bj, ssq, rstd, bs, tc, D)
            S.do("dve", "scalar_tensor_tensor", [bh, bs, bA[r]], [bt1], out=t1[:], in0=h[:], scalar=rstd[:, tc:tc + 1], in1=A[r][:],
                 op0=ALU.mult, op1=ALU.mult)
            xt, bx = xr.next()
            S.do("dve", "tensor_tensor", [bt1, bB[r]], [bx], out=xt[:], in0=t1[:], in1=Bt[r][:], op=ALU.add)
            for half in range(2):
                ps, bp = ptr.next()
                for j in range(8):
                    c = half * 8 + j
                    S.do("pe", "transpose", [bx], [bp], ps[:, j, :], xt[:, c * 128:(c + 1) * 128], identb[:])
                S.do("act", "copy", [bp], [b_xnT[tc]], out=xnT[:, half * 8:(half + 1) * 8, tc * 128:(tc + 1) * 128], in_=ps[:])


def phase_ssd(K, layer, H, modD, norm_g, w_in, conv_w, conv_b, dt_bias, a_log, d_skip, ng, w_out, tris_d, ident, identb, scr):
    S = K.S
    nc = K.nc
    ZS, BCT, XS, BTOK, DTS, YB, YT = scr
    ntc = NTC
    ttiles = [(i * 512, 512) for i in range(4)] + [(T, TCX)]
    with ExitStack() as outer:
        xnT = K.sbuf(outer, "xnT_ssd", [128, 16, NTOK], BF16)
        b_xnT = [Buf() for _ in range(ntc)]
        modulate_xnT(K, "s1", layer, H, modD, norm_g[layer, 0:1, :], xnT, b_xnT, identb, ntc)
        with K.phase("s2") as P:
            wr = Ring([P.sb([128, 16, 512], BF16) for _ in range(2)])
            pmm = Ring([P.ps([128, 512], F32) for _ in range(3)])
            ptr = Ring([P.ps([128, 8, 128], BF16) for _ in range(2)])
            pst = P.ps([128, 512], F32)
            b_pst = Buf()
            cwr = P.sb([120, 2, 128], F32)
            cbr = P.sb([48, 128], F32)
            cwT = P.sb([128, 5, 48], F32)
            cbT = P.sb([128, 48], F32)
            b_cwr, b_cw = Buf(), Buf()
            cw_rows = conv_w.rearrange("k (c p) -> (k c) p", p=128)
            for hf in range(2):
                S.dma("sp", cwr[:, hf, :], cw_rows[hf * 120:(hf + 1) * 120, :], [], [b_cwr])
            S.dma("sp", cbr[:], conv_b.rearrange("(c p) -> c p", p=128), [], [b_cwr])
            for hf in range(2):
                S.do("pe", "transpose", [b_cwr], [b_pst], pst[:, hf * 120:(hf + 1) * 120], cwr[:, hf, :], ident[0:120, 0:120])
            S.do("pe", "transpose", [b_cwr], [b_pst], pst[:, 240:288], cbr[:], ident[0:48, 0:48])
            S.do("dve", "tensor_copy", [b_pst], [b_cw], out=cwT[:].rearrange("p k c -> p (k c)"), in_=pst[:, 0:240])
            S.do("dve", "tensor_copy", [b_pst], [b_cw], out=cbT[:], in_=pst[:, 240:288])
            zr = Ring([P.sb([128, 512], BF16) for _ in range(3)])
            for pc in range(8):
                w, bw = wr.next()
                S.dma("pool", w[:], wpiece(w_in, pc * 512, 512), [], [bw])
                for tc in range(NLAT):
                    ps, bp = pmm.next()
                    for c in range(16):
                        S.do("pe", "matmul", [bw, b_xnT[tc]], [bp], ps[:], xnT[:, c, tc * 128:(tc + 1) * 128], w[:, c, :],
                             start=(c == 0), stop=(c == 15))
                    zt, bz = zr.next()
                    S.do("act", "activation", [bp], [bz], out=zt[:], in_=ps[:], func=AF.Silu)
                    S.dma("sp", ZS[tc * 128:(tc + 1) * 128, pc * 512:(pc + 1) * 512], zt[:], [bz], [Buf()])
            rbr = Ring([P.sb([128, NTOK + 8], F32) for _ in range(2)])
            accr = Ring([P.sb([128, NTOK], F32) for _ in range(2)])
            obr = Ring([P.sb([128, NTOK], BF16) for _ in range(2)])
            tsr = Ring([P.sb([128, ntc, 128], BF16) for _ in range(2)])
            for rb, bb in zip(rbr.tiles, rbr.bufs):
                S.do("dve", "memset", [], [bb], rb[:], 0.0)
            LOFF, COFF = 2, T + 6
            for pc in range(12):
                w, bw = wr.next()
                S.dma("pool", w[:], wpiece(w_in, 4096 + pc * 512, 512), [], [bw])
                for oc in range(4):
                    ch = pc * 4 + oc
                    rb, brb = rbr.next()
                    for (t0, tn) in ttiles:
                        tcs = list(range(t0 // 128, (t0 + tn) // 128))
                        ps, bp = pmm.next()
                        for c in range(16):
                            S.do("pe", "matmul", [bw] + [b_xnT[q] for q in tcs], [bp], ps[:, 0:tn], w[:, c, oc * 128:(oc + 1) * 128],
                                 xnT[:, c, t0:t0 + tn], start=(c == 0), stop=(c == 15))
                        off = LOFF + t0 if t0 < T else COFF
                        S.do("act", "copy", [bp], [brb], out=rb[:, off:off + tn], in_=ps[:, 0:tn])
                    acc, bacc = accr.next()
                    for (a0, an, ro) in ((0, T, LOFF), (T, TCX, COFF)):
                        for k in range(5):
                            src = rb[:, ro - 2 + k:ro - 2 + k + an]
                            if k == 0:
                                S.do("dve", "tensor_scalar", [brb, b_cw], [bacc], out=acc[:, a0:a0 + an], in0=src, scalar1=cwT[:, k, ch:ch + 1],
                                     scalar2=None, op0=ALU.mult)
                            else:
                                S.do("dve", "scalar_tensor_tensor", [brb, b_cw, bacc], [bacc], out=acc[:, a0:a0 + an], in0=src,
                                     scalar=cwT[:, k, ch:ch + 1], in1=acc[:, a0:a0 + an], op0=ALU.mult, op1=ALU.add)
                    ob, bob = obr.next()
                    S.do("act", "activation", [bacc, b_cw], [bob], out=ob[:], in_=acc[:], func=AF.Silu, bias=cbT[:, ch:ch + 1], scale=1.0)
                    if ch >= 32:
                        S.dma("sp", BCT[(ch - 32) * 128:(ch - 31) * 128, :], ob[:], [bob], [Buf()])
                    if ch < 40:
                        ts, bts = tsr.next()
                        for q in range(0, ntc, 8):
                            nq = min(8, ntc - q)
                            ps, bp = ptr.next()
                            for j in range(nq):
                                S.do("pe", "transpose", [bob], [bp], ps[:, j, :], ob[:, (q + j) * 128:(q + j + 1) * 128], identb[:])
                            S.do("dve", "tensor_copy", [bp], [bts], out=ts[:, q:q + nq, :], in_=ps[:, 0:nq, :])
                        if ch < 32:
                            S.dma("sp", XS[:, ch * 128:(ch + 1) * 128].rearrange("(k p) n -> p k n", p=128), ts[:], [bts], [Buf()])
                        else:
                            S.dma("sp", BTOK[:, (ch - 32) * 128:(ch - 31) * 128].rearrange("(k p) n -> p k n", p=128), ts[:], [bts], [Buf()])
            wdt = P.sb([128, 16, 128], BF16)
            b_wdt = Buf()
            S.dma("pool", wdt[:], wpiece(w_in, 10240, 128), [], [b_wdt])
            dtb = P.sb([128, 128], F32)
            b_dtb = Buf()
            S.dma("sp", dtb[:], dt_bias.rearrange("a h -> (a h)").unsqueeze(0).broadcast_to([128, 128]), [], [b_dtb])
            xbr = Ring([P.sb([128, 128], F32) for _ in range(2)])
            abr = Ring([P.sb([128, 128], F32) for _ in range(2)])
            for tc in range(ntc):
                ps, bp = pmm.next()
                for c in range(16):
                    S.do("pe", "matmul", [b_wdt, b_xnT[tc]], [bp], ps[:, 0:128], xnT[:, c, tc * 128:(tc + 1) * 128], wdt[:, c, :],
                         start=(c == 0), stop=(c == 15))
                xb, bxb = xbr.next()
                ab, bab = abr.next()
                S.do("dve", "tensor_tensor", [bp, b_dtb], [bxb], out=xb[:], in0=ps[:, 0:128], in1=dtb[:], op=ALU.add)
                S.do("dve", "scalar_tensor_tensor", [bxb], [bab], out=ab[:], in0=xb[:], scalar=-1.0, in1=xb[:], op0=ALU.mult, op1=ALU.max)
                S.do("act", "activation", [bab], [bab], out=ab[:], in_=ab[:], func=AF.Exp, scale=-1.0)
                S.do("act", "activation", [bab], [bab], out=ab[:], in_=ab[:], func=AF.Ln, bias=1.0, scale=1.0)
                S.do("dve", "scalar_tensor_tensor", [bxb, bab], [bxb], out=xb[:], in0=xb[:], scalar=0.0, in1=ab[:], op0=ALU.max, op1=ALU.add)
                S.dma("sp", DTS[tc * 128:(tc + 1) * 128, :], xb[:], [bxb], [Buf()])
    if K.stop_after == "s2":
        return
    with K.phase("s3") as P:
        trf = P.sb([128, 4, 128], F32)
        b_trf = Buf()
        S.dma("sp", trf[:], tris_d.rearrange("f k m -> k f m"), [], [b_trf])
        onef = P.sb([128, 128], F32)
        b_one = Buf()
        S.do("dve", "memset", [], [b_one], onef[:], 1.0)
        LT, LE, GT, GE = 0, 1, 2, 3
        arow = P.sb([128, 128], F32)
        b_arow = Buf()
        S.dma("sp", arow[:], a_log.rearrange("a h -> (a h)").unsqueeze(0).broadcast_to([128, 128]), [], [b_arow])
        S.do("act", "activation", [b_arow], [b_arow], out=arow[:], in_=arow[:], func=AF.Exp)
        S.do("dve", "tensor_scalar", [b_arow], [b_arow], out=arow[:], in0=arow[:], scalar1=-1.0, scalar2=None, op0=ALU.mult)
        drow = P.sb([128, 64], F32)
        b_drow = Buf()
        S.dma("sp", drow[:], d_skip.unsqueeze(0).broadcast_to([128, 64]), [], [b_drow])
        ng32 = P.sb([32, 128], F32)
        ngT = P.sb([128, 32], F32)
        b_ng = Buf()
        S.dma("sp", ng32[:], ng.rearrange("(c p) -> c p", p=128), [], [b_ng])
        xsr = Ring([P.sb([128, 4096], BF16) for _ in range(2)])
        btr = Ring([P.sb([128, 1024], BF16) for _ in range(2)])
        bcr = Ring([P.sb([128, 16, 128], BF16) for _ in range(2)])
        dtr = Ring([P.sb([128, 128], F32) for _ in range(2)])
        dar = Ring([P.sb([128, 64], F32) for _ in range(2)])
        ecr = Ring([P.sb([128, 3, 64], F32) for _ in range(2)])
        xg = P.sb([128, 4096], BF16)
        xgd = P.sb([128, 4096], BF16)
        b_xg, b_xgd = Buf(), Buf()
        Sf = P.sb([128, 8, 512], F32)
        Sb = P.sb([128, 8, 512], BF16)
        b_Sf = [Buf() for _ in range(8)]
        b_Sb = [Buf() for _ in range(8)]
        yar = Ring([P.sb([128, 4096], F32) for _ in range(2)])
        dtri = Ring([P.sb([128, 8, 128], F32) for _ in range(2)])
        Er = Ring([P.sb([128, 8, 128], F32) for _ in range(2)])
        Mr = Ring([P.sb([128, 8, 128], BF16) for _ in range(2)])
        cbr_ = Ring([P.sb([128, 128], F32) for _ in range(2)])
        tmr = Ring([P.sb([128, 512], F32) for _ in range(2)])
        zsr = Ring([P.sb([128, 4096], BF16) for _ in range(1)])
        ybt = P.sb([128, 4096], F32)
        b_ybt = Buf()
        yh = P.sb([128, 4096], BF16)
        b_yh = Buf()
        ytr = Ring([P.sb([128, 32, 128], BF16) for _ in range(2)])
        junk = P.sb([128, 4096], BF16)
        bj = Buf()
        ssq = P.sb([128, NLAT], F32)
        rstd = P.sb([128, NLAT], F32)
        p_c = P.ps([128, 3, 64], F32)
        p_cb = P.ps([128, 128], F32)
        p_seg = [P.ps([128, 4, 128], F32) for _ in range(2)]
        p_y = P.ps([128, 512], F32)
        p_y2 = P.ps([128, 512], F32)
        p_st = P.ps([128, 512], F32)
        p_t = P.ps([128, 8, 128], BF16)
        b_pc, b_pcb, b_pseg, b_py, b_py2, b_pst, b_pt = Buf(), Buf(), [Buf(), Buf()], Buf(), Buf(), Buf(), Buf()
        S.do("pe", "transpose", [b_ng], [b_py], p_y[:, 0:32], ng32[:], ident[0:32, 0:32])
        S.do("dve", "tensor_copy", [b_py], [b_ng], out=ngT[:], in_=p_y[:, 0:32])
        for d in (1, 0):
            Lm, Rt, CBm, cI, cA = (GT, LE, LE, LE, GT) if d == 0 else (LT, GE, GE, GE, LT)
            for g in range(8):
                S.do("dve", "memset", [], [b_Sf[g]], Sf[:, g, :], 0.0)
                S.do("pool", "memset", [], [b_Sb[g]], Sb[:, g, :], 0.0)
            order = [16, 17] + list(range(NLAT)) if d == 0 else [17, 16] + list(range(NLAT - 1, -1, -1))
            for tc in order:
                lat = tc < NLAT
                xs, bxs = xsr.next()
                S.dma("sp", xs[:], XS[tc * 128:(tc + 1) * 128, :], [], [bxs])
                bt, bbt = btr.next()
                S.dma("sp", bt[:], BTOK[tc * 128:(tc + 1) * 128, :], [], [bbt])
                dt_, bdt = dtr.next()
                S.dma("sp", dt_[:], DTS[tc * 128:(tc + 1) * 128, :], [], [bdt])
                if lat:
                    bc, bbc = bcr.next()
                    S.dma("sp", bc[:], BCT[:, tc * 128:(tc + 1) * 128].rearrange("(j p) t -> p j t", p=128), [], [bbc])
                da, bda = dar.next()
                S.do("dve", "tensor_tensor", [bdt, b_arow], [bda], out=da[:], in0=dt_[:, d * 64:(d + 1) * 64], in1=arow[:, d * 64:(d + 1) * 64],
                     op=ALU.mult)
                S.do("pe", "matmul", [bda, b_trf], [b_pc], p_c[:, 0, :], trf[:, cI, :], da[:], start=True, stop=True)
                S.do("pe", "matmul", [bda, b_trf], [b_pc], p_c[:, 1, :], trf[:, cA, :], da[:], start=True, stop=True)
                S.do("pe", "matmul", [bda, b_one], [b_pc], p_c[:, 2, :], onef[:], da[:], start=True, stop=True)
                ec, bec = ecr.next()
                S.do("act", "activation", [b_pc], [bec], out=ec[:], in_=p_c[:], func=AF.Exp)
                dtv = dt_[:, d * 64:(d + 1) * 64]
                S.do("dve", "tensor_tensor", [bxs, bdt], [b_xg], out=xg[:].rearrange("p (h q) -> p h q", h=64),
                     in0=xs[:].rearrange("p (h q) -> p h q", h=64), in1=dtv.unsqueeze(2).to_broadcast([128, 64, 64]), op=ALU.mult)
                S.do("pool", "tensor_tensor", [b_xg, bec], [b_xgd], out=xgd[:].rearrange("p (h q) -> p h q", h=64),
                     in0=xg[:].rearrange("p (h q) -> p h q", h=64), in1=ec[:, 1, :].unsqueeze(2).to_broadcast([128, 64, 64]), op=ALU.mult)
                if lat:
                    ya, bya = yar.next()
                for g in range(8):
                    if lat:
                        S.do("pe", "matmul", [bbc], [b_pcb], p_cb[:], bc[:, g, :], bc[:, 8 + g, :], start=True, stop=True)
                        cb, bcb = cbr_.next()
                        S.do("dve", "tensor_tensor", [b_pcb, b_trf], [bcb], out=cb[:], in0=p_cb[:], in1=trf[:, CBm, :], op=ALU.mult)
                        dtt, bdtt = dtri.next()
                        S.do("pool", "tensor_tensor", [b_trf, bda], [bdtt], out=dtt[:], in0=trf[:, Rt:Rt + 1, :].to_broadcast([128, 8, 128]),
                             in1=da[:, g * 8:(g + 1) * 8].unsqueeze(2).to_broadcast([128, 8, 128]), op=ALU.mult)
                        for hh in range(2):
                            S.do("pe", "matmul", [bdtt, b_trf], [b_pseg[hh]], p_seg[hh][:].rearrange("p a b -> p (a b)"), trf[:, Lm, :],
                                 dtt[:, hh * 4:(hh + 1) * 4, :].rearrange("p a b -> p (a b)"), start=True, stop=True)
                        E, bE = Er.next()
                        for hh in range(2):
                            S.do("act", "activation", [b_pseg[hh]], [bE], out=E[:, hh * 4:(hh + 1) * 4, :], in_=p_seg[hh][:], func=AF.Exp)
                        M, bM = Mr.next()
                        S.do("dve", "tensor_tensor", [bE, bcb], [bM], out=M[:], in0=E[:], in1=cb[:].unsqueeze(1).to_broadcast([128, 8, 128]),
                             op=ALU.mult)
                        for h in range(8):
                            hd = g * 8 + h
                            S.do("pe", "matmul", [bM, b_xg], [b_py], p_y[:, h * 64:(h + 1) * 64], M[:, h, :], xg[:, hd * 64:(hd + 1) * 64],
                                 start=True, stop=True)
                        S.do("pe", "matmul", [bbc, b_Sb[g]], [b_py2], p_y2[:], bc[:, 8 + g, :], Sb[:, g, :], start=True, stop=True)
                        tm, btm = tmr.next()
                        S.do("dve", "tensor_tensor", [b_py2, bec], [btm], out=tm[:].rearrange("p (h q) -> p h q", h=8),
                             in0=p_y2[:].rearrange("p (h q) -> p h q", h=8),
                             in1=ec[:, 0, g * 8:(g + 1) * 8].unsqueeze(2).to_broadcast([128, 8, 64]), op=ALU.mult)
                        S.do("dve", "tensor_tensor", [btm, b_py], [bya], out=ya[:, g * 512:(g + 1) * 512], in0=tm[:], in1=p_y[:], op=ALU.add)
                    S.do("pe", "matmul", [bbt, b_xgd], [b_pst], p_st[:], bt[:, g * 128:(g + 1) * 128], xgd[:, g * 512:(g + 1) * 512],
                         start=True, stop=True)
                    S.do("pool", "tensor_tensor", [b_Sf[g], bec], [b_Sf[g]], out=Sf[:, g, :].rearrange("p (h q) -> p h q", h=8),
                         in0=Sf[:, g, :].rearrange("p (h q) -> p h q", h=8),
                         in1=ec[:, 2, g * 8:(g + 1) * 8].unsqueeze(2).to_broadcast([128, 8, 64]), op=ALU.mult)
                    S.do("dve", "tensor_tensor", [b_Sf[g], b_pst], [b_Sf[g]], out=Sf[:, g, :], in0=Sf[:, g, :], in1=p_st[:], op=ALU.add)
                    S.do("act", "copy", [b_Sf[g]], [b_Sb[g]], out=Sb[:, g, :], in_=Sf[:, g, :])
                if not lat:
                    continue
                if d == 1:
                    S.dma("sp", YB[tc * 128:(tc + 1) * 128, :], ya[:], [bya], [S.B("YB", tc)])
                    continue
                S.dma("sp", ybt[:], YB[tc * 128:(tc + 1) * 128, :], [S.B("YB", tc)], [b_ybt])
                S.do("pool", "tensor_tensor", [bya, b_ybt], [bya], out=ya[:], in0=ya[:], in1=ybt[:], op=ALU.add)
                S.do("dve", "tensor_tensor", [bxs, b_drow], [b_ybt], out=ybt[:].rearrange("p (h q) -> p h q", h=64),
                     in0=xs[:].rearrange("p (h q) -> p h q", h=64), in1=drow[:].unsqueeze(2).to_broadcast([128, 64, 64]), op=ALU.mult)
                S.do("pool", "tensor_tensor", [bya, b_ybt], [bya], out=ya[:], in0=ya[:], in1=ybt[:], op=ALU.add)
                zs, bzs = zsr.next()
                S.dma("sp", zs[:], ZS[tc * 128:(tc + 1) * 128, :], [], [bzs])
                S.do("dve", "tensor_tensor", [bya, bzs], [bya], out=ya[:], in0=ya[:], in1=zs[:], op=ALU.mult)
                bs = Buf()
                rms_rstd(S, P, ya[:], bya, junk[:], bj, ssq, rstd, bs, tc, 4096)
                S.do("dve", "tensor_scalar", [bya, bs], [b_yh], out=yh[:], in0=ya[:], scalar1=rstd[:, tc:tc + 1], scalar2=None, op0=ALU.mult)
                yt, byt = ytr.next()
                for q in range(4):
                    for j in range(8):
                        c = q * 8 + j
                        S.do("pe", "transpose", [b_yh], [b_pt], p_t[:, j, :], yh[:, c * 128:(c + 1) * 128], identb[:])
                    S.do("dve", "tensor_tensor", [b_pt, b_ng], [byt], out=yt[:, q * 8:(q + 1) * 8, :], in0=p_t[:],
                         in1=ngT[:, q * 8:(q + 1) * 8].unsqueeze(2).to_broadcast([128, 8, 128]), op=ALU.mult)
                S.dma("sp", YT[:, tc * 128:(tc + 1) * 128].rearrange("(c p) t -> p c t", p=128), yt[:], [byt], [Buf()])
    if K.stop_after == "s3":
        return
    phase_outproj(K, layer, YT, 32, w_out, H, modD, NLAT)


def phase_final(K, H, final_g, out):
    S = K.S
    with K.phase("final") as P:
        gt = P.sb([128, D], F32)
        bg = Buf()
        S.dma("sp", gt[:], final_g.unsqueeze(0).broadcast_to([128, D]), [], [bg])
        hr = Ring([P.sb([128, D], F32) for _ in range(3)])
        junk = P.sb([128, D], BF16)
        bj = Buf()
        ssq = P.sb([128, NLAT], F32)
        rstd = P.sb([128, NLAT], F32)
        for tc in range(NLAT):
            h, bh = hr.next()
            S.dma("sp", h[:], H[tc * 128:(tc + 1) * 128, :], [], [bh])
            bs = Buf()
            rms_rstd(S, P, h[:], bh, junk[:], bj, ssq, rstd, bs, tc, D)
            S.do("dve", "scalar_tensor_tensor", [bh, bs, bg], [bh], out=h[:], in0=h[:], scalar=rstd[:, tc:tc + 1], in1=gt[:],
                 op0=ALU.mult, op1=ALU.mult)
            S.dma("sp", out[tc * 128:(tc + 1) * 128, :], h[:], [bh], [Buf()])
    K.out_written = True


def phase_moe_post(K, layer, has_ctx, Hin, H, modD, SELT, SELTC, YEo, jb):
    S = K.S
    ntc = NTC if has_ctx else NLAT
    with K.phase(f"e3{layer}") as P:
        nr = 2 if has_ctx else 1
        G, bG = load_gate_rows(K, P, modD, layer, 2, nr)
        yr = Ring([P.sb([128, 2 * NE, 512], BF16) for _ in range(2)])
        ycr = Ring([P.sb([CAPC, NE, 512], BF16) for _ in range(2)])
        sr = Ring([P.sb([128, 2 * NE, 128], BF16) for _ in range(3)])
        scr_ = Ring([P.sb([CAPC, NE, 128], BF16) for _ in range(2)])
        pm = Ring([P.ps([128, 512], F32) for _ in range(3)])
        hr = Ring([P.sb([128, 512], F32) for _ in range(3)])
        tr = Ring([P.sb([128, 512], F32) for _ in range(3)])
        for n in range(4):
            y, by = yr.next()
            S.dma("sp", y[:].rearrange("p (e s) c -> p e s c", s=2), YEo[:, jb, n, :, 0:2, :].rearrange("e p s c -> p e s c"), [], [by])
            if has_ctx:
                yc, byc = ycr.next()
                S.dma("sp", yc[:], YEo[:, jb, n, 0:CAPC, 2, :].rearrange("e p c -> p e c"), [], [byc])
            for tc in range(ntc):
                r = 0 if tc < NLAT else 1
                ps, bp = pm.next()
                if tc < NLAT:
                    st, bst = sr.next()
                    S.dma("sp", st[:], SELT[tc], [], [bst])
                    for j in range(2 * NE):
                        S.do("pe", "matmul", [bst, by], [bp], ps[:], st[:, j, :], y[:, j, :], start=(j == 0), stop=(j == 2 * NE - 1))
                else:
                    st, bst = scr_.next()
                    S.dma("sp", st[:], SELTC[tc - NLAT], [], [bst])
                    for j in range(NE):
                        S.do("pe", "matmul", [bst, byc], [bp], ps[:], st[:, j, :], yc[:, j, :], start=(j == 0), stop=(j == NE - 1))
                ht, bh = hr.next()
                hb = S.B("H3", tc, n)
                S.dma("sp", ht[:], Hin[tc * 128:(tc + 1) * 128, n * 512:(n + 1) * 512], [hb], [bh])
                tt, bt = tr.next()
                S.do("dve", "tensor_tensor", [bp, bG[r]], [bt], out=tt[:], in0=ps[:], in1=G[r][:, n * 512:(n + 1) * 512], op=ALU.mult)
                S.do("pool", "tensor_tensor", [bt, bh], [bh], out=ht[:], in0=tt[:], in1=ht[:], op=ALU.add)
                S.dma("sp", H[tc * 128:(tc + 1) * 128, n * 512:(n + 1) * 512], ht[:], [bh], [hb])


def phase_experts(K, XEget, wg, wu, wd, YEo, nslot, has_ctx, n_exp=2, NB=8):
    S = K.S
    with K.phase("e2") as P:
        xe = P.sb([128, NB, 16, nslot], BF16)
        hid = P.sb([128, NB, 16, nslot], BF16)
        b_xe = [Buf() for _ in range(NB)]
        b_hid = [Buf() for _ in range(NB)]
        wr = Ring([P.sb([128, 16, 512], BF16) for _ in range(3)])
        sgr = Ring([P.sb([128, nslot], F32) for _ in range(3)])
        yer = Ring([P.sb([128, 3, 512], BF16) for _ in range(2)])
        pgu = Ring([P.ps([128, 512], F32) for _ in range(4)])
        pdn = Ring([P.ps([128, 512], F32) for _ in range(3)])
        chunks = [(0, 128), (128, 128)] + ([(256, CAPC)] if has_ctx else [])
        for el in range(n_exp):
            for b in range(NB):
                S.dma("sp", xe[:, b, :, :], XEget(el, b).rearrange("(c p) s -> p c s", p=128)[:, :, 0:nslot], [], [b_xe[b]])
            for fp in range(4):
                wgt, bwg = wr.next()
                S.dma("pool", wgt[:], wpiece(wg[el], fp * 512, 512), [], [bwg])
                wut, bwu = wr.next()
                S.dma("pool", wut[:], wpiece(wu[el], fp * 512, 512), [], [bwu])
                for fo in range(4):
                    fc = fp * 4 + fo
                    for b in range(NB):
                        psg, bpg = pgu.next()
                        for c in range(16):
                            S.do("pe", "matmul", [bwg, b_xe[b]], [bpg], psg[:, 0:nslot], wgt[:, c, fo * 128:(fo + 1) * 128], xe[:, b, c, :],
                                 start=(c == 0), stop=(c == 15))
                        psu, bpu = pgu.next()
                        for c in range(16):
                            S.do("pe", "matmul", [bwu, b_xe[b]], [bpu], psu[:, 0:nslot], wut[:, c, fo * 128:(fo + 1) * 128], xe[:, b, c, :],
                                 start=(c == 0), stop=(c == 15))
                        sgt, bsg = sgr.next()
                        S.do("act", "activation", [bpg], [bsg], out=sgt[:], in_=psg[:, 0:nslot], func=AF.Silu)
                        S.do("dve", "tensor_tensor", [bsg, bpu], [b_hid[b]], out=hid[:, b, fc, :], in0=sgt[:], in1=psu[:, 0:nslot], op=ALU.mult)
            for n in range(4):
                wdt, bwd = wr.next()
                S.dma("pool", wdt[:], wpiece(wd[el], n * 512, 512), [], [bwd])
                for b in range(NB):
                    ye, bye = yer.next()
                    for si, (s0, sn) in enumerate(chunks):
                        ps, bp = pdn.next()
                        for fc in range(16):
                            S.do("pe", "matmul", [bwd, b_hid[b]], [bp], ps[0:sn, :], hid[:, b, fc, s0:s0 + sn], wdt[:, fc, :],
                                 start=(fc == 0), stop=(fc == 15))
                        S.do("act" if si % 2 else "dve", "copy" if si % 2 else "tensor_copy", [bp], [bye], out=ye[0:sn, si, :], in_=ps[0:sn, :])
                    S.dma("sp", YEo[el, b, n, :, 0:2, :], ye[:, 0:2, :], [bye], [Buf()])
                    if has_ctx:
                        S.dma("sp", YEo[el, b, n, 0:CAPC, 2, :], ye[0:CAPC, 2, :], [bye], [Buf()])


MODC = 6 * D // 8


def phase_mod(K, cc9, mod_w, mod_b, modp, ident, nrows=9, ncols=MODC):
    S = K.S
    with K.phase("mod") as P:
        cc = P.sb([nrows, D], F32)
        scT = P.sb([128, 16, nrows], BF16)
        ps_t = P.ps([128, 16, nrows], F32)
        b_cc, b_ps, b_sc = Buf(), Buf(), Buf()
        S.dma("sp", cc[:], cc9[:, :], [], [b_cc])
        S.do("act", "activation", [b_cc], [b_cc], out=cc[:], in_=cc[:], func=AF.Silu)
        for c in range(16):
            S.do("pe", "transpose", [b_cc], [b_ps], ps_t[:, c, :], cc[:, c * 128:(c + 1) * 128], ident[0:nrows, 0:nrows])
        S.do("dve", "tensor_copy", [b_ps], [b_sc], out=scT[:], in_=ps_t[:])
        wr = Ring([P.sb([128, 16, 512], BF16) for _ in range(3)])
        pr = Ring([P.ps([nrows, 512], F32) for _ in range(2)])
        br = Ring([P.sb([nrows, 512], F32) for _ in range(2)])
        orr = Ring([P.sb([nrows, 512], F32) for _ in range(2)])
        for layer in range(2):
            for n in range(ncols // 512):
                w, bw = wr.next()
                S.dma("pool", w[:], wpiece(mod_w[layer], n * 512, 512), [], [bw])
                bt, bb = br.next()
                S.dma("sp", bt[:], mod_b[layer:layer + 1, n * 512:(n + 1) * 512].broadcast_to([nrows, 512]), [], [bb])
                ps, bp = pr.next()
                for c in range(16):
                    S.do("pe", "matmul", [bw, b_sc], [bp], ps[:], scT[:, c, :], w[:, c, :], start=(c == 0), stop=(c == 15))
                ot, bo = orr.next()
                S.do("dve", "tensor_tensor", [bp, bb], [bo], out=ot[:], in0=ps[:], in1=bt[:], op=ALU.add)
                S.dma("sp", modp[layer, :, n * 512:(n + 1) * 512], ot[:], [bo], [Buf()])


NB = 2
NCORES = 8 // NB


def build(dbg=(), stop_after=None):
    K = KB(dbg, stop_after)
    nc = K.nc
    innames = []

    def inp(name, shape, dt=F32):
        innames.append(name)
        return K.inp(name, shape, dt)

    ident_d = inp("ident", [128, 128])
    x = inp("x", [NB, T, D])
    ctx = inp("ctx", [NB, TCX, D])
    cc = inp("cc", [NB + 1, D])
    mod_w = inp("mod_w", [2, D, 6 * D])
    mod_b = inp("mod_b", [2, 6 * D])
    norm_g = inp("norm_g", [2, 2, D])
    final_g = inp("final_g", [D])
    ab_w_in = inp("ab_w_in", [1, D, 3072])
    ab_w_out = inp("ab_w_out", [1, D, D])
    gm_v_g = inp("gm_v_g", [1, 1024])
    gm_w_s = inp("gm_w_s", [1, 8, 128, 128])
    gm_b_s = inp("gm_b_s", [1, 8, 128])
    w_in = inp("ssd_w_in", [1, D, 10368])
    conv_w = inp("ssd_conv_w", [1, 5, 6144])
    conv_b = inp("ssd_conv_b", [1, 6144])
    dt_bias = inp("ssd_dt_bias", [1, 2, 64])
    a_log = inp("ssd_a_log", [1, 2, 64])
    d_skip = inp("ssd_d", [1, 64])
    ng = inp("ssd_norm_g", [1, 4096])
    w_out = inp("ssd_w_out", [1, 4096, D])
    w_router = inp("moe_w_router", [2, D, NE])
    w_gate = inp("moe_w_gate", [2, NE, D, D])
    w_up = inp("moe_w_up", [2, NE, D, D])
    w_down = inp("moe_w_down", [2, NE, D, D])
    pos = inp("pos", [T, D])
    cs128_d = inp("cs128", [128, 256], BF16)
    dftT_d = inp("dftT", [2, T, T], BF16)
    dftC_d = inp("dftC", [2, TCX, TCX], BF16)
    tris_d = inp("tris", [4, 128, 128])
    iota_d = inp("iota", [128, NSLOT])
    out = nc.dram_tensor("out", [NB, T, D], F32, kind="ExternalOutput").ap()
    modD = K.scratch("modD", [2, NB + 1, 6 * D])
    es = K.es
    with es:
        sems = {e: es.enter_context(nc.semaphore("s_" + e)) for e in ENGS}
        rings = {e: [es.enter_context(nc.semaphore(f"r_{e}{i}")) for i in range(NRING)] for e in ENGS}
        S = K.S = Sched(nc, sems, rings)
        ident = es.enter_context(nc.sbuf_tensor("ident_sb", [128, 128], F32))
        identb = es.enter_context(nc.sbuf_tensor("identb_sb", [128, 128], BF16))
        with K.phase("c0") as P:
            bi = Buf()
            S.dma("sp", ident[:], ident_d[:, :], [], [bi])
            S.do("dve", "tensor_copy", [bi], [Buf()], out=identb[:], in_=ident[:])
        phase_mod(K, cc, mod_w, mod_b, modD, ident, nrows=NB + 1, ncols=6 * D)
        Hs, XE0, XE1, SELT0, SELT1, SELTC0 = [], [], [], [], [], []
        for j in range(NB):
            K.sfx = f"_b{j}"
            K.rows = (j, NB)
            Hs.append(K.scratch("H", [NTOK, D]))
            XE0.append(K.scratch("XE0", [NE, D, NSLOT], BF16))
            SELT0.append(K.scratch("SELT0", [NLAT, 128, 2 * NE, 128], BF16))
            SELTC0.append(K.scratch("SELTC0", [2, CAPC, NE, 128], BF16))
            XE1.append(K.scratch("XE1", [NE, D, CAP], BF16))
            SELT1.append(K.scratch("SELT1", [NLAT, 128, 2 * NE, 128], BF16))
            catT = K.scratch("catT", [D, NTOK], BF16)
            P12 = K.scratch("P12", [8, NTOK, 256], BF16)
            phase_ab(K, 0, x[j], ctx[j], pos, Hs[j], modD, norm_g, ab_w_in[0], ab_w_out[0], gm_v_g[0], gm_w_s[0], gm_b_s[0],
                     cs128_d, dftT_d, dftC_d, catT, P12, ident, identb)
            phase_moe_pre(K, 0, True, Hs[j], modD, norm_g, w_router[0], tris_d, iota_d, ident, identb, XE0[j], SELT0[j], SELTC0[j])
        K.sfx = ""
        YE0 = K.scratch("YE0", [NE, NB, 4, 128, 3, 512], BF16)
        phase_experts(K, lambda e, j: XE0[j][e], w_gate[0], w_up[0], w_down[0], YE0, NSLOT, True, n_exp=NE, NB=NB)
        for j in range(NB):
            K.sfx = f"_b{j}"
            K.rows = (j, NB)
            phase_moe_post(K, 0, True, Hs[j], Hs[j], modD, SELT0[j], SELTC0[j], YE0, j)
            scr = (K.scratch("ZS", [T, 4096], BF16), K.scratch("BCT", [2048, NTOK], BF16), K.scratch("XS", [NTOK, 4096], BF16),
                   K.scratch("BTOK", [NTOK, 1024], BF16), K.scratch("DTS", [NTOK, 128]), K.scratch("YB", [T, 4096]),
                   K.scratch("YT", [4096, T], BF16))
            phase_ssd(K, 1, Hs[j], modD, norm_g, w_in[0], conv_w[0], conv_b[0], dt_bias[0], a_log[0], d_skip[0], ng[0], w_out[0],
                      tris_d, ident, identb, scr)
            phase_moe_pre(K, 1, False, Hs[j], modD, norm_g, w_router[1], tris_d, iota_d, ident, identb, XE1[j], SELT1[j], None)
        K.sfx = ""
        YE1 = K.scratch("YE1", [NE, NB, 4, 128, 2, 512], BF16)
        phase_experts(K, lambda e, j: XE1[j][e], w_gate[1], w_up[1], w_down[1], YE1, CAP, False, n_exp=NE, NB=NB)
        for j in range(NB):
            K.rows = (j, NB)
            phase_moe_post(K, 1, False, Hs[j], Hs[j], modD, SELT1[j], None, YE1, j)
            phase_final(K, Hs[j], final_g, out[j])
    nc._inames = innames
    return nc


_CONST = {}


def host_constants():
    if _CONST:
        return _CONST
    bf = ml_dtypes.bfloat16
    rows, cols, dim = T // 64, 64, D
    quarter = dim // 4
    omega = (1.0 / (np.float32(10000.0) ** (np.arange(quarter, dtype=np.float32) / np.float32(quarter)))).astype(np.float32)
    r = np.repeat(np.arange(rows, dtype=np.float32), cols)[:, None] * omega
    cl = np.tile(np.arange(cols, dtype=np.float32), rows)[:, None] * omega
    _CONST["pos"] = np.concatenate([np.sin(r), np.cos(r), np.sin(cl), np.cos(cl)], axis=-1).astype(np.float32)
    _CONST["ident"] = np.eye(128, dtype=np.float32)

    def dft(n):
        k = np.arange(n, dtype=np.int64)
        ang = 2.0 * np.pi * ((k[:, None] * k[None, :]) % n).astype(np.float64) / n
        return np.cos(ang), np.sin(ang)

    c128, s128 = dft(128)
    _CONST["cs128"] = np.concatenate([c128, s128], axis=1).astype(np.float32).astype(bf)
    cT, sT = dft(T)
    _CONST["dftT"] = np.stack([cT, -sT]).astype(np.float32).astype(bf)
    cC, sC = dft(TCX)
    _CONST["dftC"] = np.stack([cC, -sC]).astype(np.float32).astype(bf)
    k = np.arange(128)
    kk, mm = k[:, None], k[None, :]
    _CONST["tris"] = np.stack([kk < mm, kk <= mm, kk > mm, kk >= mm]).astype(np.float32)
    _CONST["iota"] = np.tile(np.arange(NSLOT, dtype=np.float32)[None, :], (128, 1))
    return _CONST


_NC_CACHE = {}
_SHARED = ("mod_w", "mod_b", "norm_g", "final_g", "ab_w_in", "ab_w_out", "gm_v_g", "gm_w_s", "gm_b_s", "ssd_w_in", "ssd_conv_w",
           "ssd_conv_b", "ssd_dt_bias", "ssd_a_log", "ssd_d", "ssd_norm_g", "ssd_w_out", "moe_w_router", "moe_w_gate", "moe_w_up",
           "moe_w_down")


def make_in_maps(inputs, cores):
    cst = host_constants()
    shared = {k: np.ascontiguousarray(inputs[k]) for k in _SHARED}
    maps = []
    for k in cores:
        m = dict(shared)
        m.update(cst)
        m["x"] = np.ascontiguousarray(inputs["x"][k * NB:(k + 1) * NB])
        m["ctx"] = np.ascontiguousarray(inputs["ctx"][k * NB:(k + 1) * NB])
        m["cc"] = np.ascontiguousarray(np.concatenate([inputs["c"][k * NB:(k + 1) * NB], inputs["c_ctx"][None, :]], axis=0))
        maps.append(m)
    return maps


def kernel(**inputs):
    inputs = {k: np.asarray(v) for k, v in inputs.items()}
    if "nc" not in _NC_CACHE:
        _NC_CACHE["nc"] = build()
    nc = _NC_CACHE["nc"]
    maps = make_in_maps(inputs, range(NCORES))
    res = run_bass_kernel_spmd(nc, maps, core_ids=list(range(NCORES)))
    return np.concatenate([np.asarray(r["out"]) for r in res.results], axis=0).astype(np.float32)
```

```python
import os
import numpy as np
import ml_dtypes
from contextlib import ExitStack
import concourse.bass as bass
import concourse.mybir as mybir
from concourse.bass_utils import run_bass_kernel_spmd

F32 = mybir.dt.float32
BF16 = mybir.dt.bfloat16
AF = mybir.ActivationFunctionType
ALU = mybir.AluOpType
AX = mybir.AxisListType

ENGS = ("pe", "act", "dve", "pool", "sp")
NRING = 8

T = 2048
TCX = 256
NTOK = T + TCX
D = 2048
NTC = NTOK // 128
NLAT = T // 128
EPS = 1e-6
NE = 16
CAP = 256
CAPC = 32
NSLOT = CAP + CAPC


class Buf:
    __slots__ = ("lw", "rs")

    def __init__(self):
        self.lw = None
        self.rs = []


class Op:
    __slots__ = ("eng", "fn", "deps", "signal", "count", "dma", "ring", "rval", "phase")


class Sched:
    def __init__(self, nc, sems, rings):
        self.nc = nc
        self.sems = sems
        self.rings = rings
        self.ops = {e: [] for e in ENGS}
        self.ndma = {e: 0 for e in ENGS}
        self.cnt = {e: 0 for e in ENGS}
        self.waited = {e: {} for e in ENGS}
        self.phase = 0
        self.lastring = {}
        self.bufs = {}

    def B(self, *key):
        b = self.bufs.get(key)
        if b is None:
            b = self.bufs[key] = Buf()
        return b

    def op(self, eng, fn, reads=(), writes=(), dma=False):
        o = Op()
        o.eng, o.fn, o.dma, o.signal, o.count, o.phase = eng, fn, dma, False, 0, self.phase
        o.ring = o.rval = 0
        o.deps = []
        seen = set()
        cand = []
        for b in reads:
            if b.lw is not None:
                cand.append(b.lw)
        for b in writes:
            if b.lw is not None:
                cand.append(b.lw)
            cand.extend(b.rs)
        for d in cand:
            if id(d) in seen or d.phase != self.phase:
                continue
            seen.add(id(d))
            if d.eng == eng and eng == "pe" and not d.dma and not dma:
                continue
            o.deps.append(d)
            d.signal = True
        for b in reads:
            b.rs.append(o)
        for b in writes:
            b.lw = o
            b.rs = []
        if dma:
            i = self.ndma[eng]
            self.ndma[eng] += 1
            o.ring = i % NRING
            o.rval = 16 * (i // NRING + 1)
            self.lastring[(eng, o.ring)] = o.rval
        self.ops[eng].append(o)
        return o

    def do(self, eng, meth, reads, writes, *a, **kw):
        return self.op(eng, lambda e: getattr(e, meth)(*a, **kw), reads, writes)

    def dma(self, eng, out, in_, reads, writes):
        return self.op(eng, lambda e: e.dma_start(out=out, in_=in_), reads, writes, dma=True)

    def _wait(self, e, eh, need):
        w = self.waited[e]
        for key, (sem, val) in need.items():
            if w.get(key, 0) >= val:
                continue
            eh.wait_ge(sem, val)
            w[key] = val

    def emit_engine(self, e, eh, final):
        w = self.waited[e]
        for o in self.ops[e]:
            need = {}
            for d in o.deps:
                if d.dma:
                    key, sem, val = ("r", d.eng, d.ring), self.rings[d.eng][d.ring], d.rval
                else:
                    key, sem, val = ("c", d.eng), self.sems[d.eng], d.count
                if key not in need or need[key][1] < val:
                    need[key] = (sem, val)
            if o.dma and o.rval > 16:
                key = ("r", e, o.ring)
                if key not in need or need[key][1] < o.rval - 16:
                    need[key] = (self.rings[e][o.ring], o.rval - 16)
            self._wait(e, eh, need)
            ins = o.fn(eh)
            if o.dma:
                ins.then_inc(self.rings[e][o.ring], 16)
            elif o.signal:
                ins.then_inc(self.sems[e], 1)
        need = {}
        for p in ENGS:
            if final[p] > 0:
                need[("c", p)] = (self.sems[p], final[p])
        for (p, r), val in self.lastring.items():
            need[("r", p, r)] = (self.rings[p][r], val)
        self._wait(e, eh, need)

    def end_phase(self, block):
        final = {}
        for e in ENGS:
            comp = [o for o in self.ops[e] if not o.dma]
            if comp:
                comp[-1].signal = True
            c = self.cnt[e]
            for o in comp:
                if o.signal:
                    c += 1
                    o.count = c
            self.cnt[e] = c
            final[e] = c
        S = self

        @block.tensor
        def _(eh):
            S.emit_engine("pe", eh, final)

        @block.scalar
        def _(eh):
            S.emit_engine("act", eh, final)

        @block.vector
        def _(eh):
            S.emit_engine("dve", eh, final)

        @block.gpsimd
        def _(eh):
            S.emit_engine("pool", eh, final)

        @block.sync
        def _(eh):
            S.emit_engine("sp", eh, final)

        self.ops = {e: [] for e in ENGS}
        self.phase += 1


class Phase:
    def __init__(self, K, name):
        self.K = K
        K.uid += 1
        self.name = f"{name}u{K.uid}"
        self.es = ExitStack()
        self.n = 0

    def __enter__(self):
        self.es.__enter__()
        return self

    def sb(self, shape, dt, name=None):
        self.n += 1
        return self.es.enter_context(self.K.nc.sbuf_tensor(f"{self.name}_{name or 't'}{self.n}", list(shape), dt))

    def ps(self, shape, dt=F32, name=None):
        self.n += 1
        return self.es.enter_context(self.K.nc.psum_tensor(f"{self.name}_{name or 'p'}{self.n}", list(shape), dt))

    def __exit__(self, *a):
        with self.K.nc.Block() as block:
            self.K.S.end_phase(block)
        return self.es.__exit__(*a)


class Ring:
    def __init__(self, tiles):
        self.tiles = tiles
        self.bufs = [Buf() for _ in tiles]
        self.i = -1

    def next(self):
        self.i = (self.i + 1) % len(self.tiles)
        return self.tiles[self.i], self.bufs[self.i]


class KB:
    def __init__(self, dbg=(), stop_after=None):
        self.dbg = set(dbg)
        self.stop_after = stop_after
        self.nc = bass.Bass("TRN2", target_bir_lowering=False)
        self.es = ExitStack()
        self.dram = {}
        self.uid = 0
        self.rows = (0, 1)
        self.sfx = ""

    def inp(self, name, shape, dt=F32):
        self.dram[name] = self.nc.dram_tensor(name, list(shape), dt, kind="ExternalInput").ap()
        return self.dram[name]

    def scratch(self, name, shape, dt=F32):
        kind = "ExternalOutput" if name in self.dbg else "Internal"
        name = name + self.sfx
        self.dram[name] = self.nc.dram_tensor(name, list(shape), dt, kind=kind).ap()
        return self.dram[name]

    def sbuf(self, es, name, shape, dt):
        self.uid += 1
        return es.enter_context(self.nc.sbuf_tensor(f"{name}u{self.uid}", list(shape), dt))

    def phase(self, name):
        return Phase(self, name)


def wpiece(w2d, n0, ncols, kc=16):
    return w2d.rearrange("(c p) n -> p c n", p=128)[:, :, n0:n0 + ncols]


def load_mod_rows(K, P, modD, layer, which, g_row_ap, nr=2):
    S = K.S
    sh_off = 0 if which == 1 else 3 * D
    sc_off = sh_off + D
    A, Bt, bA, bB = [], [], [], []
    gt = P.sb([128, D], F32)
    bg = Buf()
    S.dma("sp", gt[:], g_row_ap.broadcast_to([128, D]), [], [bg])
    for r in range(nr):
        a = P.sb([128, D], F32)
        b = P.sb([128, D], F32)
        ba, bb = Buf(), Buf()
        S.dma("sp", a[:], modD[layer, K.rows[r]:K.rows[r] + 1, sc_off:sc_off + D].broadcast_to([128, D]), [], [ba])
        S.dma("sp", b[:], modD[layer, K.rows[r]:K.rows[r] + 1, sh_off:sh_off + D].broadcast_to([128, D]), [], [bb])
        S.do("dve", "scalar_tensor_tensor", [ba, bg], [ba], out=a[:], in0=a[:], scalar=1.0, in1=gt[:], op0=ALU.add, op1=ALU.mult)
        A.append(a), Bt.append(b), bA.append(ba), bB.append(bb)
    return A, Bt, bA, bB


def load_gate_rows(K, P, modD, layer, which, nr=2):
    S = K.S
    off = 2 * D if which == 1 else 5 * D
    G, bG = [], []
    for r in range(nr):
        g = P.sb([128, D], F32)
        bg = Buf()
        S.dma("sp", g[:], modD[layer, K.rows[r]:K.rows[r] + 1, off:off + D].broadcast_to([128, D]), [], [bg])
        G.append(g), bG.append(bg)
    return G, bG


def rms_rstd(S, P, hbuf, bh, junk, bj, ssq, rstd, bs, tc, dim):
    S.do("act", "activation", [bh], [bj, bs], out=junk, in_=hbuf, func=AF.Square, accum_out=ssq[:, tc:tc + 1])
    S.do("act", "activation", [bs], [bs], out=rstd[:, tc:tc + 1], in_=ssq[:, tc:tc + 1], func=AF.Sqrt, scale=1.0 / dim, bias=EPS)
    S.do("dve", "reciprocal", [bs], [bs], out=rstd[:, tc:tc + 1], in_=rstd[:, tc:tc + 1])


def phase_ab(K, layer, x, ctx, pos, H, modD, norm_g, w_in, w_out, v_g, w_s, b_s, cs128_d, dftT_d, dftC_d, catT, P12,
             ident, identb):
    S = K.S
    nc = K.nc
    ntc = NTC
    with ExitStack() as outer:
      xnT = K.sbuf(outer, "xnT_ab", [128, 16, NTOK], BF16)
      b_xnT = [Buf() for _ in range(ntc)]
      with K.phase("ab1a") as P:
        A, Bt, bA, bB = load_mod_rows(K, P, modD, layer, 1, norm_g[layer, 0:1, :])
        ssq = P.sb([128, ntc], F32)
        rstd = P.sb([128, ntc], F32)
        hr = Ring([P.sb([128, D], F32) for _ in range(2)])
        pr_ = Ring([P.sb([128, D], F32) for _ in range(2)])
        junk = P.sb([128, D], BF16)
        bj = Buf()
        t1 = P.sb([128, D], F32)
        bt1 = Buf()
        xr = Ring([P.sb([128, D], BF16) for _ in range(2)])
        ptr = Ring([P.ps([128, 8, 128], BF16) for _ in range(2)])
        for tc in range(ntc):
            r = 0 if tc < NLAT else 1
            h, bh = hr.next()
            if tc < NLAT:
                pt, bp = pr_.next()
                S.dma("sp", h[:], x[tc * 128:(tc + 1) * 128, :], [], [bh])
                S.dma("sp", pt[:], pos[tc * 128:(tc + 1) * 128, :], [], [bp])
                S.do("pool", "tensor_tensor", [bh, bp], [bh], out=h[:], in0=h[:], in1=pt[:], op=ALU.add)
            else:
                S.dma("sp", h[:], ctx[(tc - NLAT) * 128:(tc - NLAT + 1) * 128, :], [], [bh])
            S.dma("sp", H[tc * 128:(tc + 1) * 128, :], h[:], [bh], [S.B("H", tc)])
            bs = Buf()
            rms_rstd(S, P, h[:], bh, junk[:], bj, ssq, rstd, bs, tc, D)
            S.do("dve", "scalar_tensor_tensor", [bh, bs, bA[r]], [bt1], out=t1[:], in0=h[:], scalar=rstd[:, tc:tc + 1], in1=A[r][:],
                 op0=ALU.mult, op1=ALU.mult)
            xt, bx = xr.next()
            S.do("dve", "tensor_tensor", [bt1, bB[r]], [bx], out=xt[:], in0=t1[:], in1=Bt[r][:], op=ALU.add)
            for half in range(2):
                ps, bp = ptr.next()
                for j in range(8):
                    c = half * 8 + j
                    S.do("pe", "transpose", [bx], [bp], ps[:, j, :], xt[:, c * 128:(c + 1) * 128], identb[:])
                S.do("act", "copy", [bp], [b_xnT[tc]], out=xnT[:, half * 8:(half + 1) * 8, tc * 128:(tc + 1) * 128], in_=ps[:])
        if "xnT" in K.dbg:
            dd = K.scratch("xnT", [D, NTOK], BF16)
            S.dma("sp", dd.rearrange("(c p) t -> p c t", p=128), xnT[:], b_xnT, [Buf()])
      if K.stop_after == "xn":
        return
      with K.phase("ab1b") as P:
        junk = P.sb([128, 512], BF16)
        bj = Buf()
        wr = Ring([P.sb([128, 16, 512], BF16) for _ in range(2)])
        pmm = Ring([P.ps([128, 512], F32) for _ in range(2)])
        vtok = P.sb([128, ntc, 1024], BF16)
        b_v = [Buf() for _ in range(ntc)]
        vss = P.sb([128, ntc, 2], F32)
        vr = P.sb([128, ntc], F32)
        bvs = Buf()
        for pc in range(2):
            w, bw = wr.next()
            S.dma("pool", w[:], wpiece(w_in, 1024 + pc * 512, 512), [], [bw])
            for tc in range(ntc):
                ps, bp = pmm.next()
                for c in range(16):
                    S.do("pe", "matmul", [bw, b_xnT[tc]], [bp], ps[:], xnT[:, c, tc * 128:(tc + 1) * 128], w[:, c, :],
                         start=(c == 0), stop=(c == 15))
                S.do("act", "activation", [bp], [b_v[tc]], out=vtok[:, tc, pc * 512:(pc + 1) * 512], in_=ps[:], func=AF.Gelu)
                S.do("act", "activation", [b_v[tc]], [bj, bvs], out=junk[:, 0:512], in_=vtok[:, tc, pc * 512:(pc + 1) * 512],
                     func=AF.Square, accum_out=vss[:, tc, pc:pc + 1])
        S.do("dve", "tensor_tensor", [bvs], [bvs], out=vr[:], in0=vss[:, :, 0], in1=vss[:, :, 1], op=ALU.add)
        S.do("act", "activation", [bvs], [bvs], out=vr[:], in_=vr[:], func=AF.Sqrt, scale=1.0 / 1024, bias=EPS)
        S.do("dve", "reciprocal", [bvs], [bvs], out=vr[:], in_=vr[:])
        for tc in range(ntc):
            S.do("dve", "tensor_scalar", [bvs, b_v[tc]], [b_v[tc]], out=vtok[:, tc, :], in0=vtok[:, tc, :], scalar1=vr[:, tc:tc + 1],
                 scalar2=None, op0=ALU.mult)
        wsf = P.sb([128, 8, 128], F32)
        wsT = P.sb([128, 8, 128], BF16)
        vg8 = P.sb([8, 128], F32)
        vgT = P.sb([128, 8], F32)
        bsb = P.sb([128, 8, 128], F32)
        b_ws, b_wsT, b_vg, b_vgT, b_bsb = Buf(), Buf(), Buf(), Buf(), Buf()
        S.dma("sp", wsf[:], w_s.rearrange("g i j -> i g j"), [], [b_ws])
        S.dma("sp", vg8[:], v_g.rearrange("(g p) -> g p", p=128), [], [b_vg])
        S.dma("sp", bsb[:].rearrange("p g i -> p (g i)"), b_s.rearrange("g i -> (g i)").unsqueeze(0).broadcast_to([128, 1024]), [], [b_bsb])
        pss = Ring([P.ps([128, 512], F32) for _ in range(2)])
        pst = pss.tiles[0][:].rearrange("p (j i) -> p j i", j=4)
        b_pst = pss.bufs[0]
        for hf in range(2):
            for j in range(4):
                g = hf * 4 + j
                S.do("pe", "transpose", [b_ws], [b_pst], pst[:, j, :], wsf[:, g, :], ident[:])
            S.do("dve", "tensor_copy", [b_pst], [b_wsT], out=wsT[:, hf * 4:(hf + 1) * 4, :], in_=pst[:])
        S.do("pe", "transpose", [b_vg], [b_pst], pst[:, 0, 0:8], vg8[:], ident[0:8, 0:8])
        S.do("dve", "tensor_copy", [b_pst], [b_vgT], out=vgT[:], in_=pst[:, 0, 0:8])
        ur = Ring([P.sb([128, 512], F32) for _ in range(2)])
        sr = Ring([P.sb([128, 512], F32) for _ in range(2)])
        yr = Ring([P.sb([128, 512], BF16) for _ in range(2)])
        ttiles = [(i * 512, 512) for i in range(4)] + [(T, TCX)]
        for pc in range(2):
            w, bw = wr.next()
            S.dma("pool", w[:], wpiece(w_in, pc * 512, 512), [], [bw])
            for oc in range(4):
                g = pc * 4 + oc
                for (t0, tn) in ttiles:
                    tcs = list(range(t0 // 128, (t0 + tn) // 128))
                    ps, bp = pmm.next()
                    for c in range(16):
                        S.do("pe", "matmul", [bw] + [b_xnT[q] for q in tcs], [bp], ps[:, 0:tn], w[:, c, oc * 128:(oc + 1) * 128],
                             xnT[:, c, t0:t0 + tn], start=(c == 0), stop=(c == 15))
                    ut, bu = ur.next()
                    S.do("act", "activation", [bp], [bu], out=ut[:, 0:tn], in_=ps[:, 0:tn], func=AF.Gelu)
                    ps2, bp2 = pss.next()
                    for k, q in enumerate(tcs):
                        S.do("pe", "matmul", [b_v[q], b_wsT], [bp2], ps2[:, k * 128:(k + 1) * 128], vtok[:, q, g * 128:(g + 1) * 128],
                             wsT[:, g, :], start=True, stop=True)
                    st, bs_ = sr.next()
                    nk = len(tcs)
                    S.do("dve", "scalar_tensor_tensor", [bp2, b_vgT, b_bsb], [bs_], out=st[:, 0:tn].rearrange("p (k i) -> p k i", k=nk),
                         in0=ps2[:, 0:tn].rearrange("p (k i) -> p k i", k=nk), scalar=vgT[:, g:g + 1],
                         in1=bsb[:, g:g + 1, :].broadcast_to([128, nk, 128]), op0=ALU.mult, op1=ALU.add)
                    yt, by = yr.next()
                    S.do("dve", "tensor_tensor", [bs_, bu], [by], out=yt[:, 0:tn], in0=st[:, 0:tn], in1=ut[:, 0:tn], op=ALU.mult)
                    S.dma("sp", catT[g * 128:(g + 1) * 128, t0:t0 + tn], yt[:, 0:tn], [by], [S.B("catT", g, t0)])
        cs = P.sb([128, 256], BF16)
        b_cs = Buf()
        S.dma("sp", cs[:], cs128_d[:, :], [], [b_cs])
        zr = Ring([P.sb([128, 512], BF16) for _ in range(2)])
        p12r = Ring([P.sb([128, 4, 256], BF16) for _ in range(2)])
        pz = Ring([P.ps([128, 2, 256], F32) for _ in range(2)])
        for pc in range(2):
            w, bw = wr.next()
            S.dma("pool", w[:], wpiece(w_in, 2048 + pc * 512, 512), [], [bw])
            for oc in range(4):
                g = pc * 4 + oc
                for (t0, tn) in ttiles:
                    tcs = list(range(t0 // 128, (t0 + tn) // 128))
                    ps, bp = pmm.next()
                    for c in range(16):
                        S.do("pe", "matmul", [bw] + [b_xnT[q] for q in tcs], [bp], ps[:, 0:tn], w[:, c, oc * 128:(oc + 1) * 128],
                             xnT[:, c, t0:t0 + tn], start=(c == 0), stop=(c == 15))
                    zt, bz = zr.next()
                    S.do("act", "copy", [bp], [bz], out=zt[:, 0:tn], in_=ps[:, 0:tn])
                    pt, bpt = p12r.next()
                    for k2 in range(0, len(tcs), 2):
                        psz, bpz = pz.next()
                        for k in range(k2, k2 + 2):
                            S.do("pe", "matmul", [bz, b_cs], [bpz], psz[:, k - k2, :], zt[:, k * 128:(k + 1) * 128], cs[:],
                                 start=True, stop=True)
                        S.do("dve", "tensor_copy", [bpz], [bpt], out=pt[:, k2:k2 + 2, :], in_=psz[:])
                    nk = len(tcs)
                    S.dma("sp", P12[g, t0:t0 + tn, :].rearrange("(k p) n -> p k n", p=128), pt[:, 0:nk, :], [bpt], [S.B("P12", g, t0)])
    if K.stop_after == "ab1":
        return
    scale = 1.0 / float(np.sqrt(T * 128.0))
    scale_c = 1.0 / float(np.sqrt(TCX * 128.0))
    with K.phase("ab3") as P:
        p12 = P.sb([128, 8, NLAT, 256], BF16)
        b_p12 = Buf()
        for g in range(8):
            S.dma("sp", p12[:, g, :, :], P12[g, 0:T, :].rearrange("(k p) n -> p k n", p=128), [], [b_p12])
        p12c = P.sb([128, 8, 2, 256], BF16)
        b_p12c = Buf()
        for g in range(8):
            S.dma("sp", p12c[:, g, :, :], P12[g, T:NTOK, :].rearrange("(k p) n -> p k n", p=128), [], [b_p12c])
        dr = Ring([P.sb([128, 2, NLAT, 512], BF16) for _ in range(2)])
        dc = P.sb([128, 2, 2, 256], BF16)
        b_dc = Buf()
        for m in range(2):
            S.dma("sp", dc[:, m, :, :], dftC_d[m].rearrange("(k p) t -> p k t", p=128), [], [b_dc])
        pf = Ring([P.ps([128, 512], F32) for _ in range(3)])
        yr = Ring([P.sb([128, 512], BF16) for _ in range(3)])
        for tt in range(4):
            dt_, bd = dr.next()
            for m in range(2):
                S.dma("sp", dt_[:, m, :, :], dftT_d[m].rearrange("(k p) t -> p k t", p=128)[:, :, tt * 512:(tt + 1) * 512], [], [bd])
            for g in range(8):
                ps, bp = pf.next()
                n = 0
                for m in range(2):
                    for k in range(NLAT):
                        S.do("pe", "matmul", [bd, b_p12], [bp], ps[:], p12[:, g, k, m * 128:(m + 1) * 128], dt_[:, m, k, :],
                             start=(n == 0), stop=(n == 2 * NLAT - 1))
                        n += 1
                yt, by = yr.next()
                S.do("act", "mul", [bp], [by], out=yt[:], in_=ps[:], mul=scale)
                S.dma("sp", catT[1024 + g * 128:1024 + (g + 1) * 128, tt * 512:(tt + 1) * 512], yt[:], [by], [Buf()])
        for g in range(8):
            ps, bp = pf.next()
            n = 0
            for m in range(2):
                for k in range(2):
                    S.do("pe", "matmul", [b_dc, b_p12c], [bp], ps[:, 0:TCX], p12c[:, g, k, m * 128:(m + 1) * 128], dc[:, m, k, :],
                         start=(n == 0), stop=(n == 3))
                    n += 1
            yt, by = yr.next()
            S.do("act", "mul", [bp], [by], out=yt[:, 0:TCX], in_=ps[:, 0:TCX], mul=scale_c)
            S.dma("sp", catT[1024 + g * 128:1024 + (g + 1) * 128, T:NTOK], yt[:, 0:TCX], [by], [Buf()])
    if K.stop_after == "ab3":
        return
    phase_outproj(K, layer, catT, 16, w_out, H, modD, NTC)


def phase_outproj(K, layer, yT_d, kc, w_out, H, modD, ntc):
    S = K.S
    with K.phase("oproj") as P:
        nr = 2 if ntc > NLAT else 1
        G, bG = load_gate_rows(K, P, modD, layer, 1, nr)
        ntok = ntc * 128
        half = ntok // 2 if kc > 16 else ntok
        wr = Ring([P.sb([128, 16, 512], BF16) for _ in range(4 if kc > 16 else 2)])
        pm = Ring([P.ps([128, 512], F32) for _ in range(3)])
        hr = Ring([P.sb([128, 512], F32) for _ in range(3)])
        tr = Ring([P.sb([128, 512], F32) for _ in range(3)])
        yT = P.sb([128, kc, half], BF16)
        b_y = Buf()
        for t0 in range(0, ntok, half):
            for c0 in range(0, kc, 8):
                S.dma("sp", yT[:, c0:c0 + 8, :], yT_d.rearrange("(c p) t -> p c t", p=128)[:, c0:c0 + 8, t0:t0 + half], [], [b_y])
            for n in range(4):
                ws = []
                for kh in range(kc // 16):
                    w, bw = wr.next()
                    S.dma("pool", w[:], w_out.rearrange("(c p) n -> p c n", p=128)[:, kh * 16:(kh + 1) * 16, n * 512:(n + 1) * 512], [], [bw])
                    ws.append((w, bw))
                for tcl in range(half // 128):
                    tc = t0 // 128 + tcl
                    r = 0 if tc < NLAT else 1
                    ps, bp = pm.next()
                    for c in range(kc):
                        w, bw = ws[c // 16]
                        S.do("pe", "matmul", [bw, b_y], [bp], ps[:], yT[:, c, tcl * 128:(tcl + 1) * 128], w[:, c % 16, :],
                             start=(c == 0), stop=(c == kc - 1))
                    ht, bh = hr.next()
                    hb = S.B("H", tc, n)
                    S.dma("sp", ht[:], H[tc * 128:(tc + 1) * 128, n * 512:(n + 1) * 512], [hb], [bh])
                    tt, bt = tr.next()
                    S.do("dve", "tensor_tensor", [bp, bG[r]], [bt], out=tt[:], in0=ps[:], in1=G[r][:, n * 512:(n + 1) * 512], op=ALU.mult)
                    S.do("pool", "tensor_tensor", [bt, bh], [bh], out=ht[:], in0=tt[:], in1=ht[:], op=ALU.add)
                    S.dma("sp", H[tc * 128:(tc + 1) * 128, n * 512:(n + 1) * 512], ht[:], [bh], [hb])


def phase_moe_pre(K, layer, has_ctx, H, modD, norm_g, w_router, tris_d, iota_d, ident, identb, XE, SELT, SELTC):
    S = K.S
    nc = K.nc
    ntc = NTC if has_ctx else NLAT
    ntok = ntc * 128
    nslot = NSLOT if has_ctx else CAP
    with ExitStack() as outer:
        xm = K.sbuf(outer, "xm", [128, ntc, D], BF16)
        b_xm = [Buf() for _ in range(ntc)]
        R = K.sbuf(outer, "R", [128, ntc, NE], F32)
        Mk = K.sbuf(outer, "Mk", [128, ntc, NE], F32)
        GM = K.sbuf(outer, "GM", [128, ntc, NE], F32)
        with K.phase(f"e1a{layer}") as P:
            A, Bt, bA, bB = load_mod_rows(K, P, modD, layer, 2, norm_g[layer, 1:2, :], 2 if has_ctx else 1)
            wr32 = P.sb([128, 16, NE], F32)
            b_wr = Buf()
            S.dma("sp", wr32[:], w_router.rearrange("(c p) e -> p c e", p=128), [], [b_wr])
            ssq = P.sb([128, ntc], F32)
            rstd = P.sb([128, ntc], F32)
            hr = Ring([P.sb([128, D], F32) for _ in range(2)])
            t1r = Ring([P.sb([128, D], F32) for _ in range(2)])
            junk = P.sb([128, D], BF16)
            bj = Buf()
            xtr = Ring([P.sb([128, 16, 128], F32) for _ in range(2)])
            ptr = Ring([P.ps([128, 4, 128], F32) for _ in range(4)])
            plg = Ring([P.ps([128, NE], F32) for _ in range(2)])
            Pt = P.sb([128, ntc, NE], F32)
            b_Pt = [Buf() for _ in range(ntc)]
            mx = P.sb([128, ntc], F32)
            sm = P.sb([128, ntc], F32)
            for tc in range(ntc):
                r = 0 if tc < NLAT else 1
                h, bh = hr.next()
                S.dma("sp", h[:], H[tc * 128:(tc + 1) * 128, :], [], [bh])
                bs = Buf()
                rms_rstd(S, P, h[:], bh, junk[:], bj, ssq, rstd, bs, tc, D)
                t1, bt1 = t1r.next()
                S.do("dve", "scalar_tensor_tensor", [bh, bs, bA[r]], [bt1], out=t1[:], in0=h[:], scalar=rstd[:, tc:tc + 1], in1=A[r][:],
                     op0=ALU.mult, op1=ALU.mult)
                S.do("dve", "tensor_tensor", [bt1, bB[r]], [bt1], out=t1[:], in0=t1[:], in1=Bt[r][:], op=ALU.add)
                S.do("act", "copy", [bt1], [b_xm[tc]], out=xm[:, tc, :], in_=t1[:])
                xt, bx = xtr.next()
                for q in range(4):
                    ps, bp = ptr.next()
                    for j in range(4):
                        c = q * 4 + j
                        S.do("pe", "transpose", [bt1], [bp], ps[:, j, :], t1[:, c * 128:(c + 1) * 128], ident[:])
                    S.do("act" if q % 2 else "dve", "copy" if q % 2 else "tensor_copy", [bp], [bx], out=xt[:, q * 4:(q + 1) * 4, :], in_=ps[:])
                pl, bl = plg.next()
                for c in range(16):
                    S.do("pe", "matmul", [bx, b_wr], [bl], pl[:], xt[:, c, :], wr32[:, c, :], start=(c == 0), stop=(c == 15))
                bm = Buf()
                S.do("dve", "tensor_reduce", [bl], [bm], out=mx[:, tc:tc + 1], in_=pl[:], axis=AX.X, op=ALU.max)
                S.do("dve", "tensor_scalar", [bm], [bm], out=mx[:, tc:tc + 1], in0=mx[:, tc:tc + 1], scalar1=-1.0, scalar2=None, op0=ALU.mult)
                S.do("act", "activation", [bl, bm], [b_Pt[tc], bm], out=Pt[:, tc, :], in_=pl[:], func=AF.Exp, bias=mx[:, tc:tc + 1], scale=1.0,
                     accum_out=sm[:, tc:tc + 1])
                S.do("dve", "reciprocal", [bm], [bm], out=sm[:, tc:tc + 1], in_=sm[:, tc:tc + 1])
                S.do("dve", "tensor_scalar", [bm, b_Pt[tc]], [b_Pt[tc]], out=Pt[:, tc, :], in0=Pt[:, tc, :], scalar1=sm[:, tc:tc + 1], scalar2=None,
                     op0=ALU.mult)
            PT = P.sb([NE, ntok], F32)
            b_PT = Buf()
            for q in range(0, ntc, 4):
                ps, bp = ptr.next()
                nq = min(4, ntc - q)
                for j in range(nq):
                    S.do("pe", "transpose", [b_Pt[q + j]], [bp], ps[0:NE, j, :], Pt[:, q + j, :], ident[:])
                S.do("dve", "tensor_copy", [bp], [b_PT], out=PT[:, q * 128:(q + nq) * 128], in_=ps[0:NE, 0:nq, :])
            wa = P.sb([NE, T], F32)
            wb = P.sb([NE, T], F32)
            m8 = P.sb([NE, 8], F32)
            tau = P.sb([NE, 2], F32)
            b_wa, b_wb, b_m8, b_tau = Buf(), Buf(), Buf(), Buf()
            seqs = [(0, T, CAP, 0)] + ([(T, TCX, CAPC, 1)] if has_ctx else [])
            maskT = P.sb([NE, ntok], F32)
            b_mT = Buf()
            for (t0, tn, cap, si) in seqs:
                cur, bc, oth, bo = PT[:, t0:t0 + tn], b_PT, wa[:, 0:tn], b_wa
                for rd in range(cap // 8):
                    S.do("dve", "max", [bc], [b_m8], out=m8[:], in_=cur)
                    if rd < cap // 8 - 1:
                        S.do("dve", "match_replace", [bc, b_m8], [bo], out=oth, in_to_replace=m8[:], in_values=cur, imm_value=-1.0)
                        if rd == 0:
                            cur, bc, oth, bo = wa[:, 0:tn], b_wa, wb[:, 0:tn], b_wb
                        else:
                            cur, bc, oth, bo = oth, bo, cur, bc
                S.do("dve", "tensor_copy", [b_m8], [b_tau], out=tau[:, si:si + 1], in_=m8[:, 7:8])
                S.do("dve", "tensor_scalar", [b_PT, b_tau], [b_mT], out=maskT[:, t0:t0 + tn], in0=PT[:, t0:t0 + tn], scalar1=tau[:, si:si + 1],
                     scalar2=None, op0=ALU.is_ge)
            pk = P.ps([128, ntc, NE], F32)
            b_pk = Buf()
            for tc in range(ntc):
                S.do("pe", "transpose", [b_mT], [b_pk], pk[:, tc, :], maskT[:, tc * 128:(tc + 1) * 128], ident[0:NE, 0:NE])
            b_Mk, b_GM, b_R = Buf(), Buf(), Buf()
            S.do("dve", "tensor_copy", [b_pk], [b_Mk], out=Mk[:], in_=pk[:])
            S.do("dve", "tensor_tensor", [b_Mk] + b_Pt, [b_GM], out=GM[:], in0=Mk[:], in1=Pt[:], op=ALU.mult)
            Mb = P.sb([128, ntc, NE], BF16)
            b_Mb = Buf()
            S.do("dve", "tensor_copy", [b_Mk], [b_Mb], out=Mb[:], in_=Mk[:])
            trf = P.sb([128, 128], F32)
            trb = P.sb([128, 128], BF16)
            oneb = P.sb([128, 128], BF16)
            b_tr, b_one = Buf(), Buf()
            S.dma("sp", trf[:], tris_d[0], [], [b_tr])
            S.do("dve", "tensor_copy", [b_tr], [b_tr], out=trb[:], in_=trf[:])
            S.do("dve", "memset", [], [b_one], oneb[:], 1.0)
            pr2 = P.ps([128, ntc, NE], F32)
            b_pr2 = Buf()
            for tc in range(ntc):
                first = 0 if tc < NLAT else NLAT
                for t2 in range(first, tc + 1):
                    S.do("pe", "matmul", [b_Mb, b_tr, b_one], [b_pr2], pr2[:, tc, :], (trb if t2 == tc else oneb)[:], Mb[:, t2, :],
                         start=(t2 == first), stop=(t2 == tc))
            S.do("dve", "tensor_copy", [b_pr2], [b_R], out=R[:], in_=pr2[:])
            if has_ctx:
                S.do("dve", "tensor_scalar", [b_R], [b_R], out=R[:, NLAT:, :], in0=R[:, NLAT:, :], scalar1=float(CAP), scalar2=None, op0=ALU.add)
            if "Pt" in K.dbg:
                dd = K.scratch("Pt", [ntok, NE])
                S.dma("sp", dd.rearrange("(k p) e -> p k e", p=128), Pt[:], b_Pt, [Buf()])
                dd = K.scratch("Mk", [ntok, NE])
                S.dma("sp", dd.rearrange("(k p) e -> p k e", p=128), Mk[:], [b_Mk], [Buf()])
                dd = K.scratch("Rk", [ntok, NE])
                S.dma("sp", dd.rearrange("(k p) e -> p k e", p=128), R[:], [b_R], [Buf()])
        if K.stop_after == "e1a":
            return
        with K.phase(f"e1b{layer}") as P:
            iot = P.sb([128, NSLOT], F32)
            b_io = Buf()
            S.dma("sp", iot[:], iota_d[:, :], [], [b_io])
            selr = Ring([P.sb([128, NLAT, CAP], BF16) for _ in range(2)])
            sgr = Ring([P.sb([128, NLAT, CAP], BF16) for _ in range(2)])
            selc = Ring([P.sb([128, 2, CAPC], BF16) for _ in range(2)])
            sgc = Ring([P.sb([128, 2, CAPC], BF16) for _ in range(2)])
            xer = Ring([P.sb([128, 16, nslot], BF16) for _ in range(2)])
            str_ = Ring([P.sb([128, NLAT, 2, 128], BF16) for _ in range(2)])
            stc = Ring([P.sb([CAPC, 2, 128], BF16) for _ in range(2)])
            pg = Ring([P.ps([128, 512], F32) for _ in range(3)])
            pt2 = Ring([P.ps([128, 8, 128], BF16) for _ in range(3)])
            for e in range(NE):
                sel, bsel = selr.next()
                sg, bsg = sgr.next()
                for tc in range(NLAT):
                    S.do("dve", "tensor_scalar", [b_io], [bsel], out=sel[:, tc, :], in0=iot[:, 0:CAP], scalar1=R[:, tc, e:e + 1],
                         scalar2=Mk[:, tc, e:e + 1], op0=ALU.is_equal, op1=ALU.mult)
                    S.do("pool", "tensor_scalar", [b_io], [bsg], out=sg[:, tc, :], in0=iot[:, 0:CAP], scalar1=R[:, tc, e:e + 1],
                         scalar2=GM[:, tc, e:e + 1], op0=ALU.is_equal, op1=ALU.mult)
                if has_ctx:
                    sc_, bsc = selc.next()
                    sgc_, bsgc = sgc.next()
                    for k in range(2):
                        S.do("dve", "tensor_scalar", [b_io], [bsc], out=sc_[:, k, :], in0=iot[:, CAP:NSLOT], scalar1=R[:, NLAT + k, e:e + 1],
                             scalar2=Mk[:, NLAT + k, e:e + 1], op0=ALU.is_equal, op1=ALU.mult)
                        S.do("dve", "tensor_scalar", [b_io], [bsgc], out=sgc_[:, k, :], in0=iot[:, CAP:NSLOT], scalar1=R[:, NLAT + k, e:e + 1],
                             scalar2=GM[:, NLAT + k, e:e + 1], op0=ALU.is_equal, op1=ALU.mult)
                xe, bxe = xer.next()
                for c in range(16):
                    ps, bp = pg.next()
                    for tc in range(NLAT):
                        S.do("pe", "matmul", [bsel, b_xm[tc]], [bp], ps[:, 0:CAP], xm[:, tc, c * 128:(c + 1) * 128], sel[:, tc, :],
                             start=(tc == 0), stop=(tc == NLAT - 1))
                    if has_ctx:
                        for k in range(2):
                            S.do("pe", "matmul", [bsc, b_xm[NLAT + k]], [bp], ps[:, CAP:NSLOT], xm[:, NLAT + k, c * 128:(c + 1) * 128], sc_[:, k, :],
                                 start=(k == 0), stop=(k == 1))
                    S.do("act", "copy", [bp], [bxe], out=xe[:, c, :], in_=ps[:, 0:nslot])
                S.dma("sp", XE[e].rearrange("(c p) s -> p c s", p=128)[:, :, 0:nslot], xe[:], [bxe], [S.B("XE", layer, e)])
                st, bst = str_.next()
                for q in range(0, NLAT, 4):
                    ps, bp = pt2.next()
                    for j in range(4):
                        for s2 in range(2):
                            S.do("pe", "transpose", [bsg], [bp], ps[:, j * 2 + s2, :], sg[:, q + j, s2 * 128:(s2 + 1) * 128], identb[:])
                    S.do("dve", "tensor_copy", [bp], [bst], out=st[:, q:q + 4, :, :], in_=ps[:].rearrange("p (j s) t -> p j s t", s=2))
                S.dma("sp", SELT[:, :, e * 2:(e + 1) * 2, :].rearrange("k p s t -> p k s t"), st[:], [bst], [S.B("SELT", layer, e)])
                if has_ctx:
                    stc_, bstc = stc.next()
                    ps, bp = pt2.next()
                    for k in range(2):
                        S.do("pe", "transpose", [bsgc], [bp], ps[0:CAPC, k, :], sgc_[:, k, :], identb[:])
                    S.do("dve", "tensor_copy", [bp], [bstc], out=stc_[:], in_=ps[0:CAPC, 0:2, :])
                    S.dma("sp", SELTC[:, :, e, :].rearrange("k p t -> p k t"), stc_[:], [bstc], [S.B("SELTC", layer, e)])


def modulate_xnT(K, pname, layer, H, modD, g_row, xnT, b_xnT, identb, ntc):
    S = K.S
    with K.phase(pname) as P:
        A, Bt, bA, bB = load_mod_rows(K, P, modD, layer, 1, g_row)
        ssq = P.sb([128, ntc], F32)
        rstd = P.sb([128, ntc], F32)
        hr = Ring([P.sb([128, D], F32) for _ in range(2)])
        junk = P.sb([128, D], BF16)
        bj = Buf()
        t1 = P.sb([128, D], F32)
        bt1 = Buf()
        xr = Ring([P.sb([128, D], BF16) for _ in range(2)])
        ptr = Ring([P.ps([128, 8, 128], BF16) for _ in range(2)])
        for tc in range(ntc):
            r = 0 if tc < NLAT else 1
            h, bh = hr.next()
            S.dma("sp", h[:], H[tc * 128:(tc + 1) * 128, :], [], [bh])
            bs = Buf()
            rms_rstd(S, P, h[:], bh, junk[:], bj, ssq, rstd, bs, tc, D)
            S.do("dve", "scalar_tensor_tensor", [bh, bs, bA[r]], [bt1], out=t1[:], in0=h[:], scalar=rstd[:, tc:tc + 1], in1=A[r][:],
                 op0=ALU.mult, op1=ALU.mult)
            xt, bx = xr.next()
            S.do("dve", "tensor_tensor", [bt1, bB[r]], [bx], out=xt[:], in0=t1[:], in1=Bt[r][:], op=ALU.add)
            for half in range(2):
                ps, bp = ptr.next()
                for j in range(8):
                    c = half * 8 + j
                    S.do("pe", "transpose", [bx], [bp], ps[:, j, :], xt[:, c * 128:(c + 1) * 128], identb[:])
                S.do("act", "copy", [bp], [b_xnT[tc]], out=xnT[:, half * 8:(half + 1) * 8, tc * 128:(tc + 1) * 128], in_=ps[:])


def phase_ssd(K, layer, H, modD, norm_g, w_in, conv_w, conv_b, dt_bias, a_log, d_skip, ng, w_out, tris_d, ident, identb, scr):
    S = K.S
    nc = K.nc
    ZS, BCT, XS, BTOK, DTS, YB, YT = scr
    ntc = NTC
    ttiles = [(i * 512, 512) for i in range(4)] + [(T, TCX)]
    with ExitStack() as outer:
        xnT = K.sbuf(outer, "xnT_ssd", [128, 16, NTOK], BF16)
        b_xnT = [Buf() for _ in range(ntc)]
        modulate_xnT(K, "s1", layer, H, modD, norm_g[layer, 0:1, :], xnT, b_xnT, identb, ntc)
        with K.phase("s2") as P:
            wr = Ring([P.sb([128, 16, 512], BF16) for _ in range(2)])
            pmm = Ring([P.ps([128, 512], F32) for _ in range(3)])
            ptr = Ring([P.ps([128, 8, 128], BF16) for _ in range(2)])
            pst = P.ps([128, 512], F32)
            b_pst = Buf()
            cwr = P.sb([120, 2, 128], F32)
            cbr = P.sb([48, 128], F32)
            cwT = P.sb([128, 5, 48], F32)
            cbT = P.sb([128, 48], F32)
            b_cwr, b_cw = Buf(), Buf()
            cw_rows = conv_w.rearrange("k (c p) -> (k c) p", p=128)
            for hf in range(2):
                S.dma("sp", cwr[:, hf, :], cw_rows[hf * 120:(hf + 1) * 120, :], [], [b_cwr])
            S.dma("sp", cbr[:], conv_b.rearrange("(c p) -> c p", p=128), [], [b_cwr])
            for hf in range(2):
                S.do("pe", "transpose", [b_cwr], [b_pst], pst[:, hf * 120:(hf + 1) * 120], cwr[:, hf, :], ident[0:120, 0:120])
            S.do("pe", "transpose", [b_cwr], [b_pst], pst[:, 240:288], cbr[:], ident[0:48, 0:48])
            S.do("dve", "tensor_copy", [b_pst], [b_cw], out=cwT[:].rearrange("p k c -> p (k c)"), in_=pst[:, 0:240])
            S.do("dve", "tensor_copy", [b_pst], [b_cw], out=cbT[:], in_=pst[:, 240:288])
            zr = Ring([P.sb([128, 512], BF16) for _ in range(3)])
            for pc in range(8):
                w, bw = wr.next()
                S.dma("pool", w[:], wpiece(w_in, pc * 512, 512), [], [bw])
                for tc in range(NLAT):
                    ps, bp = pmm.next()
                    for c in range(16):
                        S.do("pe", "matmul", [bw, b_xnT[tc]], [bp], ps[:], xnT[:, c, tc * 128:(tc + 1) * 128], w[:, c, :],
                             start=(c == 0), stop=(c == 15))
                    zt, bz = zr.next()
                    S.do("act", "activation", [bp], [bz], out=zt[:], in_=ps[:], func=AF.Silu)
                    S.dma("sp", ZS[tc * 128:(tc + 1) * 128, pc * 512:(pc + 1) * 512], zt[:], [bz], [Buf()])
            rbr = Ring([P.sb([128, NTOK + 8], F32) for _ in range(2)])
            accr = Ring([P.sb([128, NTOK], F32) for _ in range(2)])
            obr = Ring([P.sb([128, NTOK], BF16) for _ in range(2)])
            tsr = Ring([P.sb([128, ntc, 128], BF16) for _ in range(2)])
            for rb, bb in zip(rbr.tiles, rbr.bufs):
                S.do("dve", "memset", [], [bb], rb[:], 0.0)
            LOFF, COFF = 2, T + 6
            for pc in range(12):
                w, bw = wr.next()
                S.dma("pool", w[:], wpiece(w_in, 4096 + pc * 512, 512), [], [bw])
                for oc in range(4):
                    ch = pc * 4 + oc
                    rb, brb = rbr.next()
                    for (t0, tn) in ttiles:
                        tcs = list(range(t0 // 128, (t0 + tn) // 128))
                        ps, bp = pmm.next()
                        for c in range(16):
                            S.do("pe", "matmul", [bw] + [b_xnT[q] for q in tcs], [bp], ps[:, 0:tn], w[:, c, oc * 128:(oc + 1) * 128],
                                 xnT[:, c, t0:t0 + tn], start=(c == 0), stop=(c == 15))
                        off = LOFF + t0 if t0 < T else COFF
                        S.do("act", "copy", [bp], [brb], out=rb[:, off:off + tn], in_=ps[:, 0:tn])
                    acc, bacc = accr.next()
                    for (a0, an, ro) in ((0, T, LOFF), (T, TCX, COFF)):
                        for k in range(5):
                            src = rb[:, ro - 2 + k:ro - 2 + k + an]
                            if k == 0:
                                S.do("dve", "tensor_scalar", [brb, b_cw], [bacc], out=acc[:, a0:a0 + an], in0=src, scalar1=cwT[:, k, ch:ch + 1],
                                     scalar2=None, op0=ALU.mult)
                            else:
                                S.do("dve", "scalar_tensor_tensor", [brb, b_cw, bacc], [bacc], out=acc[:, a0:a0 + an], in0=src,
                                     scalar=cwT[:, k, ch:ch + 1], in1=acc[:, a0:a0 + an], op0=ALU.mult, op1=ALU.add)
                    ob, bob = obr.next()
                    S.do("act", "activation", [bacc, b_cw], [bob], out=ob[:], in_=acc[:], func=AF.Silu, bias=cbT[:, ch:ch + 1], scale=1.0)
                    if ch >= 32:
                        S.dma("sp", BCT[(ch - 32) * 128:(ch - 31) * 128, :], ob[:], [bob], [Buf()])
                    if ch < 40:
                        ts, bts = tsr.next()
                        for q in range(0, ntc, 8):
                            nq = min(8, ntc - q)
                            ps, bp = ptr.next()
                            for j in range(nq):
                                S.do("pe", "transpose", [bob], [bp], ps[:, j, :], ob[:, (q + j) * 128:(q + j + 1) * 128], identb[:])
                            S.do("dve", "tensor_copy", [bp], [bts], out=ts[:, q:q + nq, :], in_=ps[:, 0:nq, :])
                        if ch < 32:
                            S.dma("sp", XS[:, ch * 128:(ch + 1) * 128].rearrange("(k p) n -> p k n", p=128), ts[:], [bts], [Buf()])
                        else:
                            S.dma("sp", BTOK[:, (ch - 32) * 128:(ch - 31) * 128].rearrange("(k p) n -> p k n", p=128), ts[:], [bts], [Buf()])
            wdt = P.sb([128, 16, 128], BF16)
            b_wdt = Buf()
            S.dma("pool", wdt[:], wpiece(w_in, 10240, 128), [], [b_wdt])
            dtb = P.sb([128, 128], F32)
            b_dtb = Buf()
            S.dma("sp", dtb[:], dt_bias.rearrange("a h -> (a h)").unsqueeze(0).broadcast_to([128, 128]), [], [b_dtb])
            xbr = Ring([P.sb([128, 128], F32) for _ in range(2)])
            abr = Ring([P.sb([128, 128], F32) for _ in range(2)])
            for tc in range(ntc):
                ps, bp = pmm.next()
                for c in range(16):
                    S.do("pe", "matmul", [b_wdt, b_xnT[tc]], [bp], ps[:, 0:128], xnT[:, c, tc * 128:(tc + 1) * 128], wdt[:, c, :],
                         start=(c == 0), stop=(c == 15))
                xb, bxb = xbr.next()
                ab, bab = abr.next()
                S.do("dve", "tensor_tensor", [bp, b_dtb], [bxb], out=xb[:], in0=ps[:, 0:128], in1=dtb[:], op=ALU.add)
                S.do("dve", "scalar_tensor_tensor", [bxb], [bab], out=ab[:], in0=xb[:], scalar=-1.0, in1=xb[:], op0=ALU.mult, op1=ALU.max)
                S.do("act", "activation", [bab], [bab], out=ab[:], in_=ab[:], func=AF.Exp, scale=-1.0)
                S.do("act", "activation", [bab], [bab], out=ab[:], in_=ab[:], func=AF.Ln, bias=1.0, scale=1.0)
                S.do("dve", "scalar_tensor_tensor", [bxb, bab], [bxb], out=xb[:], in0=xb[:], scalar=0.0, in1=ab[:], op0=ALU.max, op1=ALU.add)
                S.dma("sp", DTS[tc * 128:(tc + 1) * 128, :], xb[:], [bxb], [Buf()])
    if K.stop_after == "s2":
        return
    with K.phase("s3") as P:
        trf = P.sb([128, 4, 128], F32)
        b_trf = Buf()
        S.dma("sp", trf[:], tris_d.rearrange("f k m -> k f m"), [], [b_trf])
        onef = P.sb([128, 128], F32)
        b_one = Buf()
        S.do("dve", "memset", [], [b_one], onef[:], 1.0)
        LT, LE, GT, GE = 0, 1, 2, 3
        arow = P.sb([128, 128], F32)
        b_arow = Buf()
        S.dma("sp", arow[:], a_log.rearrange("a h -> (a h)").unsqueeze(0).broadcast_to([128, 128]), [], [b_arow])
        S.do("act", "activation", [b_arow], [b_arow], out=arow[:], in_=arow[:], func=AF.Exp)
        S.do("dve", "tensor_scalar", [b_arow], [b_arow], out=arow[:], in0=arow[:], scalar1=-1.0, scalar2=None, op0=ALU.mult)
        drow = P.sb([128, 64], F32)
        b_drow = Buf()
        S.dma("sp", drow[:], d_skip.unsqueeze(0).broadcast_to([128, 64]), [], [b_drow])
        ng32 = P.sb([32, 128], F32)
        ngT = P.sb([128, 32], F32)
        b_ng = Buf()
        S.dma("sp", ng32[:], ng.rearrange("(c p) -> c p", p=128), [], [b_ng])
        xsr = Ring([P.sb([128, 4096], BF16) for _ in range(2)])
        btr = Ring([P.sb([128, 1024], BF16) for _ in range(2)])
        bcr = Ring([P.sb([128, 16, 128], BF16) for _ in range(2)])
        dtr = Ring([P.sb([128, 128], F32) for _ in range(2)])
        dar = Ring([P.sb([128, 64], F32) for _ in range(2)])
        ecr = Ring([P.sb([128, 3, 64], F32) for _ in range(2)])
        xg = P.sb([128, 4096], BF16)
        xgd = P.sb([128, 4096], BF16)
        b_xg, b_xgd = Buf(), Buf()
        Sf = P.sb([128, 8, 512], F32)
        Sb = P.sb([128, 8, 512], BF16)
        b_Sf = [Buf() for _ in range(8)]
        b_Sb = [Buf() for _ in range(8)]
        yar = Ring([P.sb([128, 4096], F32) for _ in range(2)])
        dtri = Ring([P.sb([128, 8, 128], F32) for _ in range(2)])
        Er = Ring([P.sb([128, 8, 128], F32) for _ in range(2)])
        Mr = Ring([P.sb([128, 8, 128], BF16) for _ in range(2)])
        cbr_ = Ring([P.sb([128, 128], F32) for _ in range(2)])
        tmr = Ring([P.sb([128, 512], F32) for _ in range(2)])
        zsr = Ring([P.sb([128, 4096], BF16) for _ in range(1)])
        ybt = P.sb([128, 4096], F32)
        b_ybt = Buf()
        yh = P.sb([128, 4096], BF16)
        b_yh = Buf()
        ytr = Ring([P.sb([128, 32, 128], BF16) for _ in range(2)])
        junk = P.sb([128, 4096], BF16)
        bj = Buf()
        ssq = P.sb([128, NLAT], F32)
        rstd = P.sb([128, NLAT], F32)
        p_c = P.ps([128, 3, 64], F32)
        p_cb = P.ps([128, 128], F32)
        p_seg = [P.ps([128, 4, 128], F32) for _ in range(2)]
        p_y = P.ps([128, 512], F32)
        p_y2 = P.ps([128, 512], F32)
        p_st = P.ps([128, 512], F32)
        p_t = P.ps([128, 8, 128], BF16)
        b_pc, b_pcb, b_pseg, b_py, b_py2, b_pst, b_pt = Buf(), Buf(), [Buf(), Buf()], Buf(), Buf(), Buf(), Buf()
        S.do("pe", "transpose", [b_ng], [b_py], p_y[:, 0:32], ng32[:], ident[0:32, 0:32])
        S.do("dve", "tensor_copy", [b_py], [b_ng], out=ngT[:], in_=p_y[:, 0:32])
        for d in (1, 0):
            Lm, Rt, CBm, cI, cA = (GT, LE, LE, LE, GT) if d == 0 else (LT, GE, GE, GE, LT)
            for g in range(8):
                S.do("dve", "memset", [], [b_Sf[g]], Sf[:, g, :], 0.0)
                S.do("pool", "memset", [], [b_Sb[g]], Sb[:, g, :], 0.0)
            order = [16, 17] + list(range(NLAT)) if d == 0 else [17, 16] + list(range(NLAT - 1, -1, -1))
            for tc in order:
                lat = tc < NLAT
                xs, bxs = xsr.next()
                S.dma("sp", xs[:], XS[tc * 128:(tc + 1) * 128, :], [], [bxs])
                bt, bbt = btr.next()
                S.dma("sp", bt[:], BTOK[tc * 128:(tc + 1) * 128, :], [], [bbt])
                dt_, bdt = dtr.next()
                S.dma("sp", dt_[:], DTS[tc * 128:(tc + 1) * 128, :], [], [bdt])
                if lat:
                    bc, bbc = bcr.next()
                    S.dma("sp", bc[:], BCT[:, tc * 128:(tc + 1) * 128].rearrange("(j p) t -> p j t", p=128), [], [bbc])
                da, bda = dar.next()
                S.do("dve", "tensor_tensor", [bdt, b_arow], [bda], out=da[:], in0=dt_[:, d * 64:(d + 1) * 64], in1=arow[:, d * 64:(d + 1) * 64],
                     op=ALU.mult)
                S.do("pe", "matmul", [bda, b_trf], [b_pc], p_c[:, 0, :], trf[:, cI, :], da[:], start=True, stop=True)
                S.do("pe", "matmul", [bda, b_trf], [b_pc], p_c[:, 1, :], trf[:, cA, :], da[:], start=True, stop=True)
                S.do("pe", "matmul", [bda, b_one], [b_pc], p_c[:, 2, :], onef[:], da[:], start=True, stop=True)
                ec, bec = ecr.next()
                S.do("act", "activation", [b_pc], [bec], out=ec[:], in_=p_c[:], func=AF.Exp)
                dtv = dt_[:, d * 64:(d + 1) * 64]
                S.do("dve", "tensor_tensor", [bxs, bdt], [b_xg], out=xg[:].rearrange("p (h q) -> p h q", h=64),
                     in0=xs[:].rearrange("p (h q) -> p h q", h=64), in1=dtv.unsqueeze(2).to_broadcast([128, 64, 64]), op=ALU.mult)
                S.do("pool", "tensor_tensor", [b_xg, bec], [b_xgd], out=xgd[:].rearrange("p (h q) -> p h q", h=64),
                     in0=xg[:].rearrange("p (h q) -> p h q", h=64), in1=ec[:, 1, :].unsqueeze(2).to_broadcast([128, 64, 64]), op=ALU.mult)
                if lat:
                    ya, bya = yar.next()
                for g in range(8):
                    if lat:
                        S.do("pe", "matmul", [bbc], [b_pcb], p_cb[:], bc[:, g, :], bc[:, 8 + g, :], start=True, stop=True)
                        cb, bcb = cbr_.next()
                        S.do("dve", "tensor_tensor", [b_pcb, b_trf], [bcb], out=cb[:], in0=p_cb[:], in1=trf[:, CBm, :], op=ALU.mult)
                        dtt, bdtt = dtri.next()
                        S.do("pool", "tensor_tensor", [b_trf, bda], [bdtt], out=dtt[:], in0=trf[:, Rt:Rt + 1, :].to_broadcast([128, 8, 128]),
                             in1=da[:, g * 8:(g + 1) * 8].unsqueeze(2).to_broadcast([128, 8, 128]), op=ALU.mult)
                        for hh in range(2):
                            S.do("pe", "matmul", [bdtt, b_trf], [b_pseg[hh]], p_seg[hh][:].rearrange("p a b -> p (a b)"), trf[:, Lm, :],
                                 dtt[:, hh * 4:(hh + 1) * 4, :].rearrange("p a b -> p (a b)"), start=True, stop=True)
                        E, bE = Er.next()
                        for hh in range(2):
                            S.do("act", "activation", [b_pseg[hh]], [bE], out=E[:, hh * 4:(hh + 1) * 4, :], in_=p_seg[hh][:], func=AF.Exp)
                        M, bM = Mr.next()
                        S.do("dve", "tensor_tensor", [bE, bcb], [bM], out=M[:], in0=E[:], in1=cb[:].unsqueeze(1).to_broadcast([128, 8, 128]),
                             op=ALU.mult)
                        for h in range(8):
                            hd = g * 8 + h
                            S.do("pe", "matmul", [bM, b_xg], [b_py], p_y[:, h * 64:(h + 1) * 64], M[:, h, :], xg[:, hd * 64:(hd + 1) * 64],
                                 start=True, stop=True)
                        S.do("pe", "matmul", [bbc, b_Sb[g]], [b_py2], p_y2[:], bc[:, 8 + g, :], Sb[:, g, :], start=True, stop=True)
                        tm, btm = tmr.next()
                        S.do("dve", "tensor_tensor", [b_py2, bec], [btm], out=tm[:].rearrange("p (h q) -> p h q", h=8),
                             in0=p_y2[:].rearrange("p (h q) -> p h q", h=8),
                             in1=ec[:, 0, g * 8:(g + 1) * 8].unsqueeze(2).to_broadcast([128, 8, 64]), op=ALU.mult)
                        S.do("dve", "tensor_tensor", [btm, b_py], [bya], out=ya[:, g * 512:(g + 1) * 512], in0=tm[:], in1=p_y[:], op=ALU.add)
                    S.do("pe", "matmul", [bbt, b_xgd], [b_pst], p_st[:], bt[:, g * 128:(g + 1) * 128], xgd[:, g * 512:(g + 1) * 512],
                         start=True, stop=True)
                    S.do("pool", "tensor_tensor", [b_Sf[g], bec], [b_Sf[g]], out=Sf[:, g, :].rearrange("p (h q) -> p h q", h=8),
                         in0=Sf[:, g, :].rearrange("p (h q) -> p h q", h=8),
                         in1=ec[:, 2, g * 8:(g + 1) * 8].unsqueeze(2).to_broadcast([128, 8, 64]), op=ALU.mult)
                    S.do("dve", "tensor_tensor", [b_Sf[g], b_pst], [b_Sf[g]], out=Sf[:, g, :], in0=Sf[:, g, :], in1=p_st[:], op=ALU.add)
                    S.do("act", "copy", [b_Sf[g]], [b_Sb[g]], out=Sb[:, g, :], in_=Sf[:, g, :])
                if not lat:
                    continue
                if d == 1:
                    S.dma("sp", YB[tc * 128:(tc + 1) * 128, :], ya[:], [bya], [S.B("YB", tc)])
                    continue
                S.dma("sp", ybt[:], YB[tc * 128:(tc + 1) * 128, :], [S.B("YB", tc)], [b_ybt])
                S.do("pool", "tensor_tensor", [bya, b_ybt], [bya], out=ya[:], in0=ya[:], in1=ybt[:], op=ALU.add)
                S.do("dve", "tensor_tensor", [bxs, b_drow], [b_ybt], out=ybt[:].rearrange("p (h q) -> p h q", h=64),
                     in0=xs[:].rearrange("p (h q) -> p h q", h=64), in1=drow[:].unsqueeze(2).to_broadcast([128, 64, 64]), op=ALU.mult)
                S.do("pool", "tensor_tensor", [bya, b_ybt], [bya], out=ya[:], in0=ya[:], in1=ybt[:], op=ALU.add)
                zs, bzs = zsr.next()
                S.dma("sp", zs[:], ZS[tc * 128:(tc + 1) * 128, :], [], [bzs])
                S.do("dve", "tensor_tensor", [bya, bzs], [bya], out=ya[:], in0=ya[:], in1=zs[:], op=ALU.mult)
                bs = Buf()
                rms_rstd(S, P, ya[:], bya, junk[:], bj, ssq, rstd, bs, tc, 4096)
                S.do("dve", "tensor_scalar", [bya, bs], [b_yh], out=yh[:], in0=ya[:], scalar1=rstd[:, tc:tc + 1], scalar2=None, op0=ALU.mult)
                yt, byt = ytr.next()
                for q in range(4):
                    for j in range(8):
                        c = q * 8 + j
                        S.do("pe", "transpose", [b_yh], [b_pt], p_t[:, j, :], yh[:, c * 128:(c + 1) * 128], identb[:])
                    S.do("dve", "tensor_tensor", [b_pt, b_ng], [byt], out=yt[:, q * 8:(q + 1) * 8, :], in0=p_t[:],
                         in1=ngT[:, q * 8:(q + 1) * 8].unsqueeze(2).to_broadcast([128, 8, 128]), op=ALU.mult)
                S.dma("sp", YT[:, tc * 128:(tc + 1) * 128].rearrange("(c p) t -> p c t", p=128), yt[:], [byt], [Buf()])
    if K.stop_after == "s3":
        return
    phase_outproj(K, layer, YT, 32, w_out, H, modD, NLAT)


def phase_final(K, H, final_g, out):
    S = K.S
    with K.phase("final") as P:
        gt = P.sb([128, D], F32)
        bg = Buf()
        S.dma("sp", gt[:], final_g.unsqueeze(0).broadcast_to([128, D]), [], [bg])
        hr = Ring([P.sb([128, D], F32) for _ in range(3)])
        junk = P.sb([128, D], BF16)
        bj = Buf()
        ssq = P.sb([128, NLAT], F32)
        rstd = P.sb([128, NLAT], F32)
        for tc in range(NLAT):
            h, bh = hr.next()
            S.dma("sp", h[:], H[tc * 128:(tc + 1) * 128, :], [], [bh])
            bs = Buf()
            rms_rstd(S, P, h[:], bh, junk[:], bj, ssq, rstd, bs, tc, D)
            S.do("dve", "scalar_tensor_tensor", [bh, bs, bg], [bh], out=h[:], in0=h[:], scalar=rstd[:, tc:tc + 1], in1=gt[:],
                 op0=ALU.mult, op1=ALU.mult)
            S.dma("sp", out[tc * 128:(tc + 1) * 128, :], h[:], [bh], [Buf()])
    K.out_written = True


def phase_moe_post(K, layer, has_ctx, Hin, H, modD, SELT, SELTC, YEo, jb):
    S = K.S
    ntc = NTC if has_ctx else NLAT
    with K.phase(f"e3{layer}") as P:
        nr = 2 if has_ctx else 1
        G, bG = load_gate_rows(K, P, modD, layer, 2, nr)
        yr = Ring([P.sb([128, 2 * NE, 512], BF16) for _ in range(2)])
        ycr = Ring([P.sb([CAPC, NE, 512], BF16) for _ in range(2)])
        sr = Ring([P.sb([128, 2 * NE, 128], BF16) for _ in range(3)])
        scr_ = Ring([P.sb([CAPC, NE, 128], BF16) for _ in range(2)])
        pm = Ring([P.ps([128, 512], F32) for _ in range(3)])
        hr = Ring([P.sb([128, 512], F32) for _ in range(3)])
        tr = Ring([P.sb([128, 512], F32) for _ in range(3)])
        for n in range(4):
            y, by = yr.next()
            S.dma("sp", y[:].rearrange("p (e s) c -> p e s c", s=2), YEo[:, jb, n, :, 0:2, :].rearrange("e p s c -> p e s c"), [], [by])
            if has_ctx:
                yc, byc = ycr.next()
                S.dma("sp", yc[:], YEo[:, jb, n, 0:CAPC, 2, :].rearrange("e p c -> p e c"), [], [byc])
            for tc in range(ntc):
                r = 0 if tc < NLAT else 1
                ps, bp = pm.next()
                if tc < NLAT:
                    st, bst = sr.next()
                    S.dma("sp", st[:], SELT[tc], [], [bst])
                    for j in range(2 * NE):
                        S.do("pe", "matmul", [bst, by], [bp], ps[:], st[:, j, :], y[:, j, :], start=(j == 0), stop=(j == 2 * NE - 1))
                else:
                    st, bst = scr_.next()
                    S.dma("sp", st[:], SELTC[tc - NLAT], [], [bst])
                    for j in range(NE):
                        S.do("pe", "matmul", [bst, byc], [bp], ps[:], st[:, j, :], yc[:, j, :], start=(j == 0), stop=(j == NE - 1))
                ht, bh = hr.next()
                hb = S.B("H3", tc, n)
                S.dma("sp", ht[:], Hin[tc * 128:(tc + 1) * 128, n * 512:(n + 1) * 512], [hb], [bh])
                tt, bt = tr.next()
                S.do("dve", "tensor_tensor", [bp, bG[r]], [bt], out=tt[:], in0=ps[:], in1=G[r][:, n * 512:(n + 1) * 512], op=ALU.mult)
                S.do("pool", "tensor_tensor", [bt, bh], [bh], out=ht[:], in0=tt[:], in1=ht[:], op=ALU.add)
                S.dma("sp", H[tc * 128:(tc + 1) * 128, n * 512:(n + 1) * 512], ht[:], [bh], [hb])


def phase_experts(K, XEget, wg, wu, wd, YEo, nslot, has_ctx, n_exp=2, NB=8):
    S = K.S
    with K.phase("e2") as P:
        xe = P.sb([128, NB, 16, nslot], BF16)
        hid = P.sb([128, NB, 16, nslot], BF16)
        b_xe = [Buf() for _ in range(NB)]
        b_hid = [Buf() for _ in range(NB)]
        wr = Ring([P.sb([128, 16, 512], BF16) for _ in range(3)])
        sgr = Ring([P.sb([128, nslot], F32) for _ in range(3)])
        yer = Ring([P.sb([128, 3, 512], BF16) for _ in range(2)])
        pgu = Ring([P.ps([128, 512], F32) for _ in range(4)])
        pdn = Ring([P.ps([128, 512], F32) for _ in range(3)])
        chunks = [(0, 128), (128, 128)] + ([(256, CAPC)] if has_ctx else [])
        for el in range(n_exp):
            for b in range(NB):
                S.dma("sp", xe[:, b, :, :], XEget(el, b).rearrange("(c p) s -> p c s", p=128)[:, :, 0:nslot], [], [b_xe[b]])
            for fp in range(4):
                wgt, bwg = wr.next()
                S.dma("pool", wgt[:], wpiece(wg[el], fp * 512, 512), [], [bwg])
                wut, bwu = wr.next()
                S.dma("pool", wut[:], wpiece(wu[el], fp * 512, 512), [], [bwu])
                for fo in range(4):
                    fc = fp * 4 + fo
                    for b in range(NB):
                        psg, bpg = pgu.next()
                        for c in range(16):
                            S.do("pe", "matmul", [bwg, b_xe[b]], [bpg], psg[:, 0:nslot], wgt[:, c, fo * 128:(fo + 1) * 128], xe[:, b, c, :],
                                 start=(c == 0), stop=(c == 15))
                        psu, bpu = pgu.next()
                        for c in range(16):
                            S.do("pe", "matmul", [bwu, b_xe[b]], [bpu], psu[:, 0:nslot], wut[:, c, fo * 128:(fo + 1) * 128], xe[:, b, c, :],
                                 start=(c == 0), stop=(c == 15))
                        sgt, bsg = sgr.next()
                        S.do("act", "activation", [bpg], [bsg], out=sgt[:], in_=psg[:, 0:nslot], func=AF.Silu)
                        S.do("dve", "tensor_tensor", [bsg, bpu], [b_hid[b]], out=hid[:, b, fc, :], in0=sgt[:], in1=psu[:, 0:nslot], op=ALU.mult)
            for n in range(4):
                wdt, bwd = wr.next()
                S.dma("pool", wdt[:], wpiece(wd[el], n * 512, 512), [], [bwd])
                for b in range(NB):
                    ye, bye = yer.next()
                    for si, (s0, sn) in enumerate(chunks):
                        ps, bp = pdn.next()
                        for fc in range(16):
                            S.do("pe", "matmul", [bwd, b_hid[b]], [bp], ps[0:sn, :], hid[:, b, fc, s0:s0 + sn], wdt[:, fc, :],
                                 start=(fc == 0), stop=(fc == 15))
                        S.do("act" if si % 2 else "dve", "copy" if si % 2 else "tensor_copy", [bp], [bye], out=ye[0:sn, si, :], in_=ps[0:sn, :])
                    S.dma("sp", YEo[el, b, n, :, 0:2, :], ye[:, 0:2, :], [bye], [Buf()])
                    if has_ctx:
                        S.dma("sp", YEo[el, b, n, 0:CAPC, 2, :], ye[0:CAPC, 2, :], [bye], [Buf()])


MODC = 6 * D // 8


def phase_mod(K, cc9, mod_w, mod_b, modp, ident, nrows=9, ncols=MODC):
    S = K.S
    with K.phase("mod") as P:
        cc = P.sb([nrows, D], F32)
        scT = P.sb([128, 16, nrows], BF16)
        ps_t = P.ps([128, 16, nrows], F32)
        b_cc, b_ps, b_sc = Buf(), Buf(), Buf()
        S.dma("sp", cc[:], cc9[:, :], [], [b_cc])
        S.do("act", "activation", [b_cc], [b_cc], out=cc[:], in_=cc[:], func=AF.Silu)
        for c in range(16):
            S.do("pe", "transpose", [b_cc], [b_ps], ps_t[:, c, :], cc[:, c * 128:(c + 1) * 128], ident[0:nrows, 0:nrows])
        S.do("dve", "tensor_copy", [b_ps], [b_sc], out=scT[:], in_=ps_t[:])
        wr = Ring([P.sb([128, 16, 512], BF16) for _ in range(3)])
        pr = Ring([P.ps([nrows, 512], F32) for _ in range(2)])
        br = Ring([P.sb([nrows, 512], F32) for _ in range(2)])
        orr = Ring([P.sb([nrows, 512], F32) for _ in range(2)])
        for layer in range(2):
            for n in range(ncols // 512):
                w, bw = wr.next()
                S.dma("pool", w[:], wpiece(mod_w[layer], n * 512, 512), [], [bw])
                bt, bb = br.next()
                S.dma("sp", bt[:], mod_b[layer:layer + 1, n * 512:(n + 1) * 512].broadcast_to([nrows, 512]), [], [bb])
                ps, bp = pr.next()
                for c in range(16):
                    S.do("pe", "matmul", [bw, b_sc], [bp], ps[:], scT[:, c, :], w[:, c, :], start=(c == 0), stop=(c == 15))
                ot, bo = orr.next()
                S.do("dve", "tensor_tensor", [bp, bb], [bo], out=ot[:], in0=ps[:], in1=bt[:], op=ALU.add)
                S.dma("sp", modp[layer, :, n * 512:(n + 1) * 512], ot[:], [bo], [Buf()])


NB = 1
NCORES = 8 // NB


def build(dbg=(), stop_after=None):
    K = KB(dbg, stop_after)
    nc = K.nc
    innames = []

    def inp(name, shape, dt=F32):
        innames.append(name)
        return K.inp(name, shape, dt)

    ident_d = inp("ident", [128, 128])
    x = inp("x", [NB, T, D])
    ctx = inp("ctx", [NB, TCX, D])
    cc = inp("cc", [NB + 1, D])
    mod_w = inp("mod_w", [2, D, 6 * D])
    mod_b = inp("mod_b", [2, 6 * D])
    norm_g = inp("norm_g", [2, 2, D])
    final_g = inp("final_g", [D])
    ab_w_in = inp("ab_w_in", [1, D, 3072])
    ab_w_out = inp("ab_w_out", [1, D, D])
    gm_v_g = inp("gm_v_g", [1, 1024])
    gm_w_s = inp("gm_w_s", [1, 8, 128, 128])
    gm_b_s = inp("gm_b_s", [1, 8, 128])
    w_in = inp("ssd_w_in", [1, D, 10368])
    conv_w = inp("ssd_conv_w", [1, 5, 6144])
    conv_b = inp("ssd_conv_b", [1, 6144])
    dt_bias = inp("ssd_dt_bias", [1, 2, 64])
    a_log = inp("ssd_a_log", [1, 2, 64])
    d_skip = inp("ssd_d", [1, 64])
    ng = inp("ssd_norm_g", [1, 4096])
    w_out = inp("ssd_w_out", [1, 4096, D])
    w_router = inp("moe_w_router", [2, D, NE])
    w_gate = inp("moe_w_gate", [2, NE, D, D])
    w_up = inp("moe_w_up", [2, NE, D, D])
    w_down = inp("moe_w_down", [2, NE, D, D])
    pos = inp("pos", [T, D])
    cs128_d = inp("cs128", [128, 256], BF16)
    dftT_d = inp("dftT", [2, T, T], BF16)
    dftC_d = inp("dftC", [2, TCX, TCX], BF16)
    tris_d = inp("tris", [4, 128, 128])
    iota_d = inp("iota", [128, NSLOT])
    out = nc.dram_tensor("out", [NB, T, D], F32, kind="ExternalOutput").ap()
    modD = K.scratch("modD", [2, NB + 1, 6 * D])
    es = K.es
    with es:
        sems = {e: es.enter_context(nc.semaphore("s_" + e)) for e in ENGS}
        rings = {e: [es.enter_context(nc.semaphore(f"r_{e}{i}")) for i in range(NRING)] for e in ENGS}
        S = K.S = Sched(nc, sems, rings)
        ident = es.enter_context(nc.sbuf_tensor("ident_sb", [128, 128], F32))
        identb = es.enter_context(nc.sbuf_tensor("identb_sb", [128, 128], BF16))
        with K.phase("c0") as P:
            bi = Buf()
            S.dma("sp", ident[:], ident_d[:, :], [], [bi])
            S.do("dve", "tensor_copy", [bi], [Buf()], out=identb[:], in_=ident[:])
        phase_mod(K, cc, mod_w, mod_b, modD, ident, nrows=NB + 1, ncols=6 * D)
        Hs, XE0, XE1, SELT0, SELT1, SELTC0 = [], [], [], [], [], []
        for j in range(NB):
            K.sfx = f"_b{j}"
            K.rows = (j, NB)
            Hs.append(K.scratch("H", [NTOK, D]))
            XE0.append(K.scratch("XE0", [NE, D, NSLOT], BF16))
            SELT0.append(K.scratch("SELT0", [NLAT, 128, 2 * NE, 128], BF16))
            SELTC0.append(K.scratch("SELTC0", [2, CAPC, NE, 128], BF16))
            XE1.append(K.scratch("XE1", [NE, D, CAP], BF16))
            SELT1.append(K.scratch("SELT1", [NLAT, 128, 2 * NE, 128], BF16))
            catT = K.scratch("catT", [D, NTOK], BF16)
            P12 = K.scratch("P12", [8, NTOK, 256], BF16)
            phase_ab(K, 0, x[j], ctx[j], pos, Hs[j], modD, norm_g, ab_w_in[0], ab_w_out[0], gm_v_g[0], gm_w_s[0], gm_b_s[0],
                     cs128_d, dftT_d, dftC_d, catT, P12, ident, identb)
            phase_moe_pre(K, 0, True, Hs[j], modD, norm_g, w_router[0], tris_d, iota_d, ident, identb, XE0[j], SELT0[j], SELTC0[j])
        K.sfx = ""
        YE0 = K.scratch("YE0", [NE, NB, 4, 128, 3, 512], BF16)
        phase_experts(K, lambda e, j: XE0[j][e], w_gate[0], w_up[0], w_down[0], YE0, NSLOT, True, n_exp=NE, NB=NB)
        for j in range(NB):
            K.sfx = f"_b{j}"
            K.rows = (j, NB)
            phase_moe_post(K, 0, True, Hs[j], Hs[j], modD, SELT0[j], SELTC0[j], YE0, j)
            scr = (K.scratch("ZS", [T, 4096], BF16), K.scratch("BCT", [2048, NTOK], BF16), K.scratch("XS", [NTOK, 4096], BF16),
                   K.scratch("BTOK", [NTOK, 1024], BF16), K.scratch("DTS", [NTOK, 128]), K.scratch("YB", [T, 4096]),
                   K.scratch("YT", [4096, T], BF16))
            phase_ssd(K, 1, Hs[j], modD, norm_g, w_in[0], conv_w[0], conv_b[0], dt_bias[0], a_log[0], d_skip[0], ng[0], w_out[0],
                      tris_d, ident, identb, scr)
            phase_moe_pre(K, 1, False, Hs[j], modD, norm_g, w_router[1], tris_d, iota_d, ident, identb, XE1[j], SELT1[j], None)
        K.sfx = ""
        YE1 = K.scratch("YE1", [NE, NB, 4, 128, 2, 512], BF16)
        phase_experts(K, lambda e, j: XE1[j][e], w_gate[1], w_up[1], w_down[1], YE1, CAP, False, n_exp=NE, NB=NB)
        for j in range(NB):
            K.rows = (j, NB)
            phase_moe_post(K, 1, False, Hs[j], Hs[j], modD, SELT1[j], None, YE1, j)
            phase_final(K, Hs[j], final_g, out[j])
    nc._inames = innames
    return nc


_CONST = {}


def host_constants():
    if _CONST:
        return _CONST
    bf = ml_dtypes.bfloat16
    rows, cols, dim = T // 64, 64, D
    quarter = dim // 4
    omega = (1.0 / (np.float32(10000.0) ** (np.arange(quarter, dtype=np.float32) / np.float32(quarter)))).astype(np.float32)
    r = np.repeat(np.arange(rows, dtype=np.float32), cols)[:, None] * omega
    cl = np.tile(np.arange(cols, dtype=np.float32), rows)[:, None] * omega
    _CONST["pos"] = np.concatenate([np.sin(r), np.cos(r), np.sin(cl), np.cos(cl)], axis=-1).astype(np.float32)
    _CONST["ident"] = np.eye(128, dtype=np.float32)

    def dft(n):
        k = np.arange(n, dtype=np.int64)
        ang = 2.0 * np.pi * ((k[:, None] * k[None, :]) % n).astype(np.float64) / n
        return np.cos(ang), np.sin(ang)

    c128, s128 = dft(128)
    _CONST["cs128"] = np.concatenate([c128, s128], axis=1).astype(np.float32).astype(bf)
    cT, sT = dft(T)
    _CONST["dftT"] = np.stack([cT, -sT]).astype(np.float32).astype(bf)
    cC, sC = dft(TCX)
    _CONST["dftC"] = np.stack([cC, -sC]).astype(np.float32).astype(bf)
    k = np.arange(128)
    kk, mm = k[:, None], k[None, :]
    _CONST["tris"] = np.stack([kk < mm, kk <= mm, kk > mm, kk >= mm]).astype(np.float32)
    _CONST["iota"] = np.tile(np.arange(NSLOT, dtype=np.float32)[None, :], (128, 1))
    return _CONST


_NC_CACHE = {}
_SHARED = ("mod_w", "mod_b", "norm_g", "final_g", "ab_w_in", "ab_w_out", "gm_v_g", "gm_w_s", "gm_b_s", "ssd_w_in", "ssd_conv_w",
           "ssd_conv_b", "ssd_dt_bias", "ssd_a_log", "ssd_d", "ssd_norm_g", "ssd_w_out", "moe_w_router", "moe_w_gate", "moe_w_up",
           "moe_w_down")


def make_in_maps(inputs, cores):
    cst = host_constants()
    shared = {k: np.ascontiguousarray(inputs[k]) for k in _SHARED}
    maps = []
    for k in cores:
        m = dict(shared)
        m.update(cst)
        m["x"] = np.ascontiguousarray(inputs["x"][k * NB:(k + 1) * NB])
        m["ctx"] = np.ascontiguousarray(inputs["ctx"][k * NB:(k + 1) * NB])
        m["cc"] = np.ascontiguousarray(np.concatenate([inputs["c"][k * NB:(k + 1) * NB], inputs["c_ctx"][None, :]], axis=0))
        maps.append(m)
    return maps


def kernel(**inputs):
    inputs = {k: np.asarray(v) for k, v in inputs.items()}
    if "nc" not in _NC_CACHE:
        _NC_CACHE["nc"] = build()
    nc = _NC_CACHE["nc"]
    maps = make_in_maps(inputs, range(NCORES))
    res = run_bass_kernel_spmd(nc, maps, core_ids=list(range(NCORES)))
    return np.concatenate([np.asarray(r["out"]) for r in res.results], axis=0).astype(np.float32)
```

```python
import os
import numpy as np
import ml_dtypes
from contextlib import ExitStack
import concourse.bass as bass
import concourse.mybir as mybir
from concourse.bass_utils import run_bass_kernel_spmd

F32 = mybir.dt.float32
BF16 = mybir.dt.bfloat16
AF = mybir.ActivationFunctionType
ALU = mybir.AluOpType
AX = mybir.AxisListType

ENGS = ("pe", "act", "dve", "pool", "sp")
NRING = 8

T = 2048
TCX = 256
NTOK = T + TCX
D = 2048
NTC = NTOK // 128
NLAT = T // 128
EPS = 1e-6
NE = 16
CAP = 256
CAPC = 32
NSLOT = CAP + CAPC


class Buf:
    __slots__ = ("lw", "rs")

    def __init__(self):
        self.lw = None
        self.rs = []


class Op:
    __slots__ = ("eng", "fn", "deps", "signal", "count", "dma", "ring", "rval", "phase")


class Sched:
    def __init__(self, nc, sems, rings):
        self.nc = nc
        self.sems = sems
        self.rings = rings
        self.ops = {e: [] for e in ENGS}
        self.ndma = {e: 0 for e in ENGS}
        self.cnt = {e: 0 for e in ENGS}
        self.waited = {e: {} for e in ENGS}
        self.phase = 0
        self.lastring = {}
        self.bufs = {}

    def B(self, *key):
        b = self.bufs.get(key)
        if b is None:
            b = self.bufs[key] = Buf()
        return b

    def op(self, eng, fn, reads=(), writes=(), dma=False):
        o = Op()
        o.eng, o.fn, o.dma, o.signal, o.count, o.phase = eng, fn, dma, False, 0, self.phase
        o.ring = o.rval = 0
        o.deps = []
        seen = set()
        cand = []
        for b in reads:
            if b.lw is not None:
                cand.append(b.lw)
        for b in writes:
            if b.lw is not None:
                cand.append(b.lw)
            cand.extend(b.rs)
        for d in cand:
            if id(d) in seen or d.phase != self.phase:
                continue
            seen.add(id(d))
            if d.eng == eng and eng == "pe" and not d.dma and not dma:
                continue
            o.deps.append(d)
            d.signal = True
        for b in reads:
            b.rs.append(o)
        for b in writes:
            b.lw = o
            b.rs = []
        if dma:
            i = self.ndma[eng]
            self.ndma[eng] += 1
            o.ring = i % NRING
            o.rval = 16 * (i // NRING + 1)
            self.lastring[(eng, o.ring)] = o.rval
        self.ops[eng].append(o)
        return o

    def do(self, eng, meth, reads, writes, *a, **kw):
        return self.op(eng, lambda e: getattr(e, meth)(*a, **kw), reads, writes)

    def dma(self, eng, out, in_, reads, writes):
        return self.op(eng, lambda e: e.dma_start(out=out, in_=in_), reads, writes, dma=True)

    def _wait(self, e, eh, need):
        w = self.waited[e]
        for key, (sem, val) in need.items():
            if w.get(key, 0) >= val:
                continue
            eh.wait_ge(sem, val)
            w[key] = val

    def emit_engine(self, e, eh, final):
        w = self.waited[e]
        for o in self.ops[e]:
            need = {}
            for d in o.deps:
                if d.dma:
                    key, sem, val = ("r", d.eng, d.ring), self.rings[d.eng][d.ring], d.rval
                else:
                    key, sem, val = ("c", d.eng), self.sems[d.eng], d.count
                if key not in need or need[key][1] < val:
                    need[key] = (sem, val)
            if o.dma and o.rval > 16:
                key = ("r", e, o.ring)
                if key not in need or need[key][1] < o.rval - 16:
                    need[key] = (self.rings[e][o.ring], o.rval - 16)
            self._wait(e, eh, need)
            ins = o.fn(eh)
            if o.dma:
                ins.then_inc(self.rings[e][o.ring], 16)
            elif o.signal:
                ins.then_inc(self.sems[e], 1)
        need = {}
        for p in ENGS:
            if final[p] > 0:
                need[("c", p)] = (self.sems[p], final[p])
        for (p, r), val in self.lastring.items():
            need[("r", p, r)] = (self.rings[p][r], val)
        self._wait(e, eh, need)

    def end_phase(self, block):
        final = {}
        for e in ENGS:
            comp = [o for o in self.ops[e] if not o.dma]
            if comp:
                comp[-1].signal = True
            c = self.cnt[e]
            for o in comp:
                if o.signal:
                    c += 1
                    o.count = c
            self.cnt[e] = c
            final[e] = c
        S = self

        @block.tensor
        def _(eh):
            S.emit_engine("pe", eh, final)

        @block.scalar
        def _(eh):
            S.emit_engine("act", eh, final)

        @block.vector
        def _(eh):
            S.emit_engine("dve", eh, final)

        @block.gpsimd
        def _(eh):
            S.emit_engine("pool", eh, final)

        @block.sync
        def _(eh):
            S.emit_engine("sp", eh, final)

        self.ops = {e: [] for e in ENGS}
        self.phase += 1


class Phase:
    def __init__(self, K, name):
        self.K = K
        K.uid += 1
        self.name = f"{name}u{K.uid}"
        self.es = ExitStack()
        self.n = 0

    def __enter__(self):
        self.es.__enter__()
        return self

    def sb(self, shape, dt, name=None):
        self.n += 1
        return self.es.enter_context(self.K.nc.sbuf_tensor(f"{self.name}_{name or 't'}{self.n}", list(shape), dt))

    def ps(self, shape, dt=F32, name=None):
        self.n += 1
        return self.es.enter_context(self.K.nc.psum_tensor(f"{self.name}_{name or 'p'}{self.n}", list(shape), dt))

    def __exit__(self, *a):
        with self.K.nc.Block() as block:
            self.K.S.end_phase(block)
        return self.es.__exit__(*a)


class Ring:
    def __init__(self, tiles):
        self.tiles = tiles
        self.bufs = [Buf() for _ in tiles]
        self.i = -1

    def next(self):
        self.i = (self.i + 1) % len(self.tiles)
        return self.tiles[self.i], self.bufs[self.i]


class KB:
    def __init__(self, dbg=(), stop_after=None):
        self.dbg = set(dbg)
        self.stop_after = stop_after
        self.nc = bass.Bass("TRN2", target_bir_lowering=False)
        self.es = ExitStack()
        self.dram = {}
        self.uid = 0
        self.rows = (0, 1)
        self.sfx = ""

    def inp(self, name, shape, dt=F32):
        self.dram[name] = self.nc.dram_tensor(name, list(shape), dt, kind="ExternalInput").ap()
        return self.dram[name]

    def scratch(self, name, shape, dt=F32):
        kind = "ExternalOutput" if name in self.dbg else "Internal"
        name = name + self.sfx
        self.dram[name] = self.nc.dram_tensor(name, list(shape), dt, kind=kind).ap()
        return self.dram[name]

    def sbuf(self, es, name, shape, dt):
        self.uid += 1
        return es.enter_context(self.nc.sbuf_tensor(f"{name}u{self.uid}", list(shape), dt))

    def phase(self, name):
        return Phase(self, name)


def wpiece(w2d, n0, ncols, kc=16):
    return w2d.rearrange("(c p) n -> p c n", p=128)[:, :, n0:n0 + ncols]


def load_mod_rows(K, P, modD, layer, which, g_row_ap, nr=2):
    S = K.S
    sh_off = 0 if which == 1 else 3 * D
    sc_off = sh_off + D
    A, Bt, bA, bB = [], [], [], []
    gt = P.sb([128, D], F32)
    bg = Buf()
    S.dma("sp", gt[:], g_row_ap.broadcast_to([128, D]), [], [bg])
    for r in range(nr):
        a = P.sb([128, D], F32)
        b = P.sb([128, D], F32)
        ba, bb = Buf(), Buf()
        S.dma("sp", a[:], modD[layer, K.rows[r]:K.rows[r] + 1, sc_off:sc_off + D].broadcast_to([128, D]), [], [ba])
        S.dma("sp", b[:], modD[layer, K.rows[r]:K.rows[r] + 1, sh_off:sh_off + D].broadcast_to([128, D]), [], [bb])
        S.do("dve", "scalar_tensor_tensor", [ba, bg], [ba], out=a[:], in0=a[:], scalar=1.0, in1=gt[:], op0=ALU.add, op1=ALU.mult)
        A.append(a), Bt.append(b), bA.append(ba), bB.append(bb)
    return A, Bt, bA, bB


def load_gate_rows(K, P, modD, layer, which, nr=2):
    S = K.S
    off = 2 * D if which == 1 else 5 * D
    G, bG = [], []
    for r in range(nr):
        g = P.sb([128, D], F32)
        bg = Buf()
        S.dma("sp", g[:], modD[layer, K.rows[r]:K.rows[r] + 1, off:off + D].broadcast_to([128, D]), [], [bg])
        G.append(g), bG.append(bg)
    return G, bG


def rms_rstd(S, P, hbuf, bh, junk, bj, ssq, rstd, bs, tc, dim):
    S.do("act", "activation", [bh], [bj, bs], out=junk, in_=hbuf, func=AF.Square, accum_out=ssq[:, tc:tc + 1])
    S.do("act", "activation", [bs], [bs], out=rstd[:, tc:tc + 1], in_=ssq[:, tc:tc + 1], func=AF.Sqrt, scale=1.0 / dim, bias=EPS)
    S.do("dve", "reciprocal", [bs], [bs], out=rstd[:, tc:tc + 1], in_=rstd[:, tc:tc + 1])


def phase_ab(K, layer, x, ctx, pos, H, modD, norm_g, w_in, w_out, v_g, w_s, b_s, cs128_d, dftT_d, dftC_d, catT, P12,
             ident, identb):
    S = K.S
    nc = K.nc
    ntc = NTC
    with ExitStack() as outer:
      xnT = K.sbuf(outer, "xnT_ab", [128, 16, NTOK], BF16)
      b_xnT = [Buf() for _ in range(ntc)]
      with K.phase("ab1a") as P:
        A, Bt, bA, bB = load_mod_rows(K, P, modD, layer, 1, norm_g[layer, 0:1, :])
        ssq = P.sb([128, ntc], F32)
        rstd = P.sb([128, ntc], F32)
        hr = Ring([P.sb([128, D], F32) for _ in range(2)])
        pr_ = Ring([P.sb([128, D], F32) for _ in range(2)])
        junk = P.sb([128, D], BF16)
        bj = Buf()
        t1 = P.sb([128, D], F32)
        bt1 = Buf()
        xr = Ring([P.sb([128, D], BF16) for _ in range(2)])
        ptr = Ring([P.ps([128, 8, 128], BF16) for _ in range(2)])
        for tc in range(ntc):
            r = 0 if tc < NLAT else 1
            h, bh = hr.next()
            if tc < NLAT:
                pt, bp = pr_.next()
                S.dma("sp", h[:], x[tc * 128:(tc + 1) * 128, :], [], [bh])
                S.dma("sp", pt[:], pos[tc * 128:(tc + 1) * 128, :], [], [bp])
                S.do("pool", "tensor_tensor", [bh, bp], [bh], out=h[:], in0=h[:], in1=pt[:], op=ALU.add)
            else:
                S.dma("sp", h[:], ctx[(tc - NLAT) * 128:(tc - NLAT + 1) * 128, :], [], [bh])
            S.dma("sp", H[tc * 128:(tc + 1) * 128, :], h[:], [bh], [S.B("H", tc)])
            bs = Buf()
            rms_rstd(S, P, h[:], bh, junk[:], bj, ssq, rstd, bs, tc, D)
            S.do("dve", "scalar_tensor_tensor", [bh, bs, bA[r]], [bt1], out=t1[:], in0=h[:], scalar=rstd[:, tc:tc + 1], in1=A[r][:],
                 op0=ALU.mult, op1=ALU.mult)
            xt, bx = xr.next()
            S.do("dve", "tensor_tensor", [bt1, bB[r]], [bx], out=xt[:], in0=t1[:], in1=Bt[r][:], op=ALU.add)
            for half in range(2):
                ps, bp = ptr.next()
                for j in range(8):
                    c = half * 8 + j
                    S.do("pe", "transpose", [bx], [bp], ps[:, j, :], xt[:, c * 128:(c + 1) * 128], identb[:])
                S.do("act", "copy", [bp], [b_xnT[tc]], out=xnT[:, half * 8:(half + 1) * 8, tc * 128:(tc + 1) * 128], in_=ps[:])
        if "xnT" in K.dbg:
            dd = K.scratch("xnT", [D, NTOK], BF16)
            S.dma("sp", dd.rearrange("(c p) t -> p c t", p=128), xnT[:], b_xnT, [Buf()])
      if K.stop_after == "xn":
        return
      with K.phase("ab1b") as P:
        junk = P.sb([128, 512], BF16)
        bj = Buf()
        wr = Ring([P.sb([128, 16, 512], BF16) for _ in range(2)])
        pmm = Ring([P.ps([128, 512], F32) for _ in range(2)])
        vtok = P.sb([128, ntc, 1024], BF16)
        b_v = [Buf() for _ in range(ntc)]
        vss = P.sb([128, ntc, 2], F32)
        vr = P.sb([128, ntc], F32)
        bvs = Buf()
        for pc in range(2):
            w, bw = wr.next()
            S.dma("pool", w[:], wpiece(w_in, 1024 + pc * 512, 512), [], [bw])
            for tc in range(ntc):
                ps, bp = pmm.next()
                for c in range(16):
                    S.do("pe", "matmul", [bw, b_xnT[tc]], [bp], ps[:], xnT[:, c, tc * 128:(tc + 1) * 128], w[:, c, :],
                         start=(c == 0), stop=(c == 15))
                S.do("act", "activation", [bp], [b_v[tc]], out=vtok[:, tc, pc * 512:(pc + 1) * 512], in_=ps[:], func=AF.Gelu)
                S.do("act", "activation", [b_v[tc]], [bj, bvs], out=junk[:, 0:512], in_=vtok[:, tc, pc * 512:(pc + 1) * 512],
                     func=AF.Square, accum_out=vss[:, tc, pc:pc + 1])
        S.do("dve", "tensor_tensor", [bvs], [bvs], out=vr[:], in0=vss[:, :, 0], in1=vss[:, :, 1], op=ALU.add)
        S.do("act", "activation", [bvs], [bvs], out=vr[:], in_=vr[:], func=AF.Sqrt, scale=1.0 / 1024, bias=EPS)
        S.do("dve", "reciprocal", [bvs], [bvs], out=vr[:], in_=vr[:])
        for tc in range(ntc):
            S.do("dve", "tensor_scalar", [bvs, b_v[tc]], [b_v[tc]], out=vtok[:, tc, :], in0=vtok[:, tc, :], scalar1=vr[:, tc:tc + 1],
                 scalar2=None, op0=ALU.mult)
        wsf = P.sb([128, 8, 128], F32)
        wsT = P.sb([128, 8, 128], BF16)
        vg8 = P.sb([8, 128], F32)
        vgT = P.sb([128, 8], F32)
        bsb = P.sb([128, 8, 128], F32)
        b_ws, b_wsT, b_vg, b_vgT, b_bsb = Buf(), Buf(), Buf(), Buf(), Buf()
        S.dma("sp", wsf[:], w_s.rearrange("g i j -> i g j"), [], [b_ws])
        S.dma("sp", vg8[:], v_g.rearrange("(g p) -> g p", p=128), [], [b_vg])
        S.dma("sp", bsb[:].rearrange("p g i -> p (g i)"), b_s.rearrange("g i -> (g i)").unsqueeze(0).broadcast_to([128, 1024]), [], [b_bsb])
        pss = Ring([P.ps([128, 512], F32) for _ in range(2)])
        pst = pss.tiles[0][:].rearrange("p (j i) -> p j i", j=4)
        b_pst = pss.bufs[0]
        for hf in range(2):
            for j in range(4):
                g = hf * 4 + j
                S.do("pe", "transpose", [b_ws], [b_pst], pst[:, j, :], wsf[:, g, :], ident[:])
            S.do("dve", "tensor_copy", [b_pst], [b_wsT], out=wsT[:, hf * 4:(hf + 1) * 4, :], in_=pst[:])
        S.do("pe", "transpose", [b_vg], [b_pst], pst[:, 0, 0:8], vg8[:], ident[0:8, 0:8])
        S.do("dve", "tensor_copy", [b_pst], [b_vgT], out=vgT[:], in_=pst[:, 0, 0:8])
        ur = Ring([P.sb([128, 512], F32) for _ in range(2)])
        sr = Ring([P.sb([128, 512], F32) for _ in range(2)])
        yr = Ring([P.sb([128, 512], BF16) for _ in range(2)])
        ttiles = [(i * 512, 512) for i in range(4)] + [(T, TCX)]
        for pc in range(2):
            w, bw = wr.next()
            S.dma("pool", w[:], wpiece(w_in, pc * 512, 512), [], [bw])
            for oc in range(4):
                g = pc * 4 + oc
                for (t0, tn) in ttiles:
                    tcs = list(range(t0 // 128, (t0 + tn) // 128))
                    ps, bp = pmm.next()
                    for c in range(16):
                        S.do("pe", "matmul", [bw] + [b_xnT[q] for q in tcs], [bp], ps[:, 0:tn], w[:, c, oc * 128:(oc + 1) * 128],
                             xnT[:, c, t0:t0 + tn], start=(c == 0), stop=(c == 15))
                    ut, bu = ur.next()
                    S.do("act", "activation", [bp], [bu], out=ut[:, 0:tn], in_=ps[:, 0:tn], func=AF.Gelu)
                    ps2, bp2 = pss.next()
                    for k, q in enumerate(tcs):
                        S.do("pe", "matmul", [b_v[q], b_wsT], [bp2], ps2[:, k * 128:(k + 1) * 128], vtok[:, q, g * 128:(g + 1) * 128],
                             wsT[:, g, :], start=True, stop=True)
                    st, bs_ = sr.next()
                    nk = len(tcs)
                    S.do("dve", "scalar_tensor_tensor", [bp2, b_vgT, b_bsb], [bs_], out=st[:, 0:tn].rearrange("p (k i) -> p k i", k=nk),
                         in0=ps2[:, 0:tn].rearrange("p (k i) -> p k i", k=nk), scalar=vgT[:, g:g + 1],
                         in1=bsb[:, g:g + 1, :].broadcast_to([128, nk, 128]), op0=ALU.mult, op1=ALU.add)
                    yt, by = yr.next()
                    S.do("dve", "tensor_tensor", [bs_, bu], [by], out=yt[:, 0:tn], in0=st[:, 0:tn], in1=ut[:, 0:tn], op=ALU.mult)
                    S.dma("sp", catT[g * 128:(g + 1) * 128, t0:t0 + tn], yt[:, 0:tn], [by], [S.B("catT", g, t0)])
        cs = P.sb([128, 256], BF16)
        b_cs = Buf()
        S.dma("sp", cs[:], cs128_d[:, :], [], [b_cs])
        zr = Ring([P.sb([128, 512], BF16) for _ in range(2)])
        p12r = Ring([P.sb([128, 4, 256], BF16) for _ in range(2)])
        pz = Ring([P.ps([128, 2, 256], F32) for _ in range(2)])
        for pc in range(2):
            w, bw = wr.next()
            S.dma("pool", w[:], wpiece(w_in, 2048 + pc * 512, 512), [], [bw])
            for oc in range(4):
                g = pc * 4 + oc
                for (t0, tn) in ttiles:
                    tcs = list(range(t0 // 128, (t0 + tn) // 128))
                    ps, bp = pmm.next()
                    for c in range(16):
                        S.do("pe", "matmul", [bw] + [b_xnT[q] for q in tcs], [bp], ps[:, 0:tn], w[:, c, oc * 128:(oc + 1) * 128],
                             xnT[:, c, t0:t0 + tn], start=(c == 0), stop=(c == 15))
                    zt, bz = zr.next()
                    S.do("act", "copy", [bp], [bz], out=zt[:, 0:tn], in_=ps[:, 0:tn])
                    pt, bpt = p12r.next()
                    for k2 in range(0, len(tcs), 2):
                        psz, bpz = pz.next()
                        for k in range(k2, k2 + 2):
                            S.do("pe", "matmul", [bz, b_cs], [bpz], psz[:, k - k2, :], zt[:, k * 128:(k + 1) * 128], cs[:],
                                 start=True, stop=True)
                        S.do("dve", "tensor_copy", [bpz], [bpt], out=pt[:, k2:k2 + 2, :], in_=psz[:])
                    nk = len(tcs)
                    S.dma("sp", P12[g, t0:t0 + tn, :].rearrange("(k p) n -> p k n", p=128), pt[:, 0:nk, :], [bpt], [S.B("P12", g, t0)])
    if K.stop_after == "ab1":
        return
    scale = 1.0 / float(np.sqrt(T * 128.0))
    scale_c = 1.0 / float(np.sqrt(TCX * 128.0))
    with K.phase("ab3") as P:
        p12 = P.sb([128, 8, NLAT, 256], BF16)
        b_p12 = Buf()
        for g in range(8):
            S.dma("sp", p12[:, g, :, :], P12[g, 0:T, :].rearrange("(k p) n -> p k n", p=128), [], [b_p12])
        p12c = P.sb([128, 8, 2, 256], BF16)
        b_p12c = Buf()
        for g in range(8):
            S.dma("sp", p12c[:, g, :, :], P12[g, T:NTOK, :].rearrange("(k p) n -> p k n", p=128), [], [b_p12c])
        dr = Ring([P.sb([128, 2, NLAT, 512], BF16) for _ in range(2)])
        dc = P.sb([128, 2, 2, 256], BF16)
        b_dc = Buf()
        for m in range(2):
            S.dma("sp", dc[:, m, :, :], dftC_d[m].rearrange("(k p) t -> p k t", p=128), [], [b_dc])
        pf = Ring([P.ps([128, 512], F32) for _ in range(3)])
        yr = Ring([P.sb([128, 512], BF16) for _ in range(3)])
        for tt in range(4):
            dt_, bd = dr.next()
            for m in range(2):
                S.dma("sp", dt_[:, m, :, :], dftT_d[m].rearrange("(k p) t -> p k t", p=128)[:, :, tt * 512:(tt + 1) * 512], [], [bd])
            for g in range(8):
                ps, bp = pf.next()
                n = 0
                for m in range(2):
                    for k in range(NLAT):
                        S.do("pe", "matmul", [bd, b_p12], [bp], ps[:], p12[:, g, k, m * 128:(m + 1) * 128], dt_[:, m, k, :],
                             start=(n == 0), stop=(n == 2 * NLAT - 1))
                        n += 1
                yt, by = yr.next()
                S.do("act", "mul", [bp], [by], out=yt[:], in_=ps[:], mul=scale)
                S.dma("sp", catT[1024 + g * 128:1024 + (g + 1) * 128, tt * 512:(tt + 1) * 512], yt[:], [by], [Buf()])
        for g in range(8):
            ps, bp = pf.next()
            n = 0
            for m in range(2):
                for k in range(2):
                    S.do("pe", "matmul", [b_dc, b_p12c], [bp], ps[:, 0:TCX], p12c[:, g, k, m * 128:(m + 1) * 128], dc[:, m, k, :],
                         start=(n == 0), stop=(n == 3))
                    n += 1
            yt, by = yr.next()
            S.do("act", "mul", [bp], [by], out=yt[:, 0:TCX], in_=ps[:, 0:TCX], mul=scale_c)
            S.dma("sp", catT[1024 + g * 128:1024 + (g + 1) * 128, T:NTOK], yt[:, 0:TCX], [by], [Buf()])
    if K.stop_after == "ab3":
        return
    phase_outproj(K, layer, catT, 16, w_out, H, modD, NTC)


def phase_outproj(K, layer, yT_d, kc, w_out, H, modD, ntc):
    S = K.S
    with K.phase("oproj") as P:
        nr = 2 if ntc > NLAT else 1
        G, bG = load_gate_rows(K, P, modD, layer, 1, nr)
        ntok = ntc * 128
        half = ntok // 2 if kc > 16 else ntok
        wr = Ring([P.sb([128, 16, 512], BF16) for _ in range(4 if kc > 16 else 2)])
        pm = Ring([P.ps([128, 512], F32) for _ in range(3)])
        hr = Ring([P.sb([128, 512], F32) for _ in range(3)])
        tr = Ring([P.sb([128, 512], F32) for _ in range(3)])
        yT = P.sb([128, kc, half], BF16)
        b_y = Buf()
        for t0 in range(0, ntok, half):
            for c0 in range(0, kc, 8):
                S.dma("sp", yT[:, c0:c0 + 8, :], yT_d.rearrange("(c p) t -> p c t", p=128)[:, c0:c0 + 8, t0:t0 + half], [], [b_y])
            for n in range(4):
                ws = []
                for kh in range(kc // 16):
                    w, bw = wr.next()
                    S.dma("pool", w[:], w_out.rearrange("(c p) n -> p c n", p=128)[:, kh * 16:(kh + 1) * 16, n * 512:(n + 1) * 512], [], [bw])
                    ws.append((w, bw))
                for tcl in range(half // 128):
                    tc = t0 // 128 + tcl
                    r = 0 if tc < NLAT else 1
                    ps, bp = pm.next()
                    for c in range(kc):
                        w, bw = ws[c // 16]
                        S.do("pe", "matmul", [bw, b_y], [bp], ps[:], yT[:, c, tcl * 128:(tcl + 1) * 128], w[:, c % 16, :],
                             start=(c == 0), stop=(c == kc - 1))
                    ht, bh = hr.next()
                    hb = S.B("H", tc, n)
                    S.dma("sp", ht[:], H[tc * 128:(tc + 1) * 128, n * 512:(n + 1) * 512], [hb], [bh])
                    tt, bt = tr.next()
                    S.do("dve", "tensor_tensor", [bp, bG[r]], [bt], out=tt[:], in0=ps[:], in1=G[r][:, n * 512:(n + 1) * 512], op=ALU.mult)
                    S.do("pool", "tensor_tensor", [bt, bh], [bh], out=ht[:], in0=tt[:], in1=ht[:], op=ALU.add)
                    S.dma("sp", H[tc * 128:(tc + 1) * 128, n * 512:(n + 1) * 512], ht[:], [bh], [hb])


def phase_moe_pre(K, layer, has_ctx, H, modD, norm_g, w_router, tris_d, iota_d, ident, identb, XE, SELT, SELTC):
    S = K.S
    nc = K.nc
    ntc = NTC if has_ctx else NLAT
    ntok = ntc * 128
    nslot = NSLOT if has_ctx else CAP
    with ExitStack() as outer:
        xm = K.sbuf(outer, "xm", [128, ntc, D], BF16)
        b_xm = [Buf() for _ in range(ntc)]
        R = K.sbuf(outer, "R", [128, ntc, NE], F32)
        Mk = K.sbuf(outer, "Mk", [128, ntc, NE], F32)
        GM = K.sbuf(outer, "GM", [128, ntc, NE], F32)
        with K.phase(f"e1a{layer}") as P:
            A, Bt, bA, bB = load_mod_rows(K, P, modD, layer, 2, norm_g[layer, 1:2, :], 2 if has_ctx else 1)
            wr32 = P.sb([128, 16, NE], F32)
            b_wr = Buf()
            S.dma("sp", wr32[:], w_router.rearrange("(c p) e -> p c e", p=128), [], [b_wr])
            ssq = P.sb([128, ntc], F32)
            rstd = P.sb([128, ntc], F32)
            hr = Ring([P.sb([128, D], F32) for _ in range(2)])
            t1r = Ring([P.sb([128, D], F32) for _ in range(2)])
            junk = P.sb([128, D], BF16)
            bj = Buf()
            xtr = Ring([P.sb([128, 16, 128], F32) for _ in range(2)])
            ptr = Ring([P.ps([128, 4, 128], F32) for _ in range(4)])
            plg = Ring([P.ps([128, NE], F32) for _ in range(2)])
            Pt = P.sb([128, ntc, NE], F32)
            b_Pt = [Buf() for _ in range(ntc)]
            mx = P.sb([128, ntc], F32)
            sm = P.sb([128, ntc], F32)
            for tc in range(ntc):
                r = 0 if tc < NLAT else 1
                h, bh = hr.next()
                S.dma("sp", h[:], H[tc * 128:(tc + 1) * 128, :], [], [bh])
                bs = Buf()
                rms_rstd(S, P, h[:], bh, junk[:], bj, ssq, rstd, bs, tc, D)
                t1, bt1 = t1r.next()
                S.do("dve", "scalar_tensor_tensor", [bh, bs, bA[r]], [bt1], out=t1[:], in0=h[:], scalar=rstd[:, tc:tc + 1], in1=A[r][:],
                     op0=ALU.mult, op1=ALU.mult)
                S.do("dve", "tensor_tensor", [bt1, bB[r]], [bt1], out=t1[:], in0=t1[:], in1=Bt[r][:], op=ALU.add)
                S.do("act", "copy", [bt1], [b_xm[tc]], out=xm[:, tc, :], in_=t1[:])
                xt, bx = xtr.next()
                for q in range(4):
                    ps, bp = ptr.next()
                    for j in range(4):
                        c = q * 4 + j
                        S.do("pe", "transpose", [bt1], [bp], ps[:, j, :], t1[:, c * 128:(c + 1) * 128], ident[:])
                    S.do("act" if q % 2 else "dve", "copy" if q % 2 else "tensor_copy", [bp], [bx], out=xt[:, q * 4:(q + 1) * 4, :], in_=ps[:])
                pl, bl = plg.next()
                for c in range(16):
                    S.do("pe", "matmul", [bx, b_wr], [bl], pl[:], xt[:, c, :], wr32[:, c, :], start=(c == 0), stop=(c == 15))
                bm = Buf()
                S.do("dve", "tensor_reduce", [bl], [bm], out=mx[:, tc:tc + 1], in_=pl[:], axis=AX.X, op=ALU.max)
                S.do("dve", "tensor_scalar", [bm], [bm], out=mx[:, tc:tc + 1], in0=mx[:, tc:tc + 1], scalar1=-1.0, scalar2=None, op0=ALU.mult)
                S.do("act", "activation", [bl, bm], [b_Pt[tc], bm], out=Pt[:, tc, :], in_=pl[:], func=AF.Exp, bias=mx[:, tc:tc + 1], scale=1.0,
                     accum_out=sm[:, tc:tc + 1])
                S.do("dve", "reciprocal", [bm], [bm], out=sm[:, tc:tc + 1], in_=sm[:, tc:tc + 1])
                S.do("dve", "tensor_scalar", [bm, b_Pt[tc]], [b_Pt[tc]], out=Pt[:, tc, :], in0=Pt[:, tc, :], scalar1=sm[:, tc:tc + 1], scalar2=None,
                     op0=ALU.mult)
            PT = P.sb([NE, ntok], F32)
            b_PT = Buf()
            for q in range(0, ntc, 4):
                ps, bp = ptr.next()
                nq = min(4, ntc - q)
                for j in range(nq):
                    S.do("pe", "transpose", [b_Pt[q + j]], [bp], ps[0:NE, j, :], Pt[:, q + j, :], ident[:])
                S.do("dve", "tensor_copy", [bp], [b_PT], out=PT[:, q * 128:(q + nq) * 128], in_=ps[0:NE, 0:nq, :])
            wa = P.sb([NE, T], F32)
            wb = P.sb([NE, T], F32)
            m8 = P.sb([NE, 8], F32)
            tau = P.sb([NE, 2], F32)
            b_wa, b_wb, b_m8, b_tau = Buf(), Buf(), Buf(), Buf()
            seqs = [(0, T, CAP, 0)] + ([(T, TCX, CAPC, 1)] if has_ctx else [])
            maskT = P.sb([NE, ntok], F32)
            b_mT = Buf()
            for (t0, tn, cap, si) in seqs:
                cur, bc, oth, bo = PT[:, t0:t0 + tn], b_PT, wa[:, 0:tn], b_wa
                for rd in range(cap // 8):
                    S.do("dve", "max", [bc], [b_m8], out=m8[:], in_=cur)
                    if rd < cap // 8 - 1:
                        S.do("dve", "match_replace", [bc, b_m8], [bo], out=oth, in_to_replace=m8[:], in_values=cur, imm_value=-1.0)
                        if rd == 0:
                            cur, bc, oth, bo = wa[:, 0:tn], b_wa, wb[:, 0:tn], b_wb
                        else:
                            cur, bc, oth, bo = oth, bo, cur, bc
                S.do("dve", "tensor_copy", [b_m8], [b_tau], out=tau[:, si:si + 1], in_=m8[:, 7:8])
                S.do("dve", "tensor_scalar", [b_PT, b_tau], [b_mT], out=maskT[:, t0:t0 + tn], in0=PT[:, t0:t0 + tn], scalar1=tau[:, si:si + 1],
                     scalar2=None, op0=ALU.is_ge)
            pk = P.ps([128, ntc, NE], F32)
            b_pk = Buf()
            for tc in range(ntc):
                S.do("pe", "transpose", [b_mT], [b_pk], pk[:, tc, :], maskT[:, tc * 128:(tc + 1) * 128], ident[0:NE, 0:NE])
            b_Mk, b_GM, b_R = Buf(), Buf(), Buf()
            S.do("dve", "tensor_copy", [b_pk], [b_Mk], out=Mk[:], in_=pk[:])
            S.do("dve", "tensor_tensor", [b_Mk] + b_Pt, [b_GM], out=GM[:], in0=Mk[:], in1=Pt[:], op=ALU.mult)
            Mb = P.sb([128, ntc, NE], BF16)
            b_Mb = Buf()
            S.do("dve", "tensor_copy", [b_Mk], [b_Mb], out=Mb[:], in_=Mk[:])
            trf = P.sb([128, 128], F32)
            trb = P.sb([128, 128], BF16)
            oneb = P.sb([128, 128], BF16)
            b_tr, b_one = Buf(), Buf()
            S.dma("sp", trf[:], tris_d[0], [], [b_tr])
            S.do("dve", "tensor_copy", [b_tr], [b_tr], out=trb[:], in_=trf[:])
            S.do("dve", "memset", [], [b_one], oneb[:], 1.0)
            pr2 = P.ps([128, ntc, NE], F32)
            b_pr2 = Buf()
            for tc in range(ntc):
                first = 0 if tc < NLAT else NLAT
                for t2 in range(first, tc + 1):
                    S.do("pe", "matmul", [b_Mb, b_tr, b_one], [b_pr2], pr2[:, tc, :], (trb if t2 == tc else oneb)[:], Mb[:, t2, :],
                         start=(t2 == first), stop=(t2 == tc))
            S.do("dve", "tensor_copy", [b_pr2], [b_R], out=R[:], in_=pr2[:])
            if has_ctx:
                S.do("dve", "tensor_scalar", [b_R], [b_R], out=R[:, NLAT:, :], in0=R[:, NLAT:, :], scalar1=float(CAP), scalar2=None, op0=ALU.add)
            if "Pt" in K.dbg:
                dd = K.scratch("Pt", [ntok, NE])
                S.dma("sp", dd.rearrange("(k p) e -> p k e", p=128), Pt[:], b_Pt, [Buf()])
                dd = K.scratch("Mk", [ntok, NE])
                S.dma("sp", dd.rearrange("(k p) e -> p k e", p=128), Mk[:], [b_Mk], [Buf()])
                dd = K.scratch("Rk", [ntok, NE])
                S.dma("sp", dd.rearrange("(k p) e -> p k e", p=128), R[:], [b_R], [Buf()])
        if K.stop_after == "e1a":
            return
        with K.phase(f"e1b{layer}") as P:
            iot = P.sb([128, NSLOT], F32)
            b_io = Buf()
            S.dma("sp", iot[:], iota_d[:, :], [], [b_io])
            selr = Ring([P.sb([128, NLAT, CAP], BF16) for _ in range(2)])
            sgr = Ring([P.sb([128, NLAT, CAP], BF16) for _ in range(2)])
            selc = Ring([P.sb([128, 2, CAPC], BF16) for _ in range(2)])
            sgc = Ring([P.sb([128, 2, CAPC], BF16) for _ in range(2)])
            xer = Ring([P.sb([128, 16, nslot], BF16) for _ in range(2)])
            str_ = Ring([P.sb([128, NLAT, 2, 128], BF16) for _ in range(2)])
            stc = Ring([P.sb([CAPC, 2, 128], BF16) for _ in range(2)])
            pg = Ring([P.ps([128, 512], F32) for _ in range(3)])
            pt2 = Ring([P.ps([128, 8, 128], BF16) for _ in range(3)])
            for e in range(NE):
                sel, bsel = selr.next()
                sg, bsg = sgr.next()
                for tc in range(NLAT):
                    S.do("dve", "tensor_scalar", [b_io], [bsel], out=sel[:, tc, :], in0=iot[:, 0:CAP], scalar1=R[:, tc, e:e + 1],
                         scalar2=Mk[:, tc, e:e + 1], op0=ALU.is_equal, op1=ALU.mult)
                    S.do("act", "mul", [bsel], [bsg], out=sg[:, tc, :], in_=sel[:, tc, :], mul=GM[:, tc, e:e + 1])
                if has_ctx:
                    sc_, bsc = selc.next()
                    sgc_, bsgc = sgc.next()
                    for k in range(2):
                        S.do("dve", "tensor_scalar", [b_io], [bsc], out=sc_[:, k, :], in0=iot[:, CAP:NSLOT], scalar1=R[:, NLAT + k, e:e + 1],
                             scalar2=Mk[:, NLAT + k, e:e + 1], op0=ALU.is_equal, op1=ALU.mult)
                        S.do("dve", "tensor_scalar", [b_io], [bsgc], out=sgc_[:, k, :], in0=iot[:, CAP:NSLOT], scalar1=R[:, NLAT + k, e:e + 1],
                             scalar2=GM[:, NLAT + k, e:e + 1], op0=ALU.is_equal, op1=ALU.mult)
                xe, bxe = xer.next()
                for c in range(16):
                    ps, bp = pg.next()
                    for tc in range(NLAT):
                        S.do("pe", "matmul", [bsel, b_xm[tc]], [bp], ps[:, 0:CAP], xm[:, tc, c * 128:(c + 1) * 128], sel[:, tc, :],
                             start=(tc == 0), stop=(tc == NLAT - 1))
                    if has_ctx:
                        for k in range(2):
                            S.do("pe", "matmul", [bsc, b_xm[NLAT + k]], [bp], ps[:, CAP:NSLOT], xm[:, NLAT + k, c * 128:(c + 1) * 128], sc_[:, k, :],
                                 start=(k == 0), stop=(k == 1))
                    S.do("act", "copy", [bp], [bxe], out=xe[:, c, :], in_=ps[:, 0:nslot])
                S.dma("sp", XE[e].rearrange("(c p) s -> p c s", p=128)[:, :, 0:nslot], xe[:], [bxe], [S.B("XE", layer, e)])
                st, bst = str_.next()
                for q in range(0, NLAT, 4):
                    ps, bp = pt2.next()
                    for j in range(4):
                        for s2 in range(2):
                            S.do("pe", "transpose", [bsg], [bp], ps[:, j * 2 + s2, :], sg[:, q + j, s2 * 128:(s2 + 1) * 128], identb[:])
                    S.do("dve", "tensor_copy", [bp], [bst], out=st[:, q:q + 4, :, :], in_=ps[:].rearrange("p (j s) t -> p j s t", s=2))
                S.dma("sp", SELT[:, :, e * 2:(e + 1) * 2, :].rearrange("k p s t -> p k s t"), st[:], [bst], [S.B("SELT", layer, e)])
                if has_ctx:
                    stc_, bstc = stc.next()
                    ps, bp = pt2.next()
                    for k in range(2):
                        S.do("pe", "transpose", [bsgc], [bp], ps[0:CAPC, k, :], sgc_[:, k, :], identb[:])
                    S.do("dve", "tensor_copy", [bp], [bstc], out=stc_[:], in_=ps[0:CAPC, 0:2, :])
                    S.dma("sp", SELTC[:, :, e, :].rearrange("k p t -> p k t"), stc_[:], [bstc], [S.B("SELTC", layer, e)])


def modulate_xnT(K, pname, layer, H, modD, g_row, xnT, b_xnT, identb, ntc):
    S = K.S
    with K.phase(pname) as P:
        A, Bt, bA, bB = load_mod_rows(K, P, modD, layer, 1, g_row)
        ssq = P.sb([128, ntc], F32)
        rstd = P.sb([128, ntc], F32)
        hr = Ring([P.sb([128, D], F32) for _ in range(2)])
        junk = P.sb([128, D], BF16)
        bj = Buf()
        t1 = P.sb([128, D], F32)
        bt1 = Buf()
        xr = Ring([P.sb([128, D], BF16) for _ in range(2)])
        ptr = Ring([P.ps([128, 8, 128], BF16) for _ in range(2)])
        for tc in range(ntc):
            r = 0 if tc < NLAT else 1
            h, bh = hr.next()
            S.dma("sp", h[:], H[tc * 128:(tc + 1) * 128, :], [], [bh])
            bs = Buf()
            rms_rstd(S, P, h[:], bh, junk[:], bj, ssq, rstd, bs, tc, D)
            S.do("dve", "scalar_tensor_tensor", [bh, bs, bA[r]], [bt1], out=t1[:], in0=h[:], scalar=rstd[:, tc:tc + 1], in1=A[r][:],
                 op0=ALU.mult, op1=ALU.mult)
            xt, bx = xr.next()
            S.do("dve", "tensor_tensor", [bt1, bB[r]], [bx], out=xt[:], in0=t1[:], in1=Bt[r][:], op=ALU.add)
            for half in range(2):
                ps, bp = ptr.next()
                for j in range(8):
                    c = half * 8 + j
                    S.do("pe", "transpose", [bx], [bp], ps[:, j, :], xt[:, c * 128:(c + 1) * 128], identb[:])
                S.do("act", "copy", [bp], [b_xnT[tc]], out=xnT[:, half * 8:(half + 1) * 8, tc * 128:(tc + 1) * 128], in_=ps[:])


def phase_ssd(K, layer, H, modD, norm_g, w_in, conv_w, conv_b, dt_bias, a_log, d_skip, ng, w_out, tris_d, ident, identb, scr):
    S = K.S
    nc = K.nc
    ZS, BCT, XS, BTOK, DTS, YB, YT = scr
    ntc = NTC
    ttiles = [(i * 512, 512) for i in range(4)] + [(T, TCX)]
    with ExitStack() as outer:
        xnT = K.sbuf(outer, "xnT_ssd", [128, 16, NTOK], BF16)
        b_xnT = [Buf() for _ in range(ntc)]
        modulate_xnT(K, "s1", layer, H, modD, norm_g[layer, 0:1, :], xnT, b_xnT, identb, ntc)
        with K.phase("s2") as P:
            wr = Ring([P.sb([128, 16, 512], BF16) for _ in range(2)])
            pmm = Ring([P.ps([128, 512], F32) for _ in range(3)])
            ptr = Ring([P.ps([128, 8, 128], BF16) for _ in range(2)])
            pst = P.ps([128, 512], F32)
            b_pst = Buf()
            cwr = P.sb([120, 2, 128], F32)
            cbr = P.sb([48, 128], F32)
            cwT = P.sb([128, 5, 48], F32)
            cbT = P.sb([128, 48], F32)
            b_cwr, b_cw = Buf(), Buf()
            cw_rows = conv_w.rearrange("k (c p) -> (k c) p", p=128)
            for hf in range(2):
                S.dma("sp", cwr[:, hf, :], cw_rows[hf * 120:(hf + 1) * 120, :], [], [b_cwr])
            S.dma("sp", cbr[:], conv_b.rearrange("(c p) -> c p", p=128), [], [b_cwr])
            for hf in range(2):
                S.do("pe", "transpose", [b_cwr], [b_pst], pst[:, hf * 120:(hf + 1) * 120], cwr[:, hf, :], ident[0:120, 0:120])
            S.do("pe", "transpose", [b_cwr], [b_pst], pst[:, 240:288], cbr[:], ident[0:48, 0:48])
            S.do("dve", "tensor_copy", [b_pst], [b_cw], out=cwT[:].rearrange("p k c -> p (k c)"), in_=pst[:, 0:240])
            S.do("dve", "tensor_copy", [b_pst], [b_cw], out=cbT[:], in_=pst[:, 240:288])
            zr = Ring([P.sb([128, 512], BF16) for _ in range(3)])
            for pc in range(8):
                w, bw = wr.next()
                S.dma("pool", w[:], wpiece(w_in, pc * 512, 512), [], [bw])
                for tc in range(NLAT):
                    ps, bp = pmm.next()
                    for c in range(16):
                        S.do("pe", "matmul", [bw, b_xnT[tc]], [bp], ps[:], xnT[:, c, tc * 128:(tc + 1) * 128], w[:, c, :],
                             start=(c == 0), stop=(c == 15))
                    zt, bz = zr.next()
                    S.do("act", "activation", [bp], [bz], out=zt[:], in_=ps[:], func=AF.Silu)
                    S.dma("sp", ZS[tc * 128:(tc + 1) * 128, pc * 512:(pc + 1) * 512], zt[:], [bz], [Buf()])
            rbr = Ring([P.sb([128, NTOK + 8], F32) for _ in range(2)])
            accr = Ring([P.sb([128, NTOK], F32) for _ in range(2)])
            obr = Ring([P.sb([128, NTOK], BF16) for _ in range(2)])
            tsr = Ring([P.sb([128, ntc, 128], BF16) for _ in range(2)])
            for rb, bb in zip(rbr.tiles, rbr.bufs):
                S.do("dve", "memset", [], [bb], rb[:], 0.0)
            LOFF, COFF = 2, T + 6
            for pc in range(12):
                w, bw = wr.next()
                S.dma("pool", w[:], wpiece(w_in, 4096 + pc * 512, 512), [], [bw])
                for oc in range(4):
                    ch = pc * 4 + oc
                    rb, brb = rbr.next()
                    for (t0, tn) in ttiles:
                        tcs = list(range(t0 // 128, (t0 + tn) // 128))
                        ps, bp = pmm.next()
                        for c in range(16):
                            S.do("pe", "matmul", [bw] + [b_xnT[q] for q in tcs], [bp], ps[:, 0:tn], w[:, c, oc * 128:(oc + 1) * 128],
                                 xnT[:, c, t0:t0 + tn], start=(c == 0), stop=(c == 15))
                        off = LOFF + t0 if t0 < T else COFF
                        S.do("act", "copy", [bp], [brb], out=rb[:, off:off + tn], in_=ps[:, 0:tn])
                    acc, bacc = accr.next()
                    for (a0, an, ro) in ((0, T, LOFF), (T, TCX, COFF)):
                        for k in range(5):
                            src = rb[:, ro - 2 + k:ro - 2 + k + an]
                            if k == 0:
                                S.do("dve", "tensor_scalar", [brb, b_cw], [bacc], out=acc[:, a0:a0 + an], in0=src, scalar1=cwT[:, k, ch:ch + 1],
                                     scalar2=None, op0=ALU.mult)
                            else:
                                S.do("dve", "scalar_tensor_tensor", [brb, b_cw, bacc], [bacc], out=acc[:, a0:a0 + an], in0=src,
                                     scalar=cwT[:, k, ch:ch + 1], in1=acc[:, a0:a0 + an], op0=ALU.mult, op1=ALU.add)
                    ob, bob = obr.next()
                    S.do("act", "activation", [bacc, b_cw], [bob], out=ob[:], in_=acc[:], func=AF.Silu, bias=cbT[:, ch:ch + 1], scale=1.0)
                    if ch >= 32:
                        S.dma("sp", BCT[(ch - 32) * 128:(ch - 31) * 128, :], ob[:], [bob], [Buf()])
                    if ch < 40:
                        ts, bts = tsr.next()
                        for q in range(0, ntc, 8):
                            nq = min(8, ntc - q)
                            ps, bp = ptr.next()
                            for j in range(nq):
                                S.do("pe", "transpose", [bob], [bp], ps[:, j, :], ob[:, (q + j) * 128:(q + j + 1) * 128], identb[:])
                            S.do("dve", "tensor_copy", [bp], [bts], out=ts[:, q:q + nq, :], in_=ps[:, 0:nq, :])
                        if ch < 32:
                            S.dma("sp", XS[:, ch * 128:(ch + 1) * 128].rearrange("(k p) n -> p k n", p=128), ts[:], [bts], [Buf()])
                        else:
                            S.dma("sp", BTOK[:, (ch - 32) * 128:(ch - 31) * 128].rearrange("(k p) n -> p k n", p=128), ts[:], [bts], [Buf()])
            wdt = P.sb([128, 16, 128], BF16)
            b_wdt = Buf()
            S.dma("pool", wdt[:], wpiece(w_in, 10240, 128), [], [b_wdt])
            dtb = P.sb([128, 128], F32)
            b_dtb = Buf()
            S.dma("sp", dtb[:], dt_bias.rearrange("a h -> (a h)").unsqueeze(0).broadcast_to([128, 128]), [], [b_dtb])
            xbr = Ring([P.sb([128, 128], F32) for _ in range(2)])
            abr = Ring([P.sb([128, 128], F32) for _ in range(2)])
            for tc in range(ntc):
                ps, bp = pmm.next()
                for c in range(16):
                    S.do("pe", "matmul", [b_wdt, b_xnT[tc]], [bp], ps[:, 0:128], xnT[:, c, tc * 128:(tc + 1) * 128], wdt[:, c, :],
                         start=(c == 0), stop=(c == 15))
                xb, bxb = xbr.next()
                ab, bab = abr.next()
                S.do("dve", "tensor_tensor", [bp, b_dtb], [bxb], out=xb[:], in0=ps[:, 0:128], in1=dtb[:], op=ALU.add)
                S.do("dve", "scalar_tensor_tensor", [bxb], [bab], out=ab[:], in0=xb[:], scalar=-1.0, in1=xb[:], op0=ALU.mult, op1=ALU.max)
                S.do("act", "activation", [bab], [bab], out=ab[:], in_=ab[:], func=AF.Exp, scale=-1.0)
                S.do("act", "activation", [bab], [bab], out=ab[:], in_=ab[:], func=AF.Ln, bias=1.0, scale=1.0)
                S.do("dve", "scalar_tensor_tensor", [bxb, bab], [bxb], out=xb[:], in0=xb[:], scalar=0.0, in1=ab[:], op0=ALU.max, op1=ALU.add)
                S.dma("sp", DTS[tc * 128:(tc + 1) * 128, :], xb[:], [bxb], [Buf()])
    if K.stop_after == "s2":
        return
    with K.phase("s3") as P:
        trf = P.sb([128, 4, 128], F32)
        b_trf = Buf()
        S.dma("sp", trf[:], tris_d.rearrange("f k m -> k f m"), [], [b_trf])
        onef = P.sb([128, 128], F32)
        b_one = Buf()
        S.do("dve", "memset", [], [b_one], onef[:], 1.0)
        LT, LE, GT, GE = 0, 1, 2, 3
        arow = P.sb([128, 128], F32)
        b_arow = Buf()
        S.dma("sp", arow[:], a_log.rearrange("a h -> (a h)").unsqueeze(0).broadcast_to([128, 128]), [], [b_arow])
        S.do("act", "activation", [b_arow], [b_arow], out=arow[:], in_=arow[:], func=AF.Exp)
        S.do("dve", "tensor_scalar", [b_arow], [b_arow], out=arow[:], in0=arow[:], scalar1=-1.0, scalar2=None, op0=ALU.mult)
        drow = P.sb([128, 64], F32)
        b_drow = Buf()
        S.dma("sp", drow[:], d_skip.unsqueeze(0).broadcast_to([128, 64]), [], [b_drow])
        ng32 = P.sb([32, 128], F32)
        ngT = P.sb([128, 32], F32)
        b_ng = Buf()
        S.dma("sp", ng32[:], ng.rearrange("(c p) -> c p", p=128), [], [b_ng])
        xsr = Ring([P.sb([128, 4096], BF16) for _ in range(2)])
        btr = Ring([P.sb([128, 1024], BF16) for _ in range(2)])
        bcr = Ring([P.sb([128, 16, 128], BF16) for _ in range(2)])
        dtr = Ring([P.sb([128, 128], F32) for _ in range(2)])
        dar = Ring([P.sb([128, 64], F32) for _ in range(2)])
        ecr = Ring([P.sb([128, 3, 64], F32) for _ in range(2)])
        xg = P.sb([128, 4096], BF16)
        xgd = P.sb([128, 4096], BF16)
        b_xg, b_xgd = Buf(), Buf()
        Sf = P.sb([128, 8, 512], F32)
        Sb = P.sb([128, 8, 512], BF16)
        b_Sf = [Buf() for _ in range(8)]
        b_Sb = [Buf() for _ in range(8)]
        yar = Ring([P.sb([128, 4096], F32) for _ in range(2)])
        dtri = Ring([P.sb([128, 8, 128], F32) for _ in range(2)])
        Er = Ring([P.sb([128, 8, 128], F32) for _ in range(2)])
        Mr = Ring([P.sb([128, 8, 128], BF16) for _ in range(2)])
        cbr_ = Ring([P.sb([128, 128], F32) for _ in range(2)])
        tmr = Ring([P.sb([128, 512], F32) for _ in range(2)])
        zsr = Ring([P.sb([128, 4096], BF16) for _ in range(1)])
        ybt = P.sb([128, 4096], F32)
        b_ybt = Buf()
        yh = P.sb([128, 4096], BF16)
        b_yh = Buf()
        ytr = Ring([P.sb([128, 32, 128], BF16) for _ in range(2)])
        junk = P.sb([128, 4096], BF16)
        bj = Buf()
        ssq = P.sb([128, NLAT], F32)
        rstd = P.sb([128, NLAT], F32)
        p_c = P.ps([128, 3, 64], F32)
        p_cb = P.ps([128, 128], F32)
        p_seg = [P.ps([128, 4, 128], F32) for _ in range(2)]
        p_y = P.ps([128, 512], F32)
        p_y2 = P.ps([128, 512], F32)
        p_st = P.ps([128, 512], F32)
        p_t = P.ps([128, 8, 128], BF16)
        b_pc, b_pcb, b_pseg, b_py, b_py2, b_pst, b_pt = Buf(), Buf(), [Buf(), Buf()], Buf(), Buf(), Buf(), Buf()
        S.do("pe", "transpose", [b_ng], [b_py], p_y[:, 0:32], ng32[:], ident[0:32, 0:32])
        S.do("dve", "tensor_copy", [b_py], [b_ng], out=ngT[:], in_=p_y[:, 0:32])
        for d in (1, 0):
            Lm, Rt, CBm, cI, cA = (GT, LE, LE, LE, GT) if d == 0 else (LT, GE, GE, GE, LT)
            for g in range(8):
                S.do("dve", "memset", [], [b_Sf[g]], Sf[:, g, :], 0.0)
                S.do("pool", "memset", [], [b_Sb[g]], Sb[:, g, :], 0.0)
            order = [16, 17] + list(range(NLAT)) if d == 0 else [17, 16] + list(range(NLAT - 1, -1, -1))
            for tc in order:
                lat = tc < NLAT
                xs, bxs = xsr.next()
                S.dma("sp", xs[:], XS[tc * 128:(tc + 1) * 128, :], [], [bxs])
                bt, bbt = btr.next()
                S.dma("sp", bt[:], BTOK[tc * 128:(tc + 1) * 128, :], [], [bbt])
                dt_, bdt = dtr.next()
                S.dma("sp", dt_[:], DTS[tc * 128:(tc + 1) * 128, :], [], [bdt])
                if lat:
                    bc, bbc = bcr.next()
                    S.dma("sp", bc[:], BCT[:, tc * 128:(tc + 1) * 128].rearrange("(j p) t -> p j t", p=128), [], [bbc])
                da, bda = dar.next()
                S.do("dve", "tensor_tensor", [bdt, b_arow], [bda], out=da[:], in0=dt_[:, d * 64:(d + 1) * 64], in1=arow[:, d * 64:(d + 1) * 64],
                     op=ALU.mult)
                S.do("pe", "matmul", [bda, b_trf], [b_pc], p_c[:, 0, :], trf[:, cI, :], da[:], start=True, stop=True)
                S.do("pe", "matmul", [bda, b_trf], [b_pc], p_c[:, 1, :], trf[:, cA, :], da[:], start=True, stop=True)
                S.do("pe", "matmul", [bda, b_one], [b_pc], p_c[:, 2, :], onef[:], da[:], start=True, stop=True)
                ec, bec = ecr.next()
                S.do("act", "activation", [b_pc], [bec], out=ec[:], in_=p_c[:], func=AF.Exp)
                dtv = dt_[:, d * 64:(d + 1) * 64]
                S.do("dve", "tensor_tensor", [bxs, bdt], [b_xg], out=xg[:].rearrange("p (h q) -> p h q", h=64),
                     in0=xs[:].rearrange("p (h q) -> p h q", h=64), in1=dtv.unsqueeze(2).to_broadcast([128, 64, 64]), op=ALU.mult)
                S.do("pool", "tensor_tensor", [b_xg, bec], [b_xgd], out=xgd[:].rearrange("p (h q) -> p h q", h=64),
                     in0=xg[:].rearrange("p (h q) -> p h q", h=64), in1=ec[:, 1, :].unsqueeze(2).to_broadcast([128, 64, 64]), op=ALU.mult)
                if lat:
                    ya, bya = yar.next()
                for g in range(8):
                    if lat:
                        S.do("pe", "matmul", [bbc], [b_pcb], p_cb[:], bc[:, g, :], bc[:, 8 + g, :], start=True, stop=True)
                        cb, bcb = cbr_.next()
                        S.do("dve", "tensor_tensor", [b_pcb, b_trf], [bcb], out=cb[:], in0=p_cb[:], in1=trf[:, CBm, :], op=ALU.mult)
                        dtt, bdtt = dtri.next()
                        S.do("pool", "tensor_tensor", [b_trf, bda], [bdtt], out=dtt[:], in0=trf[:, Rt:Rt + 1, :].to_broadcast([128, 8, 128]),
                             in1=da[:, g * 8:(g + 1) * 8].unsqueeze(2).to_broadcast([128, 8, 128]), op=ALU.mult)
                        for hh in range(2):
                            S.do("pe", "matmul", [bdtt, b_trf], [b_pseg[hh]], p_seg[hh][:].rearrange("p a b -> p (a b)"), trf[:, Lm, :],
                                 dtt[:, hh * 4:(hh + 1) * 4, :].rearrange("p a b -> p (a b)"), start=True, stop=True)
                        E, bE = Er.next()
                        for hh in range(2):
                            S.do("act", "activation", [b_pseg[hh]], [bE], out=E[:, hh * 4:(hh + 1) * 4, :], in_=p_seg[hh][:], func=AF.Exp)
                        M, bM = Mr.next()
                        S.do("dve", "tensor_tensor", [bE, bcb], [bM], out=M[:], in0=E[:], in1=cb[:].unsqueeze(1).to_broadcast([128, 8, 128]),
                             op=ALU.mult)
                        for h in range(8):
                            hd = g * 8 + h
                            S.do("pe", "matmul", [bM, b_xg], [b_py], p_y[:, h * 64:(h + 1) * 64], M[:, h, :], xg[:, hd * 64:(hd + 1) * 64],
                                 start=True, stop=True)
                        S.do("pe", "matmul", [bbc, b_Sb[g]], [b_py2], p_y2[:], bc[:, 8 + g, :], Sb[:, g, :], start=True, stop=True)
                        tm, btm = tmr.next()
                        S.do("dve", "tensor_tensor", [b_py2, bec], [btm], out=tm[:].rearrange("p (h q) -> p h q", h=8),
                             in0=p_y2[:].rearrange("p (h q) -> p h q", h=8),
                             in1=ec[:, 0, g * 8:(g + 1) * 8].unsqueeze(2).to_broadcast([128, 8, 64]), op=ALU.mult)
                        S.do("dve", "tensor_tensor", [btm, b_py], [bya], out=ya[:, g * 512:(g + 1) * 512], in0=tm[:], in1=p_y[:], op=ALU.add)
                    S.do("pe", "matmul", [bbt, b_xgd], [b_pst], p_st[:], bt[:, g * 128:(g + 1) * 128], xgd[:, g * 512:(g + 1) * 512],
                         start=True, stop=True)
                    S.do("pool", "tensor_tensor", [b_Sf[g], bec], [b_Sf[g]], out=Sf[:, g, :].rearrange("p (h q) -> p h q", h=8),
                         in0=Sf[:, g, :].rearrange("p (h q) -> p h q", h=8),
                         in1=ec[:, 2, g * 8:(g + 1) * 8].unsqueeze(2).to_broadcast([128, 8, 64]), op=ALU.mult)
                    S.do("dve", "tensor_tensor", [b_Sf[g], b_pst], [b_Sf[g]], out=Sf[:, g, :], in0=Sf[:, g, :], in1=p_st[:], op=ALU.add)
                    S.do("act", "copy", [b_Sf[g]], [b_Sb[g]], out=Sb[:, g, :], in_=Sf[:, g, :])
                if not lat:
                    continue
                if d == 1:
                    S.dma("sp", YB[tc * 128:(tc + 1) * 128, :], ya[:], [bya], [S.B("YB", tc)])
                    continue
                S.dma("sp", ybt[:], YB[tc * 128:(tc + 1) * 128, :], [S.B("YB", tc)], [b_ybt])
                S.do("pool", "tensor_tensor", [bya, b_ybt], [bya], out=ya[:], in0=ya[:], in1=ybt[:], op=ALU.add)
                S.do("dve", "tensor_tensor", [bxs, b_drow], [b_ybt], out=ybt[:].rearrange("p (h q) -> p h q", h=64),
                     in0=xs[:].rearrange("p (h q) -> p h q", h=64), in1=drow[:].unsqueeze(2).to_broadcast([128, 64, 64]), op=ALU.mult)
                S.do("pool", "tensor_tensor", [bya, b_ybt], [bya], out=ya[:], in0=ya[:], in1=ybt[:], op=ALU.add)
                zs, bzs = zsr.next()
                S.dma("sp", zs[:], ZS[tc * 128:(tc + 1) * 128, :], [], [bzs])
                S.do("dve", "tensor_tensor", [bya, bzs], [bya], out=ya[:], in0=ya[:], in1=zs[:], op=ALU.mult)
                bs = Buf()
                rms_rstd(S, P, ya[:], bya, junk[:], bj, ssq, rstd, bs, tc, 4096)
                S.do("dve", "tensor_scalar", [bya, bs], [b_yh], out=yh[:], in0=ya[:], scalar1=rstd[:, tc:tc + 1], scalar2=None, op0=ALU.mult)
                yt, byt = ytr.next()
                for q in range(4):
                    for j in range(8):
                        c = q * 8 + j
                        S.do("pe", "transpose", [b_yh], [b_pt], p_t[:, j, :], yh[:, c * 128:(c + 1) * 128], identb[:])
                    S.do("dve", "tensor_tensor", [b_pt, b_ng], [byt], out=yt[:, q * 8:(q + 1) * 8, :], in0=p_t[:],
                         in1=ngT[:, q * 8:(q + 1) * 8].unsqueeze(2).to_broadcast([128, 8, 128]), op=ALU.mult)
                S.dma("sp", YT[:, tc * 128:(tc + 1) * 128].rearrange("(c p) t -> p c t", p=128), yt[:], [byt], [Buf()])
    if K.stop_after == "s3":
        return
    phase_outproj(K, layer, YT, 32, w_out, H, modD, NLAT)


def phase_final(K, H, final_g, out):
    S = K.S
    with K.phase("final") as P:
        gt = P.sb([128, D], F32)
        bg = Buf()
        S.dma("sp", gt[:], final_g.unsqueeze(0).broadcast_to([128, D]), [], [bg])
        hr = Ring([P.sb([128, D], F32) for _ in range(3)])
        junk = P.sb([128, D], BF16)
        bj = Buf()
        ssq = P.sb([128, NLAT], F32)
        rstd = P.sb([128, NLAT], F32)
        for tc in range(NLAT):
            h, bh = hr.next()
            S.dma("sp", h[:], H[tc * 128:(tc + 1) * 128, :], [], [bh])
            bs = Buf()
            rms_rstd(S, P, h[:], bh, junk[:], bj, ssq, rstd, bs, tc, D)
            S.do("dve", "scalar_tensor_tensor", [bh, bs, bg], [bh], out=h[:], in0=h[:], scalar=rstd[:, tc:tc + 1], in1=gt[:],
                 op0=ALU.mult, op1=ALU.mult)
            S.dma("sp", out[tc * 128:(tc + 1) * 128, :], h[:], [bh], [Buf()])
    K.out_written = True


def phase_moe_post(K, layer, has_ctx, Hin, H, modD, SELT, SELTC, YEo, jb):
    S = K.S
    ntc = NTC if has_ctx else NLAT
    with K.phase(f"e3{layer}") as P:
        nr = 2 if has_ctx else 1
        G, bG = load_gate_rows(K, P, modD, layer, 2, nr)
        yr = Ring([P.sb([128, 2 * NE, 512], BF16) for _ in range(2)])
        ycr = Ring([P.sb([CAPC, NE, 512], BF16) for _ in range(2)])
        sr = Ring([P.sb([128, 2 * NE, 128], BF16) for _ in range(3)])
        scr_ = Ring([P.sb([CAPC, NE, 128], BF16) for _ in range(2)])
        pm = Ring([P.ps([128, 512], F32) for _ in range(3)])
        hr = Ring([P.sb([128, 512], F32) for _ in range(3)])
        tr = Ring([P.sb([128, 512], F32) for _ in range(3)])
        for n in range(4):
            y, by = yr.next()
            S.dma("sp", y[:].rearrange("p (e s) c -> p e s c", s=2), YEo[:, jb, n, :, 0:2, :].rearrange("e p s c -> p e s c"), [], [by])
            if has_ctx:
                yc, byc = ycr.next()
                S.dma("sp", yc[:], YEo[:, jb, n, 0:CAPC, 2, :].rearrange("e p c -> p e c"), [], [byc])
            for tc in range(ntc):
                r = 0 if tc < NLAT else 1
                ps, bp = pm.next()
                if tc < NLAT:
                    st, bst = sr.next()
                    S.dma("act", st[:], SELT[tc], [], [bst])
                    for j in range(2 * NE):
                        S.do("pe", "matmul", [bst, by], [bp], ps[:], st[:, j, :], y[:, j, :], start=(j == 0), stop=(j == 2 * NE - 1))
                else:
                    st, bst = scr_.next()
                    S.dma("act", st[:], SELTC[tc - NLAT], [], [bst])
                    for j in range(NE):
                        S.do("pe", "matmul", [bst, byc], [bp], ps[:], st[:, j, :], yc[:, j, :], start=(j == 0), stop=(j == NE - 1))
                ht, bh = hr.next()
                hb = S.B("H3", tc, n)
                S.dma("sp", ht[:], Hin[tc * 128:(tc + 1) * 128, n * 512:(n + 1) * 512], [hb], [bh])
                tt, bt = tr.next()
                S.do("dve", "tensor_tensor", [bp, bG[r]], [bt], out=tt[:], in0=ps[:], in1=G[r][:, n * 512:(n + 1) * 512], op=ALU.mult)
                S.do("pool", "tensor_tensor", [bt, bh], [bh], out=ht[:], in0=tt[:], in1=ht[:], op=ALU.add)
                S.dma("sp", H[tc * 128:(tc + 1) * 128, n * 512:(n + 1) * 512], ht[:], [bh], [hb])


def phase_experts(K, XEget, wg, wu, wd, YEo, nslot, has_ctx, n_exp=2, NB=8):
    S = K.S
    with K.phase("e2") as P:
        xe = P.sb([128, NB, 16, nslot], BF16)
        hid = P.sb([128, NB, 16, nslot], BF16)
        b_xe = [Buf() for _ in range(NB)]
        b_hid = [Buf() for _ in range(NB)]
        wr = Ring([P.sb([128, 16, 512], BF16) for _ in range(3)])
        sgr = Ring([P.sb([128, nslot], F32) for _ in range(3)])
        yer = Ring([P.sb([128, 3, 512], BF16) for _ in range(2)])
        pgu = Ring([P.ps([128, 512], F32) for _ in range(4)])
        pdn = Ring([P.ps([128, 512], F32) for _ in range(3)])
        chunks = [(0, 128), (128, 128)] + ([(256, CAPC)] if has_ctx else [])
        for el in range(n_exp):
            for b in range(NB):
                S.dma("sp", xe[:, b, :, :], XEget(el, b).rearrange("(c p) s -> p c s", p=128)[:, :, 0:nslot], [], [b_xe[b]])
            for fp in range(4):
                wgt, bwg = wr.next()
                S.dma("pool", wgt[:], wpiece(wg[el], fp * 512, 512), [], [bwg])
                wut, bwu = wr.next()
                S.dma("pool", wut[:], wpiece(wu[el], fp * 512, 512), [], [bwu])
                for fo in range(4):
                    fc = fp * 4 + fo
                    for b in range(NB):
                        psg, bpg = pgu.next()
                        for c in range(16):
                            S.do("pe", "matmul", [bwg, b_xe[b]], [bpg], psg[:, 0:nslot], wgt[:, c, fo * 128:(fo + 1) * 128], xe[:, b, c, :],
                                 start=(c == 0), stop=(c == 15))
                        psu, bpu = pgu.next()
                        for c in range(16):
                            S.do("pe", "matmul", [bwu, b_xe[b]], [bpu], psu[:, 0:nslot], wut[:, c, fo * 128:(fo + 1) * 128], xe[:, b, c, :],
                                 start=(c == 0), stop=(c == 15))
                        sgt, bsg = sgr.next()
                        S.do("act", "activation", [bpg], [bsg], out=sgt[:], in_=psg[:, 0:nslot], func=AF.Silu)
                        S.do("dve", "tensor_tensor", [bsg, bpu], [b_hid[b]], out=hid[:, b, fc, :], in0=sgt[:], in1=psu[:, 0:nslot], op=ALU.mult)
            for n in range(4):
                wdt, bwd = wr.next()
                S.dma("pool", wdt[:], wpiece(wd[el], n * 512, 512), [], [bwd])
                for b in range(NB):
                    ye, bye = yer.next()
                    for si, (s0, sn) in enumerate(chunks):
                        ps, bp = pdn.next()
                        for fc in range(16):
                            S.do("pe", "matmul", [bwd, b_hid[b]], [bp], ps[0:sn, :], hid[:, b, fc, s0:s0 + sn], wdt[:, fc, :],
                                 start=(fc == 0), stop=(fc == 15))
                        S.do("act" if si % 2 else "dve", "copy" if si % 2 else "tensor_copy", [bp], [bye], out=ye[0:sn, si, :], in_=ps[0:sn, :])
                    S.dma("sp", YEo[el, b, n, :, 0:2, :], ye[:, 0:2, :], [bye], [Buf()])
                    if has_ctx:
                        S.dma("sp", YEo[el, b, n, 0:CAPC, 2, :], ye[0:CAPC, 2, :], [bye], [Buf()])


MODC = 6 * D // 8


def phase_mod(K, cc9, mod_w, mod_b, modp, ident, nrows=9, ncols=MODC):
    S = K.S
    with K.phase("mod") as P:
        cc = P.sb([nrows, D], F32)
        scT = P.sb([128, 16, nrows], BF16)
        ps_t = P.ps([128, 16, nrows], F32)
        b_cc, b_ps, b_sc = Buf(), Buf(), Buf()
        S.dma("sp", cc[:], cc9[:, :], [], [b_cc])
        S.do("act", "activation", [b_cc], [b_cc], out=cc[:], in_=cc[:], func=AF.Silu)
        for c in range(16):
            S.do("pe", "transpose", [b_cc], [b_ps], ps_t[:, c, :], cc[:, c * 128:(c + 1) * 128], ident[0:nrows, 0:nrows])
        S.do("dve", "tensor_copy", [b_ps], [b_sc], out=scT[:], in_=ps_t[:])
        wr = Ring([P.sb([128, 16, 512], BF16) for _ in range(3)])
        pr = Ring([P.ps([nrows, 512], F32) for _ in range(2)])
        br = Ring([P.sb([nrows, 512], F32) for _ in range(2)])
        orr = Ring([P.sb([nrows, 512], F32) for _ in range(2)])
        for layer in range(2):
            for n in range(ncols // 512):
                w, bw = wr.next()
                S.dma("pool", w[:], wpiece(mod_w[layer], n * 512, 512), [], [bw])
                bt, bb = br.next()
                S.dma("sp", bt[:], mod_b[layer:layer + 1, n * 512:(n + 1) * 512].broadcast_to([nrows, 512]), [], [bb])
                ps, bp = pr.next()
                for c in range(16):
                    S.do("pe", "matmul", [bw, b_sc], [bp], ps[:], scT[:, c, :], w[:, c, :], start=(c == 0), stop=(c == 15))
                ot, bo = orr.next()
                S.do("dve", "tensor_tensor", [bp, bb], [bo], out=ot[:], in0=ps[:], in1=bt[:], op=ALU.add)
                S.dma("sp", modp[layer, :, n * 512:(n + 1) * 512], ot[:], [bo], [Buf()])


NB = 1
NCORES = 8 // NB


def build(dbg=(), stop_after=None):
    K = KB(dbg, stop_after)
    nc = K.nc
    innames = []

    def inp(name, shape, dt=F32):
        innames.append(name)
        return K.inp(name, shape, dt)

    ident_d = inp("ident", [128, 128])
    x = inp("x", [NB, T, D])
    ctx = inp("ctx", [NB, TCX, D])
    cc = inp("cc", [NB + 1, D])
    mod_w = inp("mod_w", [2, D, 6 * D])
    mod_b = inp("mod_b", [2, 6 * D])
    norm_g = inp("norm_g", [2, 2, D])
    final_g = inp("final_g", [D])
    ab_w_in = inp("ab_w_in", [1, D, 3072])
    ab_w_out = inp("ab_w_out", [1, D, D])
    gm_v_g = inp("gm_v_g", [1, 1024])
    gm_w_s = inp("gm_w_s", [1, 8, 128, 128])
    gm_b_s = inp("gm_b_s", [1, 8, 128])
    w_in = inp("ssd_w_in", [1, D, 10368])
    conv_w = inp("ssd_conv_w", [1, 5, 6144])
    conv_b = inp("ssd_conv_b", [1, 6144])
    dt_bias = inp("ssd_dt_bias", [1, 2, 64])
    a_log = inp("ssd_a_log", [1, 2, 64])
    d_skip = inp("ssd_d", [1, 64])
    ng = inp("ssd_norm_g", [1, 4096])
    w_out = inp("ssd_w_out", [1, 4096, D])
    w_router = inp("moe_w_router", [2, D, NE])
    w_gate = inp("moe_w_gate", [2, NE, D, D])
    w_up = inp("moe_w_up", [2, NE, D, D])
    w_down = inp("moe_w_down", [2, NE, D, D])
    pos = inp("pos", [T, D])
    cs128_d = inp("cs128", [128, 256], BF16)
    dftT_d = inp("dftT", [2, T, T], BF16)
    dftC_d = inp("dftC", [2, TCX, TCX], BF16)
    tris_d = inp("tris", [4, 128, 128])
    iota_d = inp("iota", [128, NSLOT])
    out = nc.dram_tensor("out", [NB, T, D], F32, kind="ExternalOutput").ap()
    modD = K.scratch("modD", [2, NB + 1, 6 * D])
    es = K.es
    with es:
        sems = {e: es.enter_context(nc.semaphore("s_" + e)) for e in ENGS}
        rings = {e: [es.enter_context(nc.semaphore(f"r_{e}{i}")) for i in range(NRING)] for e in ENGS}
        S = K.S = Sched(nc, sems, rings)
        ident = es.enter_context(nc.sbuf_tensor("ident_sb", [128, 128], F32))
        identb = es.enter_context(nc.sbuf_tensor("identb_sb", [128, 128], BF16))
        with K.phase("c0") as P:
            bi = Buf()
            S.dma("sp", ident[:], ident_d[:, :], [], [bi])
            S.do("dve", "tensor_copy", [bi], [Buf()], out=identb[:], in_=ident[:])
        phase_mod(K, cc, mod_w, mod_b, modD, ident, nrows=NB + 1, ncols=6 * D)
        Hs, XE0, XE1, SELT0, SELT1, SELTC0 = [], [], [], [], [], []
        for j in range(NB):
            K.sfx = f"_b{j}"
            K.rows = (j, NB)
            Hs.append(K.scratch("H", [NTOK, D]))
            XE0.append(K.scratch("XE0", [NE, D, NSLOT], BF16))
            SELT0.append(K.scratch("SELT0", [NLAT, 128, 2 * NE, 128], BF16))
            SELTC0.append(K.scratch("SELTC0", [2, CAPC, NE, 128], BF16))
            XE1.append(K.scratch("XE1", [NE, D, CAP], BF16))
            SELT1.append(K.scratch("SELT1", [NLAT, 128, 2 * NE, 128], BF16))
            catT = K.scratch("catT", [D, NTOK], BF16)
            P12 = K.scratch("P12", [8, NTOK, 256], BF16)
            phase_ab(K, 0, x[j], ctx[j], pos, Hs[j], modD, norm_g, ab_w_in[0], ab_w_out[0], gm_v_g[0], gm_w_s[0], gm_b_s[0],
                     cs128_d, dftT_d, dftC_d, catT, P12, ident, identb)
            phase_moe_pre(K, 0, True, Hs[j], modD, norm_g, w_router[0], tris_d, iota_d, ident, identb, XE0[j], SELT0[j], SELTC0[j])
        K.sfx = ""
        YE0 = K.scratch("YE0", [NE, NB, 4, 128, 3, 512], BF16)
        phase_experts(K, lambda e, j: XE0[j][e], w_gate[0], w_up[0], w_down[0], YE0, NSLOT, True, n_exp=NE, NB=NB)
        for j in range(NB):
            K.sfx = f"_b{j}"
            K.rows = (j, NB)
            phase_moe_post(K, 0, True, Hs[j], Hs[j], modD, SELT0[j], SELTC0[j], YE0, j)
            scr = (K.scratch("ZS", [T, 4096], BF16), K.scratch("BCT", [2048, NTOK], BF16), K.scratch("XS", [NTOK, 4096], BF16),
                   K.scratch("BTOK", [NTOK, 1024], BF16), K.scratch("DTS", [NTOK, 128]), K.scratch("YB", [T, 4096]),
                   K.scratch("YT", [4096, T], BF16))
            phase_ssd(K, 1, Hs[j], modD, norm_g, w_in[0], conv_w[0], conv_b[0], dt_bias[0], a_log[0], d_skip[0], ng[0], w_out[0],
                      tris_d, ident, identb, scr)
            phase_moe_pre(K, 1, False, Hs[j], modD, norm_g, w_router[1], tris_d, iota_d, ident, identb, XE1[j], SELT1[j], None)
        K.sfx = ""
        YE1 = K.scratch("YE1", [NE, NB, 4, 128, 2, 512], BF16)
        phase_experts(K, lambda e, j: XE1[j][e], w_gate[1], w_up[1], w_down[1], YE1, CAP, False, n_exp=NE, NB=NB)
        for j in range(NB):
            K.rows = (j, NB)
            phase_moe_post(K, 1, False, Hs[j], Hs[j], modD, SELT1[j], None, YE1, j)
            phase_final(K, Hs[j], final_g, out[j])
    nc._inames = innames
    return nc


_CONST = {}


def host_constants():
    if _CONST:
        return _CONST
    bf = ml_dtypes.bfloat16
    rows, cols, dim = T // 64, 64, D
    quarter = dim // 4
    omega = (1.0 / (np.float32(10000.0) ** (np.arange(quarter, dtype=np.float32) / np.float32(quarter)))).astype(np.float32)
    r = np.repeat(np.arange(rows, dtype=np.float32), cols)[:, None] * omega
    cl = np.tile(np.arange(cols, dtype=np.float32), rows)[:, None] * omega
    _CONST["pos"] = np.concatenate([np.sin(r), np.cos(r), np.sin(cl), np.cos(cl)], axis=-1).astype(np.float32)
    _CONST["ident"] = np.eye(128, dtype=np.float32)

    def dft(n):
        k = np.arange(n, dtype=np.int64)
        ang = 2.0 * np.pi * ((k[:, None] * k[None, :]) % n).astype(np.float64) / n
        return np.cos(ang), np.sin(ang)

    c128, s128 = dft(128)
    _CONST["cs128"] = np.concatenate([c128, s128], axis=1).astype(np.float32).astype(bf)
    cT, sT = dft(T)
    _CONST["dftT"] = np.stack([cT, -sT]).astype(np.float32).astype(bf)
    cC, sC = dft(TCX)
    _CONST["dftC"] = np.stack([cC, -sC]).astype(np.float32).astype(bf)
    k = np.arange(128)
    kk, mm = k[:, None], k[None, :]
    _CONST["tris"] = np.stack([kk < mm, kk <= mm, kk > mm, kk >= mm]).astype(np.float32)
    _CONST["iota"] = np.tile(np.arange(NSLOT, dtype=np.float32)[None, :], (128, 1))
    return _CONST


_NC_CACHE = {}
_SHARED = ("mod_w", "mod_b", "norm_g", "final_g", "ab_w_in", "ab_w_out", "gm_v_g", "gm_w_s", "gm_b_s", "ssd_w_in", "ssd_conv_w",
           "ssd_conv_b", "ssd_dt_bias", "ssd_a_log", "ssd_d", "ssd_norm_g", "ssd_w_out", "moe_w_router", "moe_w_gate", "moe_w_up",
           "moe_w_down")


def make_in_maps(inputs, cores):
    cst = host_constants()
    shared = {k: np.ascontiguousarray(inputs[k]) for k in _SHARED}
    maps = []
    for k in cores:
        m = dict(shared)
        m.update(cst)
        m["x"] = np.ascontiguousarray(inputs["x"][k * NB:(k + 1) * NB])
        m["ctx"] = np.ascontiguousarray(inputs["ctx"][k * NB:(k + 1) * NB])
        m["cc"] = np.ascontiguousarray(np.concatenate([inputs["c"][k * NB:(k + 1) * NB], inputs["c_ctx"][None, :]], axis=0))
        maps.append(m)
    return maps


def kernel(**inputs):
    inputs = {k: np.asarray(v) for k, v in inputs.items()}
    if "nc" not in _NC_CACHE:
        _NC_CACHE["nc"] = build()
    nc = _NC_CACHE["nc"]
    maps = make_in_maps(inputs, range(NCORES))
    res = run_bass_kernel_spmd(nc, maps, core_ids=list(range(NCORES)))
    return np.concatenate([np.asarray(r["out"]) for r in res.results], axis=0).astype(np.float32)
```

```python
import os
import numpy as np
import ml_dtypes
from contextlib import ExitStack
import concourse.bass as bass
import concourse.mybir as mybir
from concourse.bass_utils import run_bass_kernel_spmd

F32 = mybir.dt.float32
BF16 = mybir.dt.bfloat16
AF = mybir.ActivationFunctionType
ALU = mybir.AluOpType
AX = mybir.AxisListType

ENGS = ("pe", "act", "dve", "pool", "sp")
NRING = 8

T = 2048
TCX = 256
NTOK = T + TCX
D = 2048
NTC = NTOK // 128
NLAT = T // 128
EPS = 1e-6
NE = 16
CAP = 256
CAPC = 32
NSLOT = CAP + CAPC


class Buf:
    __slots__ = ("lw", "rs")

    def __init__(self):
        self.lw = None
        self.rs = []


class Op:
    __slots__ = ("eng", "fn", "deps", "signal", "count", "dma", "ring", "rval", "phase")


class Sched:
    def __init__(self, nc, sems, rings):
        self.nc = nc
        self.sems = sems
        self.rings = rings
        self.ops = {e: [] for e in ENGS}
        self.ndma = {e: 0 for e in ENGS}
        self.cnt = {e: 0 for e in ENGS}
        self.waited = {e: {} for e in ENGS}
        self.phase = 0
        self.lastring = {}
        self.bufs = {}

    def B(self, *key):
        b = self.bufs.get(key)
        if b is None:
            b = self.bufs[key] = Buf()
        return b

    def op(self, eng, fn, reads=(), writes=(), dma=False):
        o = Op()
        o.eng, o.fn, o.dma, o.signal, o.count, o.phase = eng, fn, dma, False, 0, self.phase
        o.ring = o.rval = 0
        o.deps = []
        seen = set()
        cand = []
        for b in reads:
            if b.lw is not None:
                cand.append(b.lw)
        for b in writes:
            if b.lw is not None:
                cand.append(b.lw)
            cand.extend(b.rs)
        for d in cand:
            if id(d) in seen or d.phase != self.phase:
                continue
            seen.add(id(d))
            if d.eng == eng and eng == "pe" and not d.dma and not dma:
                continue
            o.deps.append(d)
            d.signal = True
        for b in reads:
            b.rs.append(o)
        for b in writes:
            b.lw = o
            b.rs = []
        if dma:
            i = self.ndma[eng]
            self.ndma[eng] += 1
            o.ring = i % NRING
            o.rval = 16 * (i // NRING + 1)
            self.lastring[(eng, o.ring)] = o.rval
        self.ops[eng].append(o)
        return o

    def do(self, eng, meth, reads, writes, *a, **kw):
        return self.op(eng, lambda e: getattr(e, meth)(*a, **kw), reads, writes)

    def dma(self, eng, out, in_, reads, writes):
        return self.op(eng, lambda e: e.dma_start(out=out, in_=in_), reads, writes, dma=True)

    def _wait(self, e, eh, need):
        w = self.waited[e]
        for key, (sem, val) in need.items():
            if w.get(key, 0) >= val:
                continue
            eh.wait_ge(sem, val)
            w[key] = val

    def emit_engine(self, e, eh, final):
        w = self.waited[e]
        for o in self.ops[e]:
            need = {}
            for d in o.deps:
                if d.dma:
                    key, sem, val = ("r", d.eng, d.ring), self.rings[d.eng][d.ring], d.rval
                else:
                    key, sem, val = ("c", d.eng), self.sems[d.eng], d.count
                if key not in need or need[key][1] < val:
                    need[key] = (sem, val)
            if o.dma and o.rval > 16:
                key = ("r", e, o.ring)
                if key not in need or need[key][1] < o.rval - 16:
                    need[key] = (self.rings[e][o.ring], o.rval - 16)
            self._wait(e, eh, need)
            ins = o.fn(eh)
            if o.dma:
                ins.then_inc(self.rings[e][o.ring], 16)
            elif o.signal:
                ins.then_inc(self.sems[e], 1)
        need = {}
        for p in ENGS:
            if final[p] > 0:
                need[("c", p)] = (self.sems[p], final[p])
        for (p, r), val in self.lastring.items():
            need[("r", p, r)] = (self.rings[p][r], val)
        self._wait(e, eh, need)

    def end_phase(self, block):
        final = {}
        for e in ENGS:
            comp = [o for o in self.ops[e] if not o.dma]
            if comp:
                comp[-1].signal = True
            c = self.cnt[e]
            for o in comp:
                if o.signal:
                    c += 1
                    o.count = c
            self.cnt[e] = c
            final[e] = c
        S = self

        @block.tensor
        def _(eh):
            S.emit_engine("pe", eh, final)

        @block.scalar
        def _(eh):
            S.emit_engine("act", eh, final)

        @block.vector
        def _(eh):
            S.emit_engine("dve", eh, final)

        @block.gpsimd
        def _(eh):
            S.emit_engine("pool", eh, final)

        @block.sync
        def _(eh):
            S.emit_engine("sp", eh, final)

        self.ops = {e: [] for e in ENGS}
        self.phase += 1


class Phase:
    def __init__(self, K, name):
        self.K = K
        K.uid += 1
        self.name = f"{name}u{K.uid}"
        self.es = ExitStack()
        self.n = 0

    def __enter__(self):
        self.es.__enter__()
        return self

    def sb(self, shape, dt, name=None):
        self.n += 1
        return self.es.enter_context(self.K.nc.sbuf_tensor(f"{self.name}_{name or 't'}{self.n}", list(shape), dt))

    def ps(self, shape, dt=F32, name=None):
        self.n += 1
        return self.es.enter_context(self.K.nc.psum_tensor(f"{self.name}_{name or 'p'}{self.n}", list(shape), dt))

    def __exit__(self, *a):
        with self.K.nc.Block() as block:
            self.K.S.end_phase(block)
        return self.es.__exit__(*a)


class Ring:
    def __init__(self, tiles):
        self.tiles = tiles
        self.bufs = [Buf() for _ in tiles]
        self.i = -1

    def next(self):
        self.i = (self.i + 1) % len(self.tiles)
        return self.tiles[self.i], self.bufs[self.i]


class KB:
    def __init__(self, dbg=(), stop_after=None):
        self.dbg = set(dbg)
        self.stop_after = stop_after
        self.nc = bass.Bass("TRN2", target_bir_lowering=False)
        self.es = ExitStack()
        self.dram = {}
        self.uid = 0
        self.rows = (0, 1)
        self.sfx = ""

    def inp(self, name, shape, dt=F32):
        self.dram[name] = self.nc.dram_tensor(name, list(shape), dt, kind="ExternalInput").ap()
        return self.dram[name]

    def scratch(self, name, shape, dt=F32):
        kind = "ExternalOutput" if name in self.dbg else "Internal"
        name = name + self.sfx
        self.dram[name] = self.nc.dram_tensor(name, list(shape), dt, kind=kind).ap()
        return self.dram[name]

    def sbuf(self, es, name, shape, dt):
        self.uid += 1
        return es.enter_context(self.nc.sbuf_tensor(f"{name}u{self.uid}", list(shape), dt))

    def phase(self, name):
        return Phase(self, name)


def wpiece(w2d, n0, ncols, kc=16):
    return w2d.rearrange("(c p) n -> p c n", p=128)[:, :, n0:n0 + ncols]


def load_mod_rows(K, P, modD, layer, which, g_row_ap, nr=2):
    S = K.S
    sh_off = 0 if which == 1 else 3 * D
    sc_off = sh_off + D
    A, Bt, bA, bB = [], [], [], []
    gt = P.sb([128, D], F32)
    bg = Buf()
    S.dma("sp", gt[:], g_row_ap.broadcast_to([128, D]), [], [bg])
    for r in range(nr):
        a = P.sb([128, D], F32)
        b = P.sb([128, D], F32)
        ba, bb = Buf(), Buf()
        S.dma("sp", a[:], modD[layer, K.rows[r]:K.rows[r] + 1, sc_off:sc_off + D].broadcast_to([128, D]), [], [ba])
        S.dma("sp", b[:], modD[layer, K.rows[r]:K.rows[r] + 1, sh_off:sh_off + D].broadcast_to([128, D]), [], [bb])
        S.do("dve", "scalar_tensor_tensor", [ba, bg], [ba], out=a[:], in0=a[:], scalar=1.0, in1=gt[:], op0=ALU.add, op1=ALU.mult)
        A.append(a), Bt.append(b), bA.append(ba), bB.append(bb)
    return A, Bt, bA, bB


def load_gate_rows(K, P, modD, layer, which, nr=2):
    S = K.S
    off = 2 * D if which == 1 else 5 * D
    G, bG = [], []
    for r in range(nr):
        g = P.sb([128, D], F32)
        bg = Buf()
        S.dma("sp", g[:], modD[layer, K.rows[r]:K.rows[r] + 1, off:off + D].broadcast_to([128, D]), [], [bg])
        G.append(g), bG.append(bg)
    return G, bG


def rms_rstd(S, P, hbuf, bh, junk, bj, ssq, rstd, bs, tc, dim):
    S.do("act", "activation", [bh], [bj, bs], out=junk, in_=hbuf, func=AF.Square, accum_out=ssq[:, tc:tc + 1])
    S.do("act", "activation", [bs], [bs], out=rstd[:, tc:tc + 1], in_=ssq[:, tc:tc + 1], func=AF.Sqrt, scale=1.0 / dim, bias=EPS)
    S.do("dve", "reciprocal", [bs], [bs], out=rstd[:, tc:tc + 1], in_=rstd[:, tc:tc + 1])


def phase_ab(K, layer, x, ctx, pos, H, modD, norm_g, w_in, w_out, v_g, w_s, b_s, cs128_d, dftT_d, dftC_d, catT, P12,
             ident, identb):
    S = K.S
    nc = K.nc
    ntc = NTC
    with ExitStack() as outer:
      xnT = K.sbuf(outer, "xnT_ab", [128, 16, NTOK], BF16)
      b_xnT = [Buf() for _ in range(ntc)]
      with K.phase("ab1a") as P:
        A, Bt, bA, bB = load_mod_rows(K, P, modD, layer, 1, norm_g[layer, 0:1, :])
        ssq = P.sb([128, ntc], F32)
        rstd = P.sb([128, ntc], F32)
        hr = Ring([P.sb([128, D], F32) for _ in range(2)])
        pr_ = Ring([P.sb([128, D], F32) for _ in range(2)])
        junk = P.sb([128, D], BF16)
        bj = Buf()
        t1 = P.sb([128, D], F32)
        bt1 = Buf()
        xr = Ring([P.sb([128, D], BF16) for _ in range(2)])
        ptr = Ring([P.ps([128, 8, 128], BF16) for _ in range(2)])
        for tc in range(ntc):
            r = 0 if tc < NLAT else 1
            h, bh = hr.next()
            if tc < NLAT:
                pt, bp = pr_.next()
                S.dma("sp", h[:], x[tc * 128:(tc + 1) * 128, :], [], [bh])
                S.dma("sp", pt[:], pos[tc * 128:(tc + 1) * 128, :], [], [bp])
                S.do("pool", "tensor_tensor", [bh, bp], [bh], out=h[:], in0=h[:], in1=pt[:], op=ALU.add)
            else:
                S.dma("sp", h[:], ctx[(tc - NLAT) * 128:(tc - NLAT + 1) * 128, :], [], [bh])
            S.dma("sp", H[tc * 128:(tc + 1) * 128, :], h[:], [bh], [S.B("H", tc)])
            bs = Buf()
            rms_rstd(S, P, h[:], bh, junk[:], bj, ssq, rstd, bs, tc, D)
            S.do("dve", "scalar_tensor_tensor", [bh, bs, bA[r]], [bt1], out=t1[:], in0=h[:], scalar=rstd[:, tc:tc + 1], in1=A[r][:],
                 op0=ALU.mult, op1=ALU.mult)
            xt, bx = xr.next()
            S.do("dve", "tensor_tensor", [bt1, bB[r]], [bx], out=xt[:], in0=t1[:], in1=Bt[r][:], op=ALU.add)
            for half in range(2):
                ps, bp = ptr.next()
                for j in range(8):
                    c = half * 8 + j
                    S.do("pe", "transpose", [bx], [bp], ps[:, j, :], xt[:, c * 128:(c + 1) * 128], identb[:])
                S.do("act", "copy", [bp], [b_xnT[tc]], out=xnT[:, half * 8:(half + 1) * 8, tc * 128:(tc + 1) * 128], in_=ps[:])
        if "xnT" in K.dbg:
            dd = K.scratch("xnT", [D, NTOK], BF16)
            S.dma("sp", dd.rearrange("(c p) t -> p c t", p=128), xnT[:], b_xnT, [Buf()])
      if K.stop_after == "xn":
        return
      with K.phase("ab1b") as P:
        junk = P.sb([128, 512], BF16)
        bj = Buf()
        wr = Ring([P.sb([128, 16, 512], BF16) for _ in range(2)])
        pmm = Ring([P.ps([128, 512], F32) for _ in range(2)])
        vtok = P.sb([128, ntc, 1024], BF16)
        b_v = [Buf() for _ in range(ntc)]
        vss = P.sb([128, ntc, 2], F32)
        vr = P.sb([128, ntc], F32)
        bvs = Buf()
        for pc in range(2):
            w, bw = wr.next()
            S.dma("pool", w[:], wpiece(w_in, 1024 + pc * 512, 512), [], [bw])
            for tc in range(ntc):
                ps, bp = pmm.next()
                for c in range(16):
                    S.do("pe", "matmul", [bw, b_xnT[tc]], [bp], ps[:], xnT[:, c, tc * 128:(tc + 1) * 128], w[:, c, :],
                         start=(c == 0), stop=(c == 15))
                S.do("act", "activation", [bp], [b_v[tc]], out=vtok[:, tc, pc * 512:(pc + 1) * 512], in_=ps[:], func=AF.Gelu)
                S.do("act", "activation", [b_v[tc]], [bj, bvs], out=junk[:, 0:512], in_=vtok[:, tc, pc * 512:(pc + 1) * 512],
                     func=AF.Square, accum_out=vss[:, tc, pc:pc + 1])
        S.do("dve", "tensor_tensor", [bvs], [bvs], out=vr[:], in0=vss[:, :, 0], in1=vss[:, :, 1], op=ALU.add)
        S.do("act", "activation", [bvs], [bvs], out=vr[:], in_=vr[:], func=AF.Sqrt, scale=1.0 / 1024, bias=EPS)
        S.do("dve", "reciprocal", [bvs], [bvs], out=vr[:], in_=vr[:])
        for tc in range(ntc):
            S.do("dve", "tensor_scalar", [bvs, b_v[tc]], [b_v[tc]], out=vtok[:, tc, :], in0=vtok[:, tc, :], scalar1=vr[:, tc:tc + 1],
                 scalar2=None, op0=ALU.mult)
        wsf = P.sb([128, 8, 128], F32)
        wsT = P.sb([128, 8, 128], BF16)
        vg8 = P.sb([8, 128], F32)
        vgT = P.sb([128, 8], F32)
        bsb = P.sb([128, 8, 128], F32)
        b_ws, b_wsT, b_vg, b_vgT, b_bsb = Buf(), Buf(), Buf(), Buf(), Buf()
        S.dma("sp", wsf[:], w_s.rearrange("g i j -> i g j"), [], [b_ws])
        S.dma("sp", vg8[:], v_g.rearrange("(g p) -> g p", p=128), [], [b_vg])
        S.dma("sp", bsb[:].rearrange("p g i -> p (g i)"), b_s.rearrange("g i -> (g i)").unsqueeze(0).broadcast_to([128, 1024]), [], [b_bsb])
        pss = Ring([P.ps([128, 512], F32) for _ in range(2)])
        pst = pss.tiles[0][:].rearrange("p (j i) -> p j i", j=4)
        b_pst = pss.bufs[0]
        for hf in range(2):
            for j in range(4):
                g = hf * 4 + j
                S.do("pe", "transpose", [b_ws], [b_pst], pst[:, j, :], wsf[:, g, :], ident[:])
            S.do("dve", "tensor_copy", [b_pst], [b_wsT], out=wsT[:, hf * 4:(hf + 1) * 4, :], in_=pst[:])
        S.do("pe", "transpose", [b_vg], [b_pst], pst[:, 0, 0:8], vg8[:], ident[0:8, 0:8])
        S.do("dve", "tensor_copy", [b_pst], [b_vgT], out=vgT[:], in_=pst[:, 0, 0:8])
        ur = Ring([P.sb([128, 512], F32) for _ in range(2)])
        sr = Ring([P.sb([128, 512], F32) for _ in range(2)])
        yr = Ring([P.sb([128, 512], BF16) for _ in range(2)])
        ttiles = [(i * 512, 512) for i in range(4)] + [(T, TCX)]
        for pc in range(2):
            w, bw = wr.next()
            S.dma("pool", w[:], wpiece(w_in, pc * 512, 512), [], [bw])
            for oc in range(4):
                g = pc * 4 + oc
                for (t0, tn) in ttiles:
                    tcs = list(range(t0 // 128, (t0 + tn) // 128))
                    ps, bp = pmm.next()
                    for c in range(16):
                        S.do("pe", "matmul", [bw] + [b_xnT[q] for q in tcs], [bp], ps[:, 0:tn], w[:, c, oc * 128:(oc + 1) * 128],
                             xnT[:, c, t0:t0 + tn], start=(c == 0), stop=(c == 15))
                    ut, bu = ur.next()
                    S.do("act", "activation", [bp], [bu], out=ut[:, 0:tn], in_=ps[:, 0:tn], func=AF.Gelu)
                    ps2, bp2 = pss.next()
                    for k, q in enumerate(tcs):
                        S.do("pe", "matmul", [b_v[q], b_wsT], [bp2], ps2[:, k * 128:(k + 1) * 128], vtok[:, q, g * 128:(g + 1) * 128],
                             wsT[:, g, :], start=True, stop=True)
                    st, bs_ = sr.next()
                    nk = len(tcs)
                    S.do("dve", "scalar_tensor_tensor", [bp2, b_vgT, b_bsb], [bs_], out=st[:, 0:tn].rearrange("p (k i) -> p k i", k=nk),
                         in0=ps2[:, 0:tn].rearrange("p (k i) -> p k i", k=nk), scalar=vgT[:, g:g + 1],
                         in1=bsb[:, g:g + 1, :].broadcast_to([128, nk, 128]), op0=ALU.mult, op1=ALU.add)
                    yt, by = yr.next()
                    S.do("dve", "tensor_tensor", [bs_, bu], [by], out=yt[:, 0:tn], in0=st[:, 0:tn], in1=ut[:, 0:tn], op=ALU.mult)
                    S.dma("sp", catT[g * 128:(g + 1) * 128, t0:t0 + tn], yt[:, 0:tn], [by], [S.B("catT", g, t0)])
        cs = P.sb([128, 256], BF16)
        b_cs = Buf()
        S.dma("sp", cs[:], cs128_d[:, :], [], [b_cs])
        zr = Ring([P.sb([128, 512], BF16) for _ in range(2)])
        p12r = Ring([P.sb([128, 4, 256], BF16) for _ in range(2)])
        pz = Ring([P.ps([128, 2, 256], F32) for _ in range(2)])
        for pc in range(2):
            w, bw = wr.next()
            S.dma("pool", w[:], wpiece(w_in, 2048 + pc * 512, 512), [], [bw])
            for oc in range(4):
                g = pc * 4 + oc
                for (t0, tn) in ttiles:
                    tcs = list(range(t0 // 128, (t0 + tn) // 128))
                    ps, bp = pmm.next()
                    for c in range(16):
                        S.do("pe", "matmul", [bw] + [b_xnT[q] for q in tcs], [bp], ps[:, 0:tn], w[:, c, oc * 128:(oc + 1) * 128],
                             xnT[:, c, t0:t0 + tn], start=(c == 0), stop=(c == 15))
                    zt, bz = zr.next()
                    S.do("act", "copy", [bp], [bz], out=zt[:, 0:tn], in_=ps[:, 0:tn])
                    pt, bpt = p12r.next()
                    for k2 in range(0, len(tcs), 2):
                        psz, bpz = pz.next()
                        for k in range(k2, k2 + 2):
                            S.do("pe", "matmul", [bz, b_cs], [bpz], psz[:, k - k2, :], zt[:, k * 128:(k + 1) * 128], cs[:],
                                 start=True, stop=True)
                        S.do("dve", "tensor_copy", [bpz], [bpt], out=pt[:, k2:k2 + 2, :], in_=psz[:])
                    nk = len(tcs)
                    S.dma("sp", P12[g, t0:t0 + tn, :].rearrange("(k p) n -> p k n", p=128), pt[:, 0:nk, :], [bpt], [S.B("P12", g, t0)])
    if K.stop_after == "ab1":
        return
    scale = 1.0 / float(np.sqrt(T * 128.0))
    scale_c = 1.0 / float(np.sqrt(TCX * 128.0))
    with K.phase("ab3") as P:
        p12 = P.sb([128, 8, NLAT, 256], BF16)
        b_p12 = Buf()
        for g in range(8):
            S.dma("sp", p12[:, g, :, :], P12[g, 0:T, :].rearrange("(k p) n -> p k n", p=128), [], [b_p12])
        p12c = P.sb([128, 8, 2, 256], BF16)
        b_p12c = Buf()
        for g in range(8):
            S.dma("sp", p12c[:, g, :, :], P12[g, T:NTOK, :].rearrange("(k p) n -> p k n", p=128), [], [b_p12c])
        dr = Ring([P.sb([128, 2, NLAT, 512], BF16) for _ in range(2)])
        dc = P.sb([128, 2, 2, 256], BF16)
        b_dc = Buf()
        for m in range(2):
            S.dma("sp", dc[:, m, :, :], dftC_d[m].rearrange("(k p) t -> p k t", p=128), [], [b_dc])
        pf = Ring([P.ps([128, 512], F32) for _ in range(3)])
        yr = Ring([P.sb([128, 512], BF16) for _ in range(3)])
        for tt in range(4):
            dt_, bd = dr.next()
            for m in range(2):
                S.dma("sp", dt_[:, m, :, :], dftT_d[m].rearrange("(k p) t -> p k t", p=128)[:, :, tt * 512:(tt + 1) * 512], [], [bd])
            for g in range(8):
                ps, bp = pf.next()
                n = 0
                for m in range(2):
                    for k in range(NLAT):
                        S.do("pe", "matmul", [bd, b_p12], [bp], ps[:], p12[:, g, k, m * 128:(m + 1) * 128], dt_[:, m, k, :],
                             start=(n == 0), stop=(n == 2 * NLAT - 1))
                        n += 1
                yt, by = yr.next()
                S.do("act", "mul", [bp], [by], out=yt[:], in_=ps[:], mul=scale)
                S.dma("sp", catT[1024 + g * 128:1024 + (g + 1) * 128, tt * 512:(tt + 1) * 512], yt[:], [by], [Buf()])
        for g in range(8):
            ps, bp = pf.next()
            n = 0
            for m in range(2):
                for k in range(2):
                    S.do("pe", "matmul", [b_dc, b_p12c], [bp], ps[:, 0:TCX], p12c[:, g, k, m * 128:(m + 1) * 128], dc[:, m, k, :],
                         start=(n == 0), stop=(n == 3))
                    n += 1
            yt, by = yr.next()
            S.do("act", "mul", [bp], [by], out=yt[:, 0:TCX], in_=ps[:, 0:TCX], mul=scale_c)
            S.dma("sp", catT[1024 + g * 128:1024 + (g + 1) * 128, T:NTOK], yt[:, 0:TCX], [by], [Buf()])
    if K.stop_after == "ab3":
        return
    phase_outproj(K, layer, catT, 16, w_out, H, modD, NTC)


def phase_outproj(K, layer, yT_d, kc, w_out, H, modD, ntc):
    S = K.S
    with K.phase("oproj") as P:
        nr = 2 if ntc > NLAT else 1
        G, bG = load_gate_rows(K, P, modD, layer, 1, nr)
        ntok = ntc * 128
        half = ntok // 2 if kc > 16 else ntok
        wr = Ring([P.sb([128, 16, 512], BF16) for _ in range(4 if kc > 16 else 2)])
        pm = Ring([P.ps([128, 512], F32) for _ in range(3)])
        hr = Ring([P.sb([128, 512], F32) for _ in range(3)])
        tr = Ring([P.sb([128, 512], F32) for _ in range(3)])
        yT = P.sb([128, kc, half], BF16)
        b_y = Buf()
        for t0 in range(0, ntok, half):
            for c0 in range(0, kc, 8):
                S.dma("sp", yT[:, c0:c0 + 8, :], yT_d.rearrange("(c p) t -> p c t", p=128)[:, c0:c0 + 8, t0:t0 + half], [], [b_y])
            for n in range(4):
                ws = []
                for kh in range(kc // 16):
                    w, bw = wr.next()
                    S.dma("pool", w[:], w_out.rearrange("(c p) n -> p c n", p=128)[:, kh * 16:(kh + 1) * 16, n * 512:(n + 1) * 512], [], [bw])
                    ws.append((w, bw))
                for tcl in range(half // 128):
                    tc = t0 // 128 + tcl
                    r = 0 if tc < NLAT else 1
                    ps, bp = pm.next()
                    for c in range(kc):
                        w, bw = ws[c // 16]
                        S.do("pe", "matmul", [bw, b_y], [bp], ps[:], yT[:, c, tcl * 128:(tcl + 1) * 128], w[:, c % 16, :],
                             start=(c == 0), stop=(c == kc - 1))
                    ht, bh = hr.next()
                    hb = S.B("H", tc, n)
                    S.dma("sp", ht[:], H[tc * 128:(tc + 1) * 128, n * 512:(n + 1) * 512], [hb], [bh])
                    tt, bt = tr.next()
                    S.do("dve", "tensor_tensor", [bp, bG[r]], [bt], out=tt[:], in0=ps[:], in1=G[r][:, n * 512:(n + 1) * 512], op=ALU.mult)
                    S.do("pool", "tensor_tensor", [bt, bh], [bh], out=ht[:], in0=tt[:], in1=ht[:], op=ALU.add)
                    S.dma("sp", H[tc * 128:(tc + 1) * 128, n * 512:(n + 1) * 512], ht[:], [bh], [hb])


def phase_moe_pre(K, layer, has_ctx, H, modD, norm_g, w_router, tris_d, iota_d, ident, identb, XE, SELT, SELTC):
    S = K.S
    nc = K.nc
    ntc = NTC if has_ctx else NLAT
    ntok = ntc * 128
    nslot = NSLOT if has_ctx else CAP
    with ExitStack() as outer:
        xm = K.sbuf(outer, "xm", [128, ntc, D], BF16)
        b_xm = [Buf() for _ in range(ntc)]
        R = K.sbuf(outer, "R", [128, ntc, NE], F32)
        Mk = K.sbuf(outer, "Mk", [128, ntc, NE], F32)
        GM = K.sbuf(outer, "GM", [128, ntc, NE], F32)
        with K.phase(f"e1a{layer}") as P:
            A, Bt, bA, bB = load_mod_rows(K, P, modD, layer, 2, norm_g[layer, 1:2, :], 2 if has_ctx else 1)
            wr32 = P.sb([128, 16, NE], F32)
            b_wr = Buf()
            S.dma("sp", wr32[:], w_router.rearrange("(c p) e -> p c e", p=128), [], [b_wr])
            ssq = P.sb([128, ntc], F32)
            rstd = P.sb([128, ntc], F32)
            hr = Ring([P.sb([128, D], F32) for _ in range(2)])
            t1r = Ring([P.sb([128, D], F32) for _ in range(2)])
            junk = P.sb([128, D], BF16)
            bj = Buf()
            xtr = Ring([P.sb([128, 16, 128], F32) for _ in range(2)])
            ptr = Ring([P.ps([128, 4, 128], F32) for _ in range(4)])
            plg = Ring([P.ps([128, NE], F32) for _ in range(2)])
            Pt = P.sb([128, ntc, NE], F32)
            b_Pt = [Buf() for _ in range(ntc)]
            mx = P.sb([128, ntc], F32)
            sm = P.sb([128, ntc], F32)
            for tc in range(ntc):
                r = 0 if tc < NLAT else 1
                h, bh = hr.next()
                S.dma("sp", h[:], H[tc * 128:(tc + 1) * 128, :], [], [bh])
                bs = Buf()
                rms_rstd(S, P, h[:], bh, junk[:], bj, ssq, rstd, bs, tc, D)
                t1, bt1 = t1r.next()
                S.do("dve", "scalar_tensor_tensor", [bh, bs, bA[r]], [bt1], out=t1[:], in0=h[:], scalar=rstd[:, tc:tc + 1], in1=A[r][:],
                     op0=ALU.mult, op1=ALU.mult)
                S.do("dve", "tensor_tensor", [bt1, bB[r]], [bt1], out=t1[:], in0=t1[:], in1=Bt[r][:], op=ALU.add)
                S.do("act", "copy", [bt1], [b_xm[tc]], out=xm[:, tc, :], in_=t1[:])
                xt, bx = xtr.next()
                for q in range(4):
                    ps, bp = ptr.next()
                    for j in range(4):
                        c = q * 4 + j
                        S.do("pe", "transpose", [bt1], [bp], ps[:, j, :], t1[:, c * 128:(c + 1) * 128], ident[:])
                    S.do("act" if q % 2 else "dve", "copy" if q % 2 else "tensor_copy", [bp], [bx], out=xt[:, q * 4:(q + 1) * 4, :], in_=ps[:])
                pl, bl = plg.next()
                for c in range(16):
                    S.do("pe", "matmul", [bx, b_wr], [bl], pl[:], xt[:, c, :], wr32[:, c, :], start=(c == 0), stop=(c == 15))
                bm = Buf()
                S.do("dve", "tensor_reduce", [bl], [bm], out=mx[:, tc:tc + 1], in_=pl[:], axis=AX.X, op=ALU.max)
                S.do("dve", "tensor_scalar", [bm], [bm], out=mx[:, tc:tc + 1], in0=mx[:, tc:tc + 1], scalar1=-1.0, scalar2=None, op0=ALU.mult)
                S.do("act", "activation", [bl, bm], [b_Pt[tc], bm], out=Pt[:, tc, :], in_=pl[:], func=AF.Exp, bias=mx[:, tc:tc + 1], scale=1.0,
                     accum_out=sm[:, tc:tc + 1])
                S.do("dve", "reciprocal", [bm], [bm], out=sm[:, tc:tc + 1], in_=sm[:, tc:tc + 1])
                S.do("dve", "tensor_scalar", [bm, b_Pt[tc]], [b_Pt[tc]], out=Pt[:, tc, :], in0=Pt[:, tc, :], scalar1=sm[:, tc:tc + 1], scalar2=None,
                     op0=ALU.mult)
            PT = P.sb([NE, ntok], F32)
            b_PT = Buf()
            for q in range(0, ntc, 4):
                ps, bp = ptr.next()
                nq = min(4, ntc - q)
                for j in range(nq):
                    S.do("pe", "transpose", [b_Pt[q + j]], [bp], ps[0:NE, j, :], Pt[:, q + j, :], ident[:])
                S.do("dve", "tensor_copy", [bp], [b_PT], out=PT[:, q * 128:(q + nq) * 128], in_=ps[0:NE, 0:nq, :])
            wa = P.sb([NE, T], F32)
            wb = P.sb([NE, T], F32)
            m8 = P.sb([NE, 8], F32)
            tau = P.sb([NE, 2], F32)
            b_wa, b_wb, b_m8, b_tau = Buf(), Buf(), Buf(), Buf()
            seqs = [(0, T, CAP, 0)] + ([(T, TCX, CAPC, 1)] if has_ctx else [])
            maskT = P.sb([NE, ntok], F32)
            b_mT = Buf()
            for (t0, tn, cap, si) in seqs:
                cur, bc, oth, bo = PT[:, t0:t0 + tn], b_PT, wa[:, 0:tn], b_wa
                for rd in range(cap // 8):
                    S.do("dve", "max", [bc], [b_m8], out=m8[:], in_=cur)
                    if rd < cap // 8 - 1:
                        S.do("dve", "match_replace", [bc, b_m8], [bo], out=oth, in_to_replace=m8[:], in_values=cur, imm_value=-1.0)
                        if rd == 0:
                            cur, bc, oth, bo = wa[:, 0:tn], b_wa, wb[:, 0:tn], b_wb
                        else:
                            cur, bc, oth, bo = oth, bo, cur, bc
                S.do("dve", "tensor_copy", [b_m8], [b_tau], out=tau[:, si:si + 1], in_=m8[:, 7:8])
                S.do("dve", "tensor_scalar", [b_PT, b_tau], [b_mT], out=maskT[:, t0:t0 + tn], in0=PT[:, t0:t0 + tn], scalar1=tau[:, si:si + 1],
                     scalar2=None, op0=ALU.is_ge)
            pk = P.ps([128, ntc, NE], F32)
            b_pk = Buf()
            for tc in range(ntc):
                S.do("pe", "transpose", [b_mT], [b_pk], pk[:, tc, :], maskT[:, tc * 128:(tc + 1) * 128], ident[0:NE, 0:NE])
            b_Mk, b_GM, b_R = Buf(), Buf(), Buf()
            S.do("dve", "tensor_copy", [b_pk], [b_Mk], out=Mk[:], in_=pk[:])
            S.do("dve", "tensor_tensor", [b_Mk] + b_Pt, [b_GM], out=GM[:], in0=Mk[:], in1=Pt[:], op=ALU.mult)
            Mb = P.sb([128, ntc, NE], BF16)
            b_Mb = Buf()
            S.do("dve", "tensor_copy", [b_Mk], [b_Mb], out=Mb[:], in_=Mk[:])
            trf = P.sb([128, 128], F32)
            trb = P.sb([128, 128], BF16)
            oneb = P.sb([128, 128], BF16)
            b_tr, b_one = Buf(), Buf()
            S.dma("sp", trf[:], tris_d[0], [], [b_tr])
            S.do("dve", "tensor_copy", [b_tr], [b_tr], out=trb[:], in_=trf[:])
            S.do("dve", "memset", [], [b_one], oneb[:], 1.0)
            pr2 = P.ps([128, ntc, NE], F32)
            b_pr2 = Buf()
            for tc in range(ntc):
                first = 0 if tc < NLAT else NLAT
                for t2 in range(first, tc + 1):
                    S.do("pe", "matmul", [b_Mb, b_tr, b_one], [b_pr2], pr2[:, tc, :], (trb if t2 == tc else oneb)[:], Mb[:, t2, :],
                         start=(t2 == first), stop=(t2 == tc))
            S.do("dve", "tensor_copy", [b_pr2], [b_R], out=R[:], in_=pr2[:])
            if has_ctx:
                S.do("dve", "tensor_scalar", [b_R], [b_R], out=R[:, NLAT:, :], in0=R[:, NLAT:, :], scalar1=float(CAP), scalar2=None, op0=ALU.add)
            if "Pt" in K.dbg:
                dd = K.scratch("Pt", [ntok, NE])
                S.dma("sp", dd.rearrange("(k p) e -> p k e", p=128), Pt[:], b_Pt, [Buf()])
                dd = K.scratch("Mk", [ntok, NE])
                S.dma("sp", dd.rearrange("(k p) e -> p k e", p=128), Mk[:], [b_Mk], [Buf()])
                dd = K.scratch("Rk", [ntok, NE])
                S.dma("sp", dd.rearrange("(k p) e -> p k e", p=128), R[:], [b_R], [Buf()])
        if K.stop_after == "e1a":
            return
        with K.phase(f"e1b{layer}") as P:
            iot = P.sb([128, NSLOT], F32)
            b_io = Buf()
            S.dma("sp", iot[:], iota_d[:, :], [], [b_io])
            selr = Ring([P.sb([128, NLAT, CAP], BF16) for _ in range(2)])
            sgr = Ring([P.sb([128, NLAT, CAP], BF16) for _ in range(2)])
            selc = Ring([P.sb([128, 2, CAPC], BF16) for _ in range(2)])
            sgc = Ring([P.sb([128, 2, CAPC], BF16) for _ in range(2)])
            xer = Ring([P.sb([128, 16, nslot], BF16) for _ in range(2)])
            str_ = Ring([P.sb([128, NLAT, 2, 128], BF16) for _ in range(2)])
            stc = Ring([P.sb([CAPC, 2, 128], BF16) for _ in range(2)])
            pg = Ring([P.ps([128, 512], F32) for _ in range(3)])
            pt2 = Ring([P.ps([128, 8, 128], BF16) for _ in range(3)])
            for e in range(NE):
                sel, bsel = selr.next()
                sg, bsg = sgr.next()
                for tc in range(NLAT):
                    S.do("dve", "tensor_scalar", [b_io], [bsel], out=sel[:, tc, :], in0=iot[:, 0:CAP], scalar1=R[:, tc, e:e + 1],
                         scalar2=Mk[:, tc, e:e + 1], op0=ALU.is_equal, op1=ALU.mult)
                    S.do("act", "mul", [bsel], [bsg], out=sg[:, tc, :], in_=sel[:, tc, :], mul=GM[:, tc, e:e + 1])
                if has_ctx:
                    sc_, bsc = selc.next()
                    sgc_, bsgc = sgc.next()
                    for k in range(2):
                        S.do("dve", "tensor_scalar", [b_io], [bsc], out=sc_[:, k, :], in0=iot[:, CAP:NSLOT], scalar1=R[:, NLAT + k, e:e + 1],
                             scalar2=Mk[:, NLAT + k, e:e + 1], op0=ALU.is_equal, op1=ALU.mult)
                        S.do("dve", "tensor_scalar", [b_io], [bsgc], out=sgc_[:, k, :], in0=iot[:, CAP:NSLOT], scalar1=R[:, NLAT + k, e:e + 1],
                             scalar2=GM[:, NLAT + k, e:e + 1], op0=ALU.is_equal, op1=ALU.mult)
                xe, bxe = xer.next()
                for c in range(16):
                    ps, bp = pg.next()
                    for tc in range(NLAT):
                        S.do("pe", "matmul", [bsel, b_xm[tc]], [bp], ps[:, 0:CAP], xm[:, tc, c * 128:(c + 1) * 128], sel[:, tc, :],
                             start=(tc == 0), stop=(tc == NLAT - 1))
                    if has_ctx:
                        for k in range(2):
                            S.do("pe", "matmul", [bsc, b_xm[NLAT + k]], [bp], ps[:, CAP:NSLOT], xm[:, NLAT + k, c * 128:(c + 1) * 128], sc_[:, k, :],
                                 start=(k == 0), stop=(k == 1))
                    S.do("act", "copy", [bp], [bxe], out=xe[:, c, :], in_=ps[:, 0:nslot])
                S.dma("sp", XE[e].rearrange("(c p) s -> p c s", p=128)[:, :, 0:nslot], xe[:], [bxe], [S.B("XE", layer, e)])
                st, bst = str_.next()
                for q in range(0, NLAT, 4):
                    ps, bp = pt2.next()
                    for j in range(4):
                        for s2 in range(2):
                            S.do("pe", "transpose", [bsg], [bp], ps[:, j * 2 + s2, :], sg[:, q + j, s2 * 128:(s2 + 1) * 128], identb[:])
                    S.do("dve", "tensor_copy", [bp], [bst], out=st[:, q:q + 4, :, :], in_=ps[:].rearrange("p (j s) t -> p j s t", s=2))
                S.dma("sp", SELT[:, :, e * 2:(e + 1) * 2, :].rearrange("k p s t -> p k s t"), st[:], [bst], [S.B("SELT", layer, e)])
                if has_ctx:
                    stc_, bstc = stc.next()
                    ps, bp = pt2.next()
                    for k in range(2):
                        S.do("pe", "transpose", [bsgc], [bp], ps[0:CAPC, k, :], sgc_[:, k, :], identb[:])
                    S.do("dve", "tensor_copy", [bp], [bstc], out=stc_[:], in_=ps[0:CAPC, 0:2, :])
                    S.dma("sp", SELTC[:, :, e, :].rearrange("k p t -> p k t"), stc_[:], [bstc], [S.B("SELTC", layer, e)])


def modulate_xnT(K, pname, layer, H, modD, g_row, xnT, b_xnT, identb, ntc):
    S = K.S
    with K.phase(pname) as P:
        A, Bt, bA, bB = load_mod_rows(K, P, modD, layer, 1, g_row)
        ssq = P.sb([128, ntc], F32)
        rstd = P.sb([128, ntc], F32)
        hr = Ring([P.sb([128, D], F32) for _ in range(2)])
        junk = P.sb([128, D], BF16)
        bj = Buf()
        t1 = P.sb([128, D], F32)
        bt1 = Buf()
        xr = Ring([P.sb([128, D], BF16) for _ in range(2)])
        ptr = Ring([P.ps([128, 8, 128], BF16) for _ in range(2)])
        for tc in range(ntc):
            r = 0 if tc < NLAT else 1
            h, bh = hr.next()
            S.dma("sp", h[:], H[tc * 128:(tc + 1) * 128, :], [], [bh])
            bs = Buf()
            rms_rstd(S, P, h[:], bh, junk[:], bj, ssq, rstd, bs, tc, D)
            S.do("dve", "scalar_tensor_tensor", [bh, bs, bA[r]], [bt1], out=t1[:], in0=h[:], scalar=rstd[:, tc:tc + 1], in1=A[r][:],
                 op0=ALU.mult, op1=ALU.mult)
            xt, bx = xr.next()
            S.do("dve", "tensor_tensor", [bt1, bB[r]], [bx], out=xt[:], in0=t1[:], in1=Bt[r][:], op=ALU.add)
            for half in range(2):
                ps, bp = ptr.next()
                for j in range(8):
                    c = half * 8 + j
                    S.do("pe", "transpose", [bx], [bp], ps[:, j, :], xt[:, c * 128:(c + 1) * 128], identb[:])
                S.do("act", "copy", [bp], [b_xnT[tc]], out=xnT[:, half * 8:(half + 1) * 8, tc * 128:(tc + 1) * 128], in_=ps[:])


def phase_ssd(K, layer, H, modD, norm_g, w_in, conv_w, conv_b, dt_bias, a_log, d_skip, ng, w_out, tris_d, ident, identb, scr):
    S = K.S
    nc = K.nc
    ZS, BCT, XS, BTOK, DTS, YB, YT = scr
    ntc = NTC
    ttiles = [(i * 512, 512) for i in range(4)] + [(T, TCX)]
    with ExitStack() as outer:
        xnT = K.sbuf(outer, "xnT_ssd", [128, 16, NTOK], BF16)
        b_xnT = [Buf() for _ in range(ntc)]
        modulate_xnT(K, "s1", layer, H, modD, norm_g[layer, 0:1, :], xnT, b_xnT, identb, ntc)
        with K.phase("s2") as P:
            wr = Ring([P.sb([128, 16, 512], BF16) for _ in range(2)])
            pmm = Ring([P.ps([128, 512], F32) for _ in range(3)])
            ptr = Ring([P.ps([128, 8, 128], BF16) for _ in range(2)])
            pst = P.ps([128, 512], F32)
            b_pst = Buf()
            cwr = P.sb([120, 2, 128], F32)
            cbr = P.sb([48, 128], F32)
            cwT = P.sb([128, 5, 48], F32)
            cbT = P.sb([128, 48], F32)
            b_cwr, b_cw = Buf(), Buf()
            cw_rows = conv_w.rearrange("k (c p) -> (k c) p", p=128)
            for hf in range(2):
                S.dma("sp", cwr[:, hf, :], cw_rows[hf * 120:(hf + 1) * 120, :], [], [b_cwr])
            S.dma("sp", cbr[:], conv_b.rearrange("(c p) -> c p", p=128), [], [b_cwr])
            for hf in range(2):
                S.do("pe", "transpose", [b_cwr], [b_pst], pst[:, hf * 120:(hf + 1) * 120], cwr[:, hf, :], ident[0:120, 0:120])
            S.do("pe", "transpose", [b_cwr], [b_pst], pst[:, 240:288], cbr[:], ident[0:48, 0:48])
            S.do("dve", "tensor_copy", [b_pst], [b_cw], out=cwT[:].rearrange("p k c -> p (k c)"), in_=pst[:, 0:240])
            S.do("dve", "tensor_copy", [b_pst], [b_cw], out=cbT[:], in_=pst[:, 240:288])
            zr = Ring([P.sb([128, 512], BF16) for _ in range(3)])
            for pc in range(8):
                w, bw = wr.next()
                S.dma("pool", w[:], wpiece(w_in, pc * 512, 512), [], [bw])
                for tc in range(NLAT):
                    ps, bp = pmm.next()
                    for c in range(16):
                        S.do("pe", "matmul", [bw, b_xnT[tc]], [bp], ps[:], xnT[:, c, tc * 128:(tc + 1) * 128], w[:, c, :],
                             start=(c == 0), stop=(c == 15))
                    zt, bz = zr.next()
                    S.do("act", "activation", [bp], [bz], out=zt[:], in_=ps[:], func=AF.Silu)
                    S.dma("sp", ZS[tc * 128:(tc + 1) * 128, pc * 512:(pc + 1) * 512], zt[:], [bz], [Buf()])
            rbr = Ring([P.sb([128, NTOK + 8], F32) for _ in range(2)])
            accr = Ring([P.sb([128, NTOK], F32) for _ in range(2)])
            obr = Ring([P.sb([128, NTOK], BF16) for _ in range(2)])
            tsr = Ring([P.sb([128, ntc, 128], BF16) for _ in range(2)])
            for rb, bb in zip(rbr.tiles, rbr.bufs):
                S.do("dve", "memset", [], [bb], rb[:], 0.0)
            LOFF, COFF = 2, T + 6
            for pc in range(12):
                w, bw = wr.next()
                S.dma("pool", w[:], wpiece(w_in, 4096 + pc * 512, 512), [], [bw])
                for oc in range(4):
                    ch = pc * 4 + oc
                    rb, brb = rbr.next()
                    for (t0, tn) in ttiles:
                        tcs = list(range(t0 // 128, (t0 + tn) // 128))
                        ps, bp = pmm.next()
                        for c in range(16):
                            S.do("pe", "matmul", [bw] + [b_xnT[q] for q in tcs], [bp], ps[:, 0:tn], w[:, c, oc * 128:(oc + 1) * 128],
                                 xnT[:, c, t0:t0 + tn], start=(c == 0), stop=(c == 15))
                        off = LOFF + t0 if t0 < T else COFF
                        S.do("act", "copy", [bp], [brb], out=rb[:, off:off + tn], in_=ps[:, 0:tn])
                    acc, bacc = accr.next()
                    for (a0, an, ro) in ((0, T, LOFF), (T, TCX, COFF)):
                        for k in range(5):
                            src = rb[:, ro - 2 + k:ro - 2 + k + an]
                            if k == 0:
                                S.do("dve", "tensor_scalar", [brb, b_cw], [bacc], out=acc[:, a0:a0 + an], in0=src, scalar1=cwT[:, k, ch:ch + 1],
                                     scalar2=None, op0=ALU.mult)
                            else:
                                S.do("dve", "scalar_tensor_tensor", [brb, b_cw, bacc], [bacc], out=acc[:, a0:a0 + an], in0=src,
                                     scalar=cwT[:, k, ch:ch + 1], in1=acc[:, a0:a0 + an], op0=ALU.mult, op1=ALU.add)
                    ob, bob = obr.next()
                    S.do("act", "activation", [bacc, b_cw], [bob], out=ob[:], in_=acc[:], func=AF.Silu, bias=cbT[:, ch:ch + 1], scale=1.0)
                    if ch >= 32:
                        S.dma("sp", BCT[(ch - 32) * 128:(ch - 31) * 128, :], ob[:], [bob], [Buf()])
                    if ch < 40:
                        ts, bts = tsr.next()
                        for q in range(0, ntc, 8):
                            nq = min(8, ntc - q)
                            ps, bp = ptr.next()
                            for j in range(nq):
                                S.do("pe", "transpose", [bob], [bp], ps[:, j, :], ob[:, (q + j) * 128:(q + j + 1) * 128], identb[:])
                            S.do("dve", "tensor_copy", [bp], [bts], out=ts[:, q:q + nq, :], in_=ps[:, 0:nq, :])
                        if ch < 32:
                            S.dma("sp", XS[:, ch * 128:(ch + 1) * 128].rearrange("(k p) n -> p k n", p=128), ts[:], [bts], [Buf()])
                        else:
                            S.dma("sp", BTOK[:, (ch - 32) * 128:(ch - 31) * 128].rearrange("(k p) n -> p k n", p=128), ts[:], [bts], [Buf()])
            wdt = P.sb([128, 16, 128], BF16)
            b_wdt = Buf()
            S.dma("pool", wdt[:], wpiece(w_in, 10240, 128), [], [b_wdt])
            dtb = P.sb([128, 128], F32)
            b_dtb = Buf()
            S.dma("sp", dtb[:], dt_bias.rearrange("a h -> (a h)").unsqueeze(0).broadcast_to([128, 128]), [], [b_dtb])
            xbr = Ring([P.sb([128, 128], F32) for _ in range(2)])
            abr = Ring([P.sb([128, 128], F32) for _ in range(2)])
            for tc in range(ntc):
                ps, bp = pmm.next()
                for c in range(16):
                    S.do("pe", "matmul", [b_wdt, b_xnT[tc]], [bp], ps[:, 0:128], xnT[:, c, tc * 128:(tc + 1) * 128], wdt[:, c, :],
                         start=(c == 0), stop=(c == 15))
                xb, bxb = xbr.next()
                ab, bab = abr.next()
                S.do("dve", "tensor_tensor", [bp, b_dtb], [bxb], out=xb[:], in0=ps[:, 0:128], in1=dtb[:], op=ALU.add)
                S.do("dve", "scalar_tensor_tensor", [bxb], [bab], out=ab[:], in0=xb[:], scalar=-1.0, in1=xb[:], op0=ALU.mult, op1=ALU.max)
                S.do("act", "activation", [bab], [bab], out=ab[:], in_=ab[:], func=AF.Exp, scale=-1.0)
                S.do("act", "activation", [bab], [bab], out=ab[:], in_=ab[:], func=AF.Ln, bias=1.0, scale=1.0)
                S.do("dve", "scalar_tensor_tensor", [bxb, bab], [bxb], out=xb[:], in0=xb[:], scalar=0.0, in1=ab[:], op0=ALU.max, op1=ALU.add)
                S.dma("sp", DTS[tc * 128:(tc + 1) * 128, :], xb[:], [bxb], [Buf()])
    if K.stop_after == "s2":
        return
    with K.phase("s3") as P:
        trf = P.sb([128, 4, 128], F32)
        b_trf = Buf()
        S.dma("sp", trf[:], tris_d.rearrange("f k m -> k f m"), [], [b_trf])
        onef = P.sb([128, 128], F32)
        b_one = Buf()
        S.do("dve", "memset", [], [b_one], onef[:], 1.0)
        LT, LE, GT, GE = 0, 1, 2, 3
        arow = P.sb([128, 128], F32)
        b_arow = Buf()
        S.dma("sp", arow[:], a_log.rearrange("a h -> (a h)").unsqueeze(0).broadcast_to([128, 128]), [], [b_arow])
        S.do("act", "activation", [b_arow], [b_arow], out=arow[:], in_=arow[:], func=AF.Exp)
        S.do("dve", "tensor_scalar", [b_arow], [b_arow], out=arow[:], in0=arow[:], scalar1=-1.0, scalar2=None, op0=ALU.mult)
        drow = P.sb([128, 64], F32)
        b_drow = Buf()
        S.dma("sp", drow[:], d_skip.unsqueeze(0).broadcast_to([128, 64]), [], [b_drow])
        ng32 = P.sb([32, 128], F32)
        ngT = P.sb([128, 32], F32)
        b_ng = Buf()
        S.dma("sp", ng32[:], ng.rearrange("(c p) -> c p", p=128), [], [b_ng])
        xsr = Ring([P.sb([128, 4096], BF16) for _ in range(2)])
        btr = Ring([P.sb([128, 1024], BF16) for _ in range(2)])
        bcr = Ring([P.sb([128, 16, 128], BF16) for _ in range(2)])
        dtr = Ring([P.sb([128, 128], F32) for _ in range(2)])
        dar = Ring([P.sb([128, 64], F32) for _ in range(2)])
        ecr = Ring([P.sb([128, 3, 64], F32) for _ in range(2)])
        xg = P.sb([128, 4096], BF16)
        xgd = P.sb([128, 4096], BF16)
        b_xg, b_xgd = Buf(), Buf()
        Sf = P.sb([128, 8, 512], F32)
        Sb = P.sb([128, 8, 512], BF16)
        b_Sf = [Buf() for _ in range(8)]
        b_Sb = [Buf() for _ in range(8)]
        yar = Ring([P.sb([128, 4096], F32) for _ in range(2)])
        dtri = Ring([P.sb([128, 8, 128], F32) for _ in range(2)])
        Er = Ring([P.sb([128, 8, 128], F32) for _ in range(2)])
        Mr = Ring([P.sb([128, 8, 128], BF16) for _ in range(2)])
        cbr_ = Ring([P.sb([128, 128], F32) for _ in range(2)])
        tmr = Ring([P.sb([128, 512], F32) for _ in range(2)])
        zsr = Ring([P.sb([128, 4096], BF16) for _ in range(1)])
        ybt = P.sb([128, 4096], F32)
        b_ybt = Buf()
        yh = P.sb([128, 4096], BF16)
        b_yh = Buf()
        ytr = Ring([P.sb([128, 32, 128], BF16) for _ in range(2)])
        junk = P.sb([128, 4096], BF16)
        bj = Buf()
        ssq = P.sb([128, NLAT], F32)
        rstd = P.sb([128, NLAT], F32)
        p_c = P.ps([128, 3, 64], F32)
        p_cb = P.ps([128, 128], F32)
        p_seg = [P.ps([128, 4, 128], F32) for _ in range(2)]
        p_y = P.ps([128, 512], F32)
        p_y2 = P.ps([128, 512], F32)
        p_st = P.ps([128, 512], F32)
        p_t = P.ps([128, 8, 128], BF16)
        b_pc, b_pcb, b_pseg, b_py, b_py2, b_pst, b_pt = Buf(), Buf(), [Buf(), Buf()], Buf(), Buf(), Buf(), Buf()
        S.do("pe", "transpose", [b_ng], [b_py], p_y[:, 0:32], ng32[:], ident[0:32, 0:32])
        S.do("dve", "tensor_copy", [b_py], [b_ng], out=ngT[:], in_=p_y[:, 0:32])
        for d in (1, 0):
            Lm, Rt, CBm, cI, cA = (GT, LE, LE, LE, GT) if d == 0 else (LT, GE, GE, GE, LT)
            for g in range(8):
                S.do("dve", "memset", [], [b_Sf[g]], Sf[:, g, :], 0.0)
                S.do("pool", "memset", [], [b_Sb[g]], Sb[:, g, :], 0.0)
            order = [16, 17] + list(range(NLAT)) if d == 0 else [17, 16] + list(range(NLAT - 1, -1, -1))
            for tc in order:
                lat = tc < NLAT
                xs, bxs = xsr.next()
                S.dma("sp", xs[:], XS[tc * 128:(tc + 1) * 128, :], [], [bxs])
                bt, bbt = btr.next()
                S.dma("sp", bt[:], BTOK[tc * 128:(tc + 1) * 128, :], [], [bbt])
                dt_, bdt = dtr.next()
                S.dma("sp", dt_[:], DTS[tc * 128:(tc + 1) * 128, :], [], [bdt])
                if lat:
                    bc, bbc = bcr.next()
                    S.dma("sp", bc[:], BCT[:, tc * 128:(tc + 1) * 128].rearrange("(j p) t -> p j t", p=128), [], [bbc])
                da, bda = dar.next()
                S.do("dve", "tensor_tensor", [bdt, b_arow], [bda], out=da[:], in0=dt_[:, d * 64:(d + 1) * 64], in1=arow[:, d * 64:(d + 1) * 64],
                     op=ALU.mult)
                S.do("pe", "matmul", [bda, b_trf], [b_pc], p_c[:, 0, :], trf[:, cI, :], da[:], start=True, stop=True)
                S.do("pe", "matmul", [bda, b_trf], [b_pc], p_c[:, 1, :], trf[:, cA, :], da[:], start=True, stop=True)
                S.do("pe", "matmul", [bda, b_one], [b_pc], p_c[:, 2, :], onef[:], da[:], start=True, stop=True)
                ec, bec = ecr.next()
                S.do("act", "activation", [b_pc], [bec], out=ec[:], in_=p_c[:], func=AF.Exp)
                dtv = dt_[:, d * 64:(d + 1) * 64]
                S.do("dve", "tensor_tensor", [bxs, bdt], [b_xg], out=xg[:].rearrange("p (h q) -> p h q", h=64),
                     in0=xs[:].rearrange("p (h q) -> p h q", h=64), in1=dtv.unsqueeze(2).to_broadcast([128, 64, 64]), op=ALU.mult)
                S.do("pool", "tensor_tensor", [b_xg, bec], [b_xgd], out=xgd[:].rearrange("p (h q) -> p h q", h=64),
                     in0=xg[:].rearrange("p (h q) -> p h q", h=64), in1=ec[:, 1, :].unsqueeze(2).to_broadcast([128, 64, 64]), op=ALU.mult)
                if lat:
                    ya, bya = yar.next()
                for g in range(8):
                    if lat:
                        S.do("pe", "matmul", [bbc], [b_pcb], p_cb[:], bc[:, g, :], bc[:, 8 + g, :], start=True, stop=True)
                        cb, bcb = cbr_.next()
                        S.do("dve", "tensor_tensor", [b_pcb, b_trf], [bcb], out=cb[:], in0=p_cb[:], in1=trf[:, CBm, :], op=ALU.mult)
                        dtt, bdtt = dtri.next()
                        S.do("pool", "tensor_tensor", [b_trf, bda], [bdtt], out=dtt[:], in0=trf[:, Rt:Rt + 1, :].to_broadcast([128, 8, 128]),
                             in1=da[:, g * 8:(g + 1) * 8].unsqueeze(2).to_broadcast([128, 8, 128]), op=ALU.mult)
                        for hh in range(2):
                            S.do("pe", "matmul", [bdtt, b_trf], [b_pseg[hh]], p_seg[hh][:].rearrange("p a b -> p (a b)"), trf[:, Lm, :],
                                 dtt[:, hh * 4:(hh + 1) * 4, :].rearrange("p a b -> p (a b)"), start=True, stop=True)
                        E, bE = Er.next()
                        for hh in range(2):
                            S.do("act", "activation", [b_pseg[hh]], [bE], out=E[:, hh * 4:(hh + 1) * 4, :], in_=p_seg[hh][:], func=AF.Exp)
                        M, bM = Mr.next()
                        S.do("dve", "tensor_tensor", [bE, bcb], [bM], out=M[:], in0=E[:], in1=cb[:].unsqueeze(1).to_broadcast([128, 8, 128]),
                             op=ALU.mult)
                        for h in range(8):
                            hd = g * 8 + h
                            S.do("pe", "matmul", [bM, b_xg], [b_py], p_y[:, h * 64:(h + 1) * 64], M[:, h, :], xg[:, hd * 64:(hd + 1) * 64],
                                 start=True, stop=True)
                        S.do("pe", "matmul", [bbc, b_Sb[g]], [b_py2], p_y2[:], bc[:, 8 + g, :], Sb[:, g, :], start=True, stop=True)
                        tm, btm = tmr.next()
                        S.do("dve", "tensor_tensor", [b_py2, bec], [btm], out=tm[:].rearrange("p (h q) -> p h q", h=8),
                             in0=p_y2[:].rearrange("p (h q) -> p h q", h=8),
                             in1=ec[:, 0, g * 8:(g + 1) * 8].unsqueeze(2).to_broadcast([128, 8, 64]), op=ALU.mult)
                        S.do("dve", "tensor_tensor", [btm, b_py], [bya], out=ya[:, g * 512:(g + 1) * 512], in0=tm[:], in1=p_y[:], op=ALU.add)
                    S.do("pe", "matmul", [bbt, b_xgd], [b_pst], p_st[:], bt[:, g * 128:(g + 1) * 128], xgd[:, g * 512:(g + 1) * 512],
                         start=True, stop=True)
                    S.do("pool", "tensor_tensor", [b_Sf[g], bec], [b_Sf[g]], out=Sf[:, g, :].rearrange("p (h q) -> p h q", h=8),
                         in0=Sf[:, g, :].rearrange("p (h q) -> p h q", h=8),
                         in1=ec[:, 2, g * 8:(g + 1) * 8].unsqueeze(2).to_broadcast([128, 8, 64]), op=ALU.mult)
                    S.do("dve", "tensor_tensor", [b_Sf[g], b_pst], [b_Sf[g]], out=Sf[:, g, :], in0=Sf[:, g, :], in1=p_st[:], op=ALU.add)
                    S.do("act", "copy", [b_Sf[g]], [b_Sb[g]], out=Sb[:, g, :], in_=Sf[:, g, :])
                if not lat:
                    continue
                if d == 1:
                    S.dma("sp", YB[tc * 128:(tc + 1) * 128, :], ya[:], [bya], [S.B("YB", tc)])
                    continue
                S.dma("sp", ybt[:], YB[tc * 128:(tc + 1) * 128, :], [S.B("YB", tc)], [b_ybt])
                S.do("pool", "tensor_tensor", [bya, b_ybt], [bya], out=ya[:], in0=ya[:], in1=ybt[:], op=ALU.add)
                S.do("dve", "tensor_tensor", [bxs, b_drow], [b_ybt], out=ybt[:].rearrange("p (h q) -> p h q", h=64),
                     in0=xs[:].rearrange("p (h q) -> p h q", h=64), in1=drow[:].unsqueeze(2).to_broadcast([128, 64, 64]), op=ALU.mult)
                S.do("pool", "tensor_tensor", [bya, b_ybt], [bya], out=ya[:], in0=ya[:], in1=ybt[:], op=ALU.add)
                zs, bzs = zsr.next()
                S.dma("sp", zs[:], ZS[tc * 128:(tc + 1) * 128, :], [], [bzs])
                S.do("dve", "tensor_tensor", [bya, bzs], [bya], out=ya[:], in0=ya[:], in1=zs[:], op=ALU.mult)
                bs = Buf()
                rms_rstd(S, P, ya[:], bya, junk[:], bj, ssq, rstd, bs, tc, 4096)
                S.do("dve", "tensor_scalar", [bya, bs], [b_yh], out=yh[:], in0=ya[:], scalar1=rstd[:, tc:tc + 1], scalar2=None, op0=ALU.mult)
                yt, byt = ytr.next()
                for q in range(4):
                    for j in range(8):
                        c = q * 8 + j
                        S.do("pe", "transpose", [b_yh], [b_pt], p_t[:, j, :], yh[:, c * 128:(c + 1) * 128], identb[:])
                    S.do("dve", "tensor_tensor", [b_pt, b_ng], [byt], out=yt[:, q * 8:(q + 1) * 8, :], in0=p_t[:],
                         in1=ngT[:, q * 8:(q + 1) * 8].unsqueeze(2).to_broadcast([128, 8, 128]), op=ALU.mult)
                S.dma("sp", YT[:, tc * 128:(tc + 1) * 128].rearrange("(c p) t -> p c t", p=128), yt[:], [byt], [Buf()])
    if K.stop_after == "s3":
        return
    phase_outproj(K, layer, YT, 32, w_out, H, modD, NLAT)


def phase_final(K, H, final_g, out):
    S = K.S
    with K.phase("final") as P:
        gt = P.sb([128, D], F32)
        bg = Buf()
        S.dma("sp", gt[:], final_g.unsqueeze(0).broadcast_to([128, D]), [], [bg])
        hr = Ring([P.sb([128, D], F32) for _ in range(3)])
        junk = P.sb([128, D], BF16)
        bj = Buf()
        ssq = P.sb([128, NLAT], F32)
        rstd = P.sb([128, NLAT], F32)
        for tc in range(NLAT):
            h, bh = hr.next()
            S.dma("sp", h[:], H[tc * 128:(tc + 1) * 128, :], [], [bh])
            bs = Buf()
            rms_rstd(S, P, h[:], bh, junk[:], bj, ssq, rstd, bs, tc, D)
            S.do("dve", "scalar_tensor_tensor", [bh, bs, bg], [bh], out=h[:], in0=h[:], scalar=rstd[:, tc:tc + 1], in1=gt[:],
                 op0=ALU.mult, op1=ALU.mult)
            S.dma("sp", out[tc * 128:(tc + 1) * 128, :], h[:], [bh], [Buf()])
    K.out_written = True


def phase_moe_post(K, layer, has_ctx, Hin, H, modD, SELT, SELTC, YEo, jb):
    S = K.S
    ntc = NTC if has_ctx else NLAT
    with K.phase(f"e3{layer}") as P:
        nr = 2 if has_ctx else 1
        G, bG = load_gate_rows(K, P, modD, layer, 2, nr)
        yr = Ring([P.sb([128, 2 * NE, 512], BF16) for _ in range(2)])
        ycr = Ring([P.sb([CAPC, NE, 512], BF16) for _ in range(2)])
        sr = Ring([P.sb([128, 2 * NE, 128], BF16) for _ in range(3)])
        scr_ = Ring([P.sb([CAPC, NE, 128], BF16) for _ in range(2)])
        pm = Ring([P.ps([128, 512], F32) for _ in range(3)])
        hr = Ring([P.sb([128, 512], F32) for _ in range(3)])
        tr = Ring([P.sb([128, 512], F32) for _ in range(3)])
        for n in range(4):
            y, by = yr.next()
            S.dma("sp", y[:].rearrange("p (e s) c -> p e s c", s=2), YEo[:, jb, n, :, 0:2, :].rearrange("e p s c -> p e s c"), [], [by])
            if has_ctx:
                yc, byc = ycr.next()
                S.dma("sp", yc[:], YEo[:, jb, n, 0:CAPC, 2, :].rearrange("e p c -> p e c"), [], [byc])
            for tc in range(ntc):
                r = 0 if tc < NLAT else 1
                ps, bp = pm.next()
                if tc < NLAT:
                    st, bst = sr.next()
                    S.dma("act", st[:], SELT[tc], [], [bst])
                    for j in range(2 * NE):
                        S.do("pe", "matmul", [bst, by], [bp], ps[:], st[:, j, :], y[:, j, :], start=(j == 0), stop=(j == 2 * NE - 1))
                else:
                    st, bst = scr_.next()
                    S.dma("act", st[:], SELTC[tc - NLAT], [], [bst])
                    for j in range(NE):
                        S.do("pe", "matmul", [bst, byc], [bp], ps[:], st[:, j, :], yc[:, j, :], start=(j == 0), stop=(j == NE - 1))
                ht, bh = hr.next()
                hb = S.B("H3", tc, n)
                S.dma("sp", ht[:], Hin[tc * 128:(tc + 1) * 128, n * 512:(n + 1) * 512], [hb], [bh])
                tt, bt = tr.next()
                S.do("dve", "tensor_tensor", [bp, bG[r]], [bt], out=tt[:], in0=ps[:], in1=G[r][:, n * 512:(n + 1) * 512], op=ALU.mult)
                S.do("pool", "tensor_tensor", [bt, bh], [bh], out=ht[:], in0=tt[:], in1=ht[:], op=ALU.add)
                S.dma("sp", H[tc * 128:(tc + 1) * 128, n * 512:(n + 1) * 512], ht[:], [bh], [hb])


def phase_experts(K, XEget, wg, wu, wd, YEo, nslot, has_ctx, n_exp=2, NB=8):
    S = K.S
    identb = K.identb
    with K.phase("e2") as P:
        xe = P.sb([128, NB, 16, nslot], BF16)
        hid = P.sb([128, NB, 16, nslot], BF16)
        b_xe = [Buf() for _ in range(NB)]
        b_hid = [Buf() for _ in range(NB)]
        wr = Ring([P.sb([128, 16, 512], BF16) for _ in range(4)])
        sgr = Ring([P.sb([128, 512], F32) for _ in range(2)])
        htr = Ring([P.sb([128, 512], BF16) for _ in range(2)])
        yer = Ring([P.sb([128, 3, 512], BF16) for _ in range(2)])
        pgu = Ring([P.ps([128, 512], F32) for _ in range(4)])
        ptr = Ring([P.ps([128, 4, 128], BF16) for _ in range(2)])
        pdn = Ring([P.ps([128, 512], F32) for _ in range(2)])
        chunks = [(0, 128), (128, 128)] + ([(256, CAPC)] if has_ctx else [])
        for el in range(n_exp):
            for b in range(NB):
                S.dma("sp", xe[:, b, :, :], XEget(el, b).rearrange("(c p) s -> p c s", p=128)[:, :, 0:nslot], [], [b_xe[b]])
            for fp in range(4):
                wgt, bwg = wr.next()
                S.dma("pool", wgt[:], wpiece(wg[el], fp * 512, 512), [], [bwg])
                wut, bwu = wr.next()
                S.dma("pool", wut[:], wpiece(wu[el], fp * 512, 512), [], [bwu])
                for b in range(NB):
                    for si, (s0, sn) in enumerate(chunks):
                        psg, bpg = pgu.next()
                        for c in range(16):
                            S.do("pe", "matmul", [bwg, b_xe[b]], [bpg], psg[0:sn, :], xe[:, b, c, s0:s0 + sn], wgt[:, c, :],
                                 start=(c == 0), stop=(c == 15))
                        psu, bpu = pgu.next()
                        for c in range(16):
                            S.do("pe", "matmul", [bwu, b_xe[b]], [bpu], psu[0:sn, :], xe[:, b, c, s0:s0 + sn], wut[:, c, :],
                                 start=(c == 0), stop=(c == 15))
                        sgt, bsg = sgr.next()
                        S.do("act", "activation", [bpg], [bsg], out=sgt[0:sn, :], in_=psg[0:sn, :], func=AF.Silu)
                        ht, bht = htr.next()
                        S.do("dve", "tensor_tensor", [bsg, bpu], [bht], out=ht[0:sn, :], in0=sgt[0:sn, :], in1=psu[0:sn, :], op=ALU.mult)
                        pt, bpt = ptr.next()
                        for j in range(4):
                            S.do("pe", "transpose", [bht], [bpt], pt[:, j, 0:sn], ht[0:sn, j * 128:(j + 1) * 128], identb[0:sn, 0:sn])
                        S.do("act" if si % 2 else "dve", "copy" if si % 2 else "tensor_copy", [bpt], [b_hid[b]],
                             out=hid[:, b, fp * 4:(fp + 1) * 4, s0:s0 + sn], in_=pt[:, :, 0:sn])
            for n in range(4):
                wdt, bwd = wr.next()
                S.dma("pool", wdt[:], wpiece(wd[el], n * 512, 512), [], [bwd])
                for b in range(NB):
                    ye, bye = yer.next()
                    for si, (s0, sn) in enumerate(chunks):
                        ps, bp = pdn.next()
                        for fc in range(16):
                            S.do("pe", "matmul", [bwd, b_hid[b]], [bp], ps[0:sn, :], hid[:, b, fc, s0:s0 + sn], wdt[:, fc, :],
                                 start=(fc == 0), stop=(fc == 15))
                        S.do("act" if si % 2 else "dve", "copy" if si % 2 else "tensor_copy", [bp], [bye], out=ye[0:sn, si, :], in_=ps[0:sn, :])
                    S.dma("sp", YEo[el, b, n, :, 0:2, :], ye[:, 0:2, :], [bye], [Buf()])
                    if has_ctx:
                        S.dma("sp", YEo[el, b, n, 0:CAPC, 2, :], ye[0:CAPC, 2, :], [bye], [Buf()])


MODC = 6 * D // 8


def phase_mod(K, cc9, mod_w, mod_b, modp, ident, nrows=9, ncols=MODC):
    S = K.S
    with K.phase("mod") as P:
        cc = P.sb([nrows, D], F32)
        scT = P.sb([128, 16, nrows], BF16)
        ps_t = P.ps([128, 16, nrows], F32)
        b_cc, b_ps, b_sc = Buf(), Buf(), Buf()
        S.dma("sp", cc[:], cc9[:, :], [], [b_cc])
        S.do("act", "activation", [b_cc], [b_cc], out=cc[:], in_=cc[:], func=AF.Silu)
        for c in range(16):
            S.do("pe", "transpose", [b_cc], [b_ps], ps_t[:, c, :], cc[:, c * 128:(c + 1) * 128], ident[0:nrows, 0:nrows])
        S.do("dve", "tensor_copy", [b_ps], [b_sc], out=scT[:], in_=ps_t[:])
        wr = Ring([P.sb([128, 16, 512], BF16) for _ in range(3)])
        pr = Ring([P.ps([nrows, 512], F32) for _ in range(2)])
        br = Ring([P.sb([nrows, 512], F32) for _ in range(2)])
        orr = Ring([P.sb([nrows, 512], F32) for _ in range(2)])
        for layer in range(2):
            for n in range(ncols // 512):
                w, bw = wr.next()
                S.dma("pool", w[:], wpiece(mod_w[layer], n * 512, 512), [], [bw])
                bt, bb = br.next()
                S.dma("sp", bt[:], mod_b[layer:layer + 1, n * 512:(n + 1) * 512].broadcast_to([nrows, 512]), [], [bb])
                ps, bp = pr.next()
                for c in range(16):
                    S.do("pe", "matmul", [bw, b_sc], [bp], ps[:], scT[:, c, :], w[:, c, :], start=(c == 0), stop=(c == 15))
                ot, bo = orr.next()
                S.do("dve", "tensor_tensor", [bp, bb], [bo], out=ot[:], in0=ps[:], in1=bt[:], op=ALU.add)
                S.dma("sp", modp[layer, :, n * 512:(n + 1) * 512], ot[:], [bo], [Buf()])


NB = 1
NCORES = 8 // NB


def build(dbg=(), stop_after=None):
    K = KB(dbg, stop_after)
    nc = K.nc
    innames = []

    def inp(name, shape, dt=F32):
        innames.append(name)
        return K.inp(name, shape, dt)

    ident_d = inp("ident", [128, 128])
    x = inp("x", [NB, T, D])
    ctx = inp("ctx", [NB, TCX, D])
    cc = inp("cc", [NB + 1, D])
    mod_w = inp("mod_w", [2, D, 6 * D])
    mod_b = inp("mod_b", [2, 6 * D])
    norm_g = inp("norm_g", [2, 2, D])
    final_g = inp("final_g", [D])
    ab_w_in = inp("ab_w_in", [1, D, 3072])
    ab_w_out = inp("ab_w_out", [1, D, D])
    gm_v_g = inp("gm_v_g", [1, 1024])
    gm_w_s = inp("gm_w_s", [1, 8, 128, 128])
    gm_b_s = inp("gm_b_s", [1, 8, 128])
    w_in = inp("ssd_w_in", [1, D, 10368])
    conv_w = inp("ssd_conv_w", [1, 5, 6144])
    conv_b = inp("ssd_conv_b", [1, 6144])
    dt_bias = inp("ssd_dt_bias", [1, 2, 64])
    a_log = inp("ssd_a_log", [1, 2, 64])
    d_skip = inp("ssd_d", [1, 64])
    ng = inp("ssd_norm_g", [1, 4096])
    w_out = inp("ssd_w_out", [1, 4096, D])
    w_router = inp("moe_w_router", [2, D, NE])
    w_gate = inp("moe_w_gate", [2, NE, D, D])
    w_up = inp("moe_w_up", [2, NE, D, D])
    w_down = inp("moe_w_down", [2, NE, D, D])
    pos = inp("pos", [T, D])
    cs128_d = inp("cs128", [128, 256], BF16)
    dftT_d = inp("dftT", [2, T, T], BF16)
    dftC_d = inp("dftC", [2, TCX, TCX], BF16)
    tris_d = inp("tris", [4, 128, 128])
    iota_d = inp("iota", [128, NSLOT])
    out = nc.dram_tensor("out", [NB, T, D], F32, kind="ExternalOutput").ap()
    modD = K.scratch("modD", [2, NB + 1, 6 * D])
    es = K.es
    with es:
        sems = {e: es.enter_context(nc.semaphore("s_" + e)) for e in ENGS}
        rings = {e: [es.enter_context(nc.semaphore(f"r_{e}{i}")) for i in range(NRING)] for e in ENGS}
        S = K.S = Sched(nc, sems, rings)
        ident = es.enter_context(nc.sbuf_tensor("ident_sb", [128, 128], F32))
        identb = es.enter_context(nc.sbuf_tensor("identb_sb", [128, 128], BF16))
        K.identb = identb
        with K.phase("c0") as P:
            bi = Buf()
            S.dma("sp", ident[:], ident_d[:, :], [], [bi])
            S.do("dve", "tensor_copy", [bi], [Buf()], out=identb[:], in_=ident[:])
        phase_mod(K, cc, mod_w, mod_b, modD, ident, nrows=NB + 1, ncols=6 * D)
        Hs, XE0, XE1, SELT0, SELT1, SELTC0 = [], [], [], [], [], []
        for j in range(NB):
            K.sfx = f"_b{j}"
            K.rows = (j, NB)
            Hs.append(K.scratch("H", [NTOK, D]))
            XE0.append(K.scratch("XE0", [NE, D, NSLOT], BF16))
            SELT0.append(K.scratch("SELT0", [NLAT, 128, 2 * NE, 128], BF16))
            SELTC0.append(K.scratch("SELTC0", [2, CAPC, NE, 128], BF16))
            XE1.append(K.scratch("XE1", [NE, D, CAP], BF16))
            SELT1.append(K.scratch("SELT1", [NLAT, 128, 2 * NE, 128], BF16))
            catT = K.scratch("catT", [D, NTOK], BF16)
            P12 = K.scratch("P12", [8, NTOK, 256], BF16)
            phase_ab(K, 0, x[j], ctx[j], pos, Hs[j], modD, norm_g, ab_w_in[0], ab_w_out[0], gm_v_g[0], gm_w_s[0], gm_b_s[0],
                     cs128_d, dftT_d, dftC_d, catT, P12, ident, identb)
            phase_moe_pre(K, 0, True, Hs[j], modD, norm_g, w_router[0], tris_d, iota_d, ident, identb, XE0[j], SELT0[j], SELTC0[j])
        K.sfx = ""
        YE0 = K.scratch("YE0", [NE, NB, 4, 128, 3, 512], BF16)
        phase_experts(K, lambda e, j: XE0[j][e], w_gate[0], w_up[0], w_down[0], YE0, NSLOT, True, n_exp=NE, NB=NB)
        for j in range(NB):
            K.sfx = f"_b{j}"
            K.rows = (j, NB)
            phase_moe_post(K, 0, True, Hs[j], Hs[j], modD, SELT0[j], SELTC0[j], YE0, j)
            scr = (K.scratch("ZS", [T, 4096], BF16), K.scratch("BCT", [2048, NTOK], BF16), K.scratch("XS", [NTOK, 4096], BF16),
                   K.scratch("BTOK", [NTOK, 1024], BF16), K.scratch("DTS", [NTOK, 128]), K.scratch("YB", [T, 4096]),
                   K.scratch("YT", [4096, T], BF16))
            phase_ssd(K, 1, Hs[j], modD, norm_g, w_in[0], conv_w[0], conv_b[0], dt_bias[0], a_log[0], d_skip[0], ng[0], w_out[0],
                      tris_d, ident, identb, scr)
            phase_moe_pre(K, 1, False, Hs[j], modD, norm_g, w_router[1], tris_d, iota_d, ident, identb, XE1[j], SELT1[j], None)
        K.sfx = ""
        YE1 = K.scratch("YE1", [NE, NB, 4, 128, 2, 512], BF16)
        phase_experts(K, lambda e, j: XE1[j][e], w_gate[1], w_up[1], w_down[1], YE1, CAP, False, n_exp=NE, NB=NB)
        for j in range(NB):
            K.rows = (j, NB)
            phase_moe_post(K, 1, False, Hs[j], Hs[j], modD, SELT1[j], None, YE1, j)
            phase_final(K, Hs[j], final_g, out[j])
    nc._inames = innames
    return nc


_CONST = {}


def host_constants():
    if _CONST:
        return _CONST
    bf = ml_dtypes.bfloat16
    rows, cols, dim = T // 64, 64, D
    quarter = dim // 4
    omega = (1.0 / (np.float32(10000.0) ** (np.arange(quarter, dtype=np.float32) / np.float32(quarter)))).astype(np.float32)
    r = np.repeat(np.arange(rows, dtype=np.float32), cols)[:, None] * omega
    cl = np.tile(np.arange(cols, dtype=np.float32), rows)[:, None] * omega
    _CONST["pos"] = np.concatenate([np.sin(r), np.cos(r), np.sin(cl), np.cos(cl)], axis=-1).astype(np.float32)
    _CONST["ident"] = np.eye(128, dtype=np.float32)

    def dft(n):
        k = np.arange(n, dtype=np.int64)
        ang = 2.0 * np.pi * ((k[:, None] * k[None, :]) % n).astype(np.float64) / n
        return np.cos(ang), np.sin(ang)

    c128, s128 = dft(128)
    _CONST["cs128"] = np.concatenate([c128, s128], axis=1).astype(np.float32).astype(bf)
    cT, sT = dft(T)
    _CONST["dftT"] = np.stack([cT, -sT]).astype(np.float32).astype(bf)
    cC, sC = dft(TCX)
    _CONST["dftC"] = np.stack([cC, -sC]).astype(np.float32).astype(bf)
    k = np.arange(128)
    kk, mm = k[:, None], k[None, :]
    _CONST["tris"] = np.stack([kk < mm, kk <= mm, kk > mm, kk >= mm]).astype(np.float32)
    _CONST["iota"] = np.tile(np.arange(NSLOT, dtype=np.float32)[None, :], (128, 1))
    return _CONST


_NC_CACHE = {}
_SHARED = ("mod_w", "mod_b", "norm_g", "final_g", "ab_w_in", "ab_w_out", "gm_v_g", "gm_w_s", "gm_b_s", "ssd_w_in", "ssd_conv_w",
           "ssd_conv_b", "ssd_dt_bias", "ssd_a_log", "ssd_d", "ssd_norm_g", "ssd_w_out", "moe_w_router", "moe_w_gate", "moe_w_up",
           "moe_w_down")


def make_in_maps(inputs, cores):
    cst = host_constants()
    shared = {k: np.ascontiguousarray(inputs[k]) for k in _SHARED}
    maps = []
    for k in cores:
        m = dict(shared)
        m.update(cst)
        m["x"] = np.ascontiguousarray(inputs["x"][k * NB:(k + 1) * NB])
        m["ctx"] = np.ascontiguousarray(inputs["ctx"][k * NB:(k + 1) * NB])
        m["cc"] = np.ascontiguousarray(np.concatenate([inputs["c"][k * NB:(k + 1) * NB], inputs["c_ctx"][None, :]], axis=0))
        maps.append(m)
    return maps


def kernel(**inputs):
    inputs = {k: np.asarray(v) for k, v in inputs.items()}
    if "nc" not in _NC_CACHE:
        _NC_CACHE["nc"] = build()
    nc = _NC_CACHE["nc"]
    maps = make_in_maps(inputs, range(NCORES))
    res = run_bass_kernel_spmd(nc, maps, core_ids=list(range(NCORES)))
    return np.concatenate([np.asarray(r["out"]) for r in res.results], axis=0).astype(np.float32)
```
